# Optimizing a Trainium2 kernel written in Bass

```python
import jax, jax.numpy as jnp
from jax import lax
import numpy as np

D_MODEL = 1024
BATCH = 32
SEQ = 2048
DEPTH = 1

HEAD_DIM = 64
FOX_HEADS = 8
NSA_HEADS = 8
NSA_KV_GROUPS = 2
NSA_HEADS_PER_GROUP = NSA_HEADS // NSA_KV_GROUPS
FOX_WIDTH = FOX_HEADS * HEAD_DIM
NSA_WIDTH = NSA_HEADS * HEAD_DIM
NSA_KV_WIDTH = NSA_KV_GROUPS * HEAD_DIM
D_FF = 4 * D_MODEL
ROPE_THETA = 10000.0
Q_BLOCK = 128
CMP_BLOCK = 32
CMP_STRIDE = 16
SEL_BLOCK = 64
SEL_TOPN = 8
SEL_Q_BLOCK = 64
WINDOW = 512
RMS_EPS = 1e-6
NEG_INF = -1e30
FORCED_BONUS = 1e4
FORGET_BIAS_INIT = 2.0
ATTN_SCALE = HEAD_DIM ** -0.5
IN_SPLITS = (FOX_WIDTH, FOX_WIDTH, FOX_WIDTH, FOX_HEADS,
             NSA_WIDTH, NSA_KV_WIDTH, NSA_KV_WIDTH, NSA_KV_WIDTH, NSA_KV_WIDTH, NSA_KV_WIDTH, NSA_KV_WIDTH,
             3 * NSA_HEADS, D_MODEL, D_MODEL)
IN_COLS = 3 * FOX_WIDTH + FOX_HEADS + NSA_WIDTH + 6 * NSA_KV_WIDTH + 3 * NSA_HEADS + 2 * D_MODEL

kernel_name = 'fox_nsa_gated_hybrid_block'


def _rms_norm(x, g):
    xf = x.astype(jnp.float32)
    y = xf * lax.rsqrt(jnp.mean(xf * xf, axis=-1, keepdims=True) + RMS_EPS)
    return (y * g.astype(jnp.float32)).astype(x.dtype)


def _heads(t, n):
    b, s, _ = t.shape
    return t.reshape(b, s, n, HEAD_DIM).transpose(0, 2, 1, 3)


def _rope(t):
    s, d = t.shape[2], t.shape[3]
    inv = jnp.power(jnp.float32(ROPE_THETA), -jnp.arange(0, d, 2, dtype=jnp.float32) / d)
    ang = jnp.arange(s, dtype=jnp.float32)[:, None] * inv[None, :]
    cos, sin = jnp.cos(ang), jnp.sin(ang)
    tf = t.astype(jnp.float32)
    t1, t2 = tf[..., : d // 2], tf[..., d // 2:]
    return jnp.concatenate([t1 * cos - t2 * sin, t2 * cos + t1 * sin], axis=-1).astype(t.dtype)


def _masked_softmax(s, mask):
    s = jnp.where(mask, s, NEG_INF)
    m = jnp.max(s, axis=-1, keepdims=True)
    p = jnp.where(mask, jnp.exp(s - m), 0.0)
    return p / jnp.maximum(jnp.sum(p, axis=-1, keepdims=True), 1e-30)


def _forgetting_attention(q, k, v, f_logit):
    b, h, s, d = q.shape
    nb = s // Q_BLOCK
    c = jnp.cumsum(jax.nn.log_sigmoid(f_logit.astype(jnp.float32)), axis=1).transpose(0, 2, 1)
    kf, vf = k.astype(jnp.float32), v.astype(jnp.float32)
    qb = q.astype(jnp.float32).reshape(b, h, nb, Q_BLOCK, d).transpose(2, 0, 1, 3, 4)
    cb = c.reshape(b, h, nb, Q_BLOCK).transpose(2, 0, 1, 3)
    k_pos = jnp.arange(s)

    def one_block(args):
        i, q_i, c_i = args
        sc = jnp.einsum('bhqd,bhkd->bhqk', q_i, kf) * ATTN_SCALE + c_i[..., None] - c[:, :, None, :]
        q_pos = i * Q_BLOCK + jnp.arange(Q_BLOCK)
        mask = k_pos[None, :] <= q_pos[:, None]
        p = _masked_softmax(sc, mask)
        return jnp.einsum('bhqk,bhkd->bhqd', p, vf)

    out = lax.map(one_block, (jnp.arange(nb), qb, cb))
    out = out.transpose(1, 0, 3, 2, 4).reshape(b, s, h * d)
    return out.astype(q.dtype)


def _compress(t, pos, w1, w2):
    s = t.shape[2]
    n_cmp = (s - CMP_BLOCK) // CMP_STRIDE + 1
    idx = jnp.arange(n_cmp)[:, None] * CMP_STRIDE + jnp.arange(CMP_BLOCK)[None, :]
    blocks = t[:, :, idx, :] + pos
    flat = blocks.reshape(blocks.shape[0], blocks.shape[1], n_cmp, CMP_BLOCK * HEAD_DIM)
    return jax.nn.silu(flat @ w1) @ w2


def _selected_attention(qg, k, v, idx):
    b, g, hg, s, d = qg.shape
    n_sel = s // SEL_BLOCK
    n_top = idx.shape[-1]
    nqb = s // SEL_Q_BLOCK
    ksb = k.reshape(b, g, n_sel, SEL_BLOCK, d)
    vsb = v.reshape(b, g, n_sel, SEL_BLOCK, d)
    qb = qg.reshape(b, g, hg, nqb, SEL_Q_BLOCK, d).transpose(3, 0, 1, 2, 4, 5)
    ib = idx.reshape(b, g, nqb, SEL_Q_BLOCK, n_top).transpose(2, 0, 1, 3, 4)
    bi = jnp.arange(b)[:, None, None, None]
    gi = jnp.arange(g)[None, :, None, None]

    def one_block(args):
        i, q_i, idx_i = args
        kg = ksb[bi, gi, idx_i]
        vg = vsb[bi, gi, idx_i]
        sc = jnp.einsum('bghqd,bgqnld->bghqnl', q_i, kg) * ATTN_SCALE
        key_pos = idx_i[..., None] * SEL_BLOCK + jnp.arange(SEL_BLOCK)
        q_pos = i * SEL_Q_BLOCK + jnp.arange(SEL_Q_BLOCK)
        mask = key_pos <= q_pos[None, None, :, None, None]
        m = n_top * SEL_BLOCK
        p = _masked_softmax(sc.reshape(b, g, hg, SEL_Q_BLOCK, m),
                            mask.reshape(b, g, 1, SEL_Q_BLOCK, m))
        return jnp.einsum('bghqm,bgqmd->bghqd', p, vg.reshape(b, g, SEL_Q_BLOCK, m, d))

    out = lax.map(one_block, (jnp.arange(nqb), qb, ib))
    return out.transpose(1, 2, 3, 0, 4, 5).reshape(b, g, hg, s, d)


def _window_attention(qg, k, v):
    b, g, hg, s, d = qg.shape
    nb = s // Q_BLOCK
    span = WINDOW + Q_BLOCK
    kp = jnp.pad(k, ((0, 0), (0, 0), (WINDOW, 0), (0, 0)))
    vp = jnp.pad(v, ((0, 0), (0, 0), (WINDOW, 0), (0, 0)))
    qb = qg.reshape(b, g, hg, nb, Q_BLOCK, d).transpose(3, 0, 1, 2, 4, 5)

    def one_block(args):
        i, q_i = args
        start = i * Q_BLOCK
        k_i = lax.dynamic_slice_in_dim(kp, start, span, axis=2)
        v_i = lax.dynamic_slice_in_dim(vp, start, span, axis=2)
        q_pos = start + jnp.arange(Q_BLOCK)
        k_pos = start - WINDOW + jnp.arange(span)
        mask = ((k_pos[None, :] <= q_pos[:, None]) & (k_pos[None, :] > q_pos[:, None] - WINDOW)
                & (k_pos[None, :] >= 0))
        sc = jnp.einsum('bghqd,bgkd->bghqk', q_i, k_i) * ATTN_SCALE
        p = _masked_softmax(sc, mask)
        return jnp.einsum('bghqk,bgkd->bghqd', p, v_i)

    out = lax.map(one_block, (jnp.arange(nb), qb))
    return out.transpose(1, 2, 3, 0, 4, 5).reshape(b, g, hg, s, d)


def _native_sparse_attention(nq, kc, vc, ks, vs, kw, vw, gate_logits,
                             pos_k, w1_k, w2_k, pos_v, w1_v, w2_v):
    b, s, _ = nq.shape
    g, hg = NSA_KV_GROUPS, NSA_HEADS_PER_GROUP
    f32 = jnp.float32
    q = _rope(_heads(nq, NSA_HEADS)).astype(f32)
    qg = q.reshape(b, g, hg, s, HEAD_DIM)
    k_cmp = _compress(_rope(_heads(kc, g)).astype(f32), pos_k, w1_k, w2_k)
    v_cmp = _compress(_heads(vc, g).astype(f32), pos_v, w1_v, w2_v)
    n_cmp = k_cmp.shape[2]
    t_pos = jnp.arange(s)
    cmp_end = jnp.arange(n_cmp) * CMP_STRIDE + CMP_BLOCK - 1
    cmp_mask = cmp_end[None, :] <= t_pos[:, None]
    sc = jnp.einsum('bghtd,bgnd->bghtn', qg, k_cmp) * ATTN_SCALE
    p_cmp = _masked_softmax(sc, cmp_mask)
    o_cmp = jnp.einsum('bghtn,bgnd->bghtd', p_cmp, v_cmp)
    n_sel = s // SEL_BLOCK
    ci = jnp.arange(n_cmp)[:, None] * CMP_STRIDE
    sj = jnp.arange(n_sel)[None, :] * SEL_BLOCK
    overlap = ((ci < sj + SEL_BLOCK) & (ci + CMP_BLOCK > sj)).astype(f32)
    p_slc = jnp.einsum('bgtn,ns->bgts', jnp.sum(p_cmp, axis=2), overlap)
    cur = t_pos // SEL_BLOCK
    jj = jnp.arange(n_sel)
    forced = (jj[None, :] == 0) | (jj[None, :] == cur[:, None]) | (jj[None, :] == cur[:, None] - 1)
    valid = jj[None, :] <= cur[:, None]
    score = jnp.where(valid, p_slc + FORCED_BONUS * forced.astype(f32), NEG_INF)
    n_top = min(SEL_TOPN, n_sel)
    _, idx = lax.top_k(score, n_top)
    k_sel = _rope(_heads(ks, g)).astype(f32)
    v_sel = _heads(vs, g).astype(f32)
    o_sel = _selected_attention(qg, k_sel, v_sel, idx)
    k_win = _rope(_heads(kw, g)).astype(f32)
    v_win = _heads(vw, g).astype(f32)
    o_win = _window_attention(qg, k_win, v_win)
    gates = jax.nn.sigmoid(gate_logits.astype(f32)).reshape(b, s, NSA_HEADS, 3).transpose(0, 2, 1, 3)
    gates = gates.reshape(b, g, hg, s, 3)
    o = gates[..., 0:1] * o_cmp + gates[..., 1:2] * o_sel + gates[..., 2:3] * o_win
    o = o.reshape(b, NSA_HEADS, s, HEAD_DIM).transpose(0, 2, 1, 3).reshape(b, s, NSA_WIDTH)
    return o.astype(nq.dtype)


def setup_inputs(seed: int = 0) -> dict:
    key = jax.random.key(seed)
    ks = jax.random.split(key, 20)
    f32 = jnp.float32

    def nrm(k, shape, scale):
        return jax.random.normal(k, shape, f32) * scale

    return {
        'x': nrm(ks[0], (BATCH, SEQ, D_MODEL), 1.0),
        'norm_mix_pre': 1.0 + nrm(ks[1], (DEPTH, D_MODEL), 0.05),
        'norm_mix_post': 1.0 + nrm(ks[2], (DEPTH, D_MODEL), 0.05),
        'norm_mlp_pre': 1.0 + nrm(ks[3], (DEPTH, D_MODEL), 0.05),
        'norm_mlp_post': 1.0 + nrm(ks[4], (DEPTH, D_MODEL), 0.05),
        'w_in': nrm(ks[5], (DEPTH, D_MODEL, IN_COLS), D_MODEL ** -0.5),
        'b_forget': FORGET_BIAS_INIT + nrm(ks[6], (DEPTH, FOX_HEADS), 0.1),
        'cmp_pos_k': nrm(ks[7], (DEPTH, CMP_BLOCK, HEAD_DIM), 0.1),
        'cmp_w1_k': nrm(ks[8], (DEPTH, CMP_BLOCK * HEAD_DIM, HEAD_DIM), (CMP_BLOCK * HEAD_DIM) ** -0.5),
        'cmp_w2_k': nrm(ks[9], (DEPTH, HEAD_DIM, HEAD_DIM), HEAD_DIM ** -0.5),
        'cmp_pos_v': nrm(ks[10], (DEPTH, CMP_BLOCK, HEAD_DIM), 0.1),
        'cmp_w1_v': nrm(ks[11], (DEPTH, CMP_BLOCK * HEAD_DIM, HEAD_DIM), (CMP_BLOCK * HEAD_DIM) ** -0.5),
        'cmp_w2_v': nrm(ks[12], (DEPTH, HEAD_DIM, HEAD_DIM), HEAD_DIM ** -0.5),
        'w_fox_out': nrm(ks[13], (DEPTH, FOX_WIDTH, D_MODEL), FOX_WIDTH ** -0.5),
        'w_nsa_out': nrm(ks[14], (DEPTH, NSA_WIDTH, D_MODEL), NSA_WIDTH ** -0.5),
        'w_o': nrm(ks[15], (DEPTH, D_MODEL, D_MODEL), D_MODEL ** -0.5),
        'w_up': nrm(ks[16], (DEPTH, D_MODEL, D_FF), D_MODEL ** -0.5),
        'w_down': nrm(ks[17], (DEPTH, D_FF, D_MODEL), D_FF ** -0.5),
    }


def reference(x, norm_mix_pre, norm_mix_post, norm_mlp_pre, norm_mlp_post, w_in, b_forget,
              cmp_pos_k, cmp_w1_k, cmp_w2_k, cmp_pos_v, cmp_w1_v, cmp_w2_v,
              w_fox_out, w_nsa_out, w_o, w_up, w_down):
    split_points = [int(p) for p in np.cumsum(IN_SPLITS)[:-1]]
    for layer in range(DEPTH):
        h = _rms_norm(x, norm_mix_pre[layer])
        proj = h @ w_in[layer]
        (fq, fk, fv, ff, nq, kc, vc, ksl, vsl, kwn, vwn, ng, ga, gb) = jnp.split(proj, split_points, axis=-1)
        fox = _forgetting_attention(_heads(fq, FOX_HEADS), _heads(fk, FOX_HEADS), _heads(fv, FOX_HEADS),
                                    ff + b_forget[layer])
        nsa = _native_sparse_attention(nq, kc, vc, ksl, vsl, kwn, vwn, ng,
                                       cmp_pos_k[layer], cmp_w1_k[layer], cmp_w2_k[layer],
                                       cmp_pos_v[layer], cmp_w1_v[layer], cmp_w2_v[layer])
        mix = jax.nn.sigmoid(ga) * (fox @ w_fox_out[layer]) + jax.nn.sigmoid(gb) * (nsa @ w_nsa_out[layer])
        x = x + _rms_norm(mix @ w_o[layer], norm_mix_post[layer])
        h = _rms_norm(x, norm_mlp_pre[layer])
        u = jnp.square(jax.nn.relu(h @ w_up[layer]))
        x = x + _rms_norm(u @ w_down[layer], norm_mlp_post[layer])
    return x
```

```python
import numpy as np
from contextlib import ExitStack
import concourse.bass as bass
import concourse.mybir as mybir
from concourse.bass_utils import run_bass_kernel_spmd

F32 = mybir.dt.float32
BF16 = mybir.dt.bfloat16
AF = mybir.ActivationFunctionType
ALU = mybir.AluOpType

T = 2048
D = 1024
NT = 16
NEGM = -30000.0
SEQ_PER_CORE = 4
NCORES = 8


class Buf:
    __slots__ = ("name", "last_w", "readers", "dma_readers")

    def __init__(self, name):
        self.name = name
        self.last_w = None
        self.readers = {}
        self.dma_readers = []


class Op:
    __slots__ = ("eng", "meth", "kw", "deps", "idx", "dma", "tok", "marked", "semval")


COMPUTE = ("pe", "act", "dve", "pool")
QUEUES = ("sp", "pool")
NDSEM = 12


class Prog:
    def __init__(self):
        self.streams = {e: [] for e in ("pe", "act", "dve", "pool", "sp")}
        self.ndma = {q: 0 for q in QUEUES}
        self.dma_ops = {q: [] for q in QUEUES}
        self.barrier_deps = {e: [] for e in self.streams}
        self.dmas_since_barrier = []
        self.final_deps = []

    def add(self, eng, meth, kw, reads=(), writes=(), dma=False, prefetch=False, final=False):
        op = Op()
        op.eng, op.meth, op.kw, op.dma = eng, meth, kw, dma
        op.marked = False
        op.semval = None
        stream = self.streams[eng]
        op.idx = len(stream)
        deps = set()
        for b in reads:
            if b.last_w is not None:
                deps.add(b.last_w)
        for b in writes:
            if b.last_w is not None:
                deps.add(b.last_w)
            for e, i in b.readers.items():
                deps.add(("c", e, i))
            for t in b.dma_readers:
                deps.add(t)
        for t in self.barrier_deps[eng]:
            deps.add(t)
        self.barrier_deps[eng] = []
        if dma:
            j = self.ndma[eng]
            self.ndma[eng] += 1
            op.tok = ("d", eng, j)
            if j >= NDSEM:
                deps.add(("d", eng, j - NDSEM))
            self.dma_ops[eng].append(op)
            if not prefetch:
                self.dmas_since_barrier.append(op.tok)
            if final:
                self.final_deps.append(op.tok)
        else:
            op.tok = ("c", eng, op.idx)
        fdeps = []
        for d in deps:
            if d[0] == "c" and d[1] == eng and not dma:
                if eng == "pe":
                    continue
                fdeps.append(d)
            else:
                fdeps.append(d)
        op.deps = fdeps
        for b in reads:
            if dma:
                b.dma_readers.append(op.tok)
            else:
                b.readers[eng] = op.idx
        for b in writes:
            b.last_w = op.tok
            b.readers = {}
            b.dma_readers = []
        stream.append(op)
        return op

    def barrier(self):
        toks = []
        for e in COMPUTE:
            if self.streams[e]:
                last = self.streams[e][-1]
                if not last.dma:
                    toks.append(last.tok)
                else:
                    for o in reversed(self.streams[e]):
                        if not o.dma:
                            toks.append(o.tok)
                            break
        toks += self.dmas_since_barrier
        self.dmas_since_barrier = []
        for e in self.streams:
            self.barrier_deps[e] = list(self.barrier_deps[e]) + toks

    def finalize(self):
        op = self.add("sp", "nop_final", {}, (), ())
        op.deps = list(self.final_deps)
        for e, stream in self.streams.items():
            for op in stream:
                for d in op.deps:
                    if d[0] == "c":
                        self.streams[d[1]][d[2]].marked = True
        for e in COMPUTE:
            n = 0
            for op in self.streams[e]:
                if op.dma:
                    continue
                if op.marked:
                    n += 1
                    op.semval = n

    def emit(self, nc, es):
        esem = {e: es.enter_context(nc.semaphore("s_" + e)) for e in COMPUTE}
        dsem = {q: [es.enter_context(nc.semaphore("d_%s%d" % (q, i))) for i in range(NDSEM)]
                for q in QUEUES}
        block = es.enter_context(nc.Block())
        streams = self.streams

        def tokwait(d):
            if d[0] == "c":
                return ("c" + d[1], esem[d[1]], streams[d[1]][d[2]].semval)
            q, j = d[1], d[2]
            return ("d%s%d" % (q, j % NDSEM), dsem[q][j % NDSEM], 16 * (j // NDSEM + 1))

        def run(eng, name):
            waited = {}
            for op in streams[name]:
                for d in op.deps:
                    key, sem, val = tokwait(d)
                    if waited.get(key, 0) >= val:
                        continue
                    waited[key] = val
                    eng.wait_ge(sem, val)
                if op.meth == "nop_final":
                    continue
                ins = getattr(eng, op.meth)(**op.kw)
                if op.dma:
                    j = op.tok[2]
                    ins.then_inc(dsem[name][j % NDSEM], 16)
                elif op.marked:
                    ins.then_inc(esem[name], 1)

        @block.sync
        def _(e):
            run(e, "sp")

        @block.gpsimd
        def _(e):
            run(e, "pool")

        @block.scalar
        def _(e):
            run(e, "act")

        @block.vector
        def _(e):
            run(e, "dve")

        @block.tensor
        def _(e):
            run(e, "pe")


class Ring:
    def __init__(self, items):
        self.items = items
        self.i = 0

    def next(self):
        it = self.items[self.i % len(self.items)]
        self.i += 1
        return it


def build_program(nseq=SEQ_PER_CORE, dbg=False):
    nc = bass.Bass("TRN2", target_bir_lowering=False)
    P = Prog()
    es = ExitStack()

    def din(name, shape, dt=F32):
        return nc.dram_tensor(name, list(shape), dt, kind="ExternalInput").ap()

    x_d = din("x", [nseq * T, D])
    out_d = nc.dram_tensor("out", [nseq * T, D], F32, kind="ExternalOutput").ap()
    w_fqk = din("w_fqk", [8, 128, 1024])
    w_fv = din("w_fv", [2, 128, 8 * 256])
    w_ffng = din("w_ffng", [128, 8 * 32])
    w_nq = din("w_nq", [8, 128, 1024])
    w_nk = din("w_nk", [7, 128, 1024])
    w_nv = din("w_nv", [128, 8 * 256])
    w_gab = din("w_gab", [16, 128, 1024])
    w_fo = din("w_fo", [8, 128, 512])
    w_no = din("w_no", [8, 128, 512])
    w_o = din("w_o", [2, 128, 8 * 512])
    w_up = din("w_up", [16, 128, 8 * 256])
    w_dn = din("w_dn", [16, 128, 2 * 1024])
    w1k_d = din("w1k", [128, 2048])
    w1v_d = din("w1v", [128, 2048])
    w2k_d = din("w2k", [64, 64])
    w2v_d = din("w2v", [64, 64])
    posk_d = din("posk", [64, 32])
    posv_d = din("posv", [64, 32])
    rope_d = din("rope", [4, 128, 1024])
    cmpmask_d = din("cmpmask", [128, 2048])
    fbt_d = din("fbt", [128, 16 * 32])
    tri_d = din("tri", [128, 256])
    ident_d = din("ident", [128, 128])
    u_d = din("umat", [128, 128])
    ov_d = din("ov", [128, 32])
    erows_d = din("erows", [32, 2048])
    gpre1_d = din("gpre1", [128, 8])
    gpre2_d = din("gpre2", [128, 8])
    gpost1_d = din("gpost1", [1, 1024])
    gpost2_d = din("gpost2", [1, 1024])
    bfor_d = din("bfor", [1, 8])
    dbg_d = {}
    if dbg:
        for nm, shp in (("d_fox", [128, 16 * 512]), ("d_nsa", [128, 16 * 512]),
                        ("d_negc", [128, 128])):
            dbg_d[nm] = nc.dram_tensor(nm, shp, F32, kind="ExternalOutput").ap()

    def sb(name, shape, dt):
        return es.enter_context(nc.sbuf_tensor(name, list(shape), dt))

    def ps(name, shape, dt):
        return es.enter_context(nc.psum_tensor(name, list(shape), dt))

    hT = sb("hT", [128, 8, T], BF16)
    hT_b = [[Buf("hT%d_%d" % (k, c)) for c in range(4)] for k in range(8)]
    ARENA_ELEMS = 50176
    arena = sb("arena", [128, ARENA_ELEMS], BF16)
    ident_f = sb("ident_f", [128, 128], F32)
    ident_b = sb("ident_b", [128, 128], BF16)
    u_f = sb("u_f", [128, 128], F32)
    ones_f = sb("ones_f", [128, 128], F32)
    zeros_b = sb("zeros_b", [128, 512], BF16)
    tri_b = sb("tri_b", [128, 256], BF16)
    cmpmask_b = sb("cmpmask_b", [128, T], BF16)
    fbt = sb("fbt_s", [128, 16, 32], F32)
    gpre1 = sb("gpre1_s", [128, 8], F32)
    gpre2 = sb("gpre2_s", [128, 8], F32)
    bfor = sb("bfor_s", [128, 8], F32)
    onescol = sb("onescol", [128, 1], F32)
    w1k = sb("w1k_s", [128, 32, 64], BF16)
    w1v = sb("w1v_s", [128, 32, 64], BF16)
    w2k = sb("w2k_s", [64, 64], BF16)
    w2v = sb("w2v_s", [64, 64], BF16)
    posk = sb("posk_s", [64, 32], BF16)
    posv = sb("posv_s", [64, 32], BF16)
    cbk = sb("cbk", [64, 1], F32)
    cbv = sb("cbv", [64, 1], F32)
    vcmp = sb("vcmp", [128, 2, 97], BF16)
    kcmp = sb("kcmp", [96, 2, 128], BF16)
    sTk = sb("sTk", [64, 2, 128], BF16)
    sTv = sb("sTv", [64, 2, 128], BF16)
    mbw = sb("mbw", [128, 2, 4, 96], BF16)
    mbT = sb("mbT", [96, 2, 512], BF16)
    const_b = Buf("consts")

    lp = sb("lp", [128, 16, 8], F32)
    lsum = sb("lsum", [128, 17, 8], F32)
    negc = sb("negc", [128, 16, 8], F32)
    caugT = sb("caugT", [33, T], BF16)
    gates = sb("gates", [128, 16, 24], F32)
    psl = sb("psl", [128, 4, 32], F32)

    xt_ring = Ring([(sb("xt%d" % i, [128, 1024], F32), Buf("xt%d" % i)) for i in range(2)])
    t1_ring = Ring([(sb("t1_%d" % i, [128, 512], F32), Buf("t1_%d" % i)) for i in range(3)])
    hn_ring = Ring([(sb("hn%d" % i, [128, 1024], BF16), Buf("hn%d" % i)) for i in range(2)])
    junk_b = Buf("junk")
    pt_ring = Ring([(sb("pt%d" % i, [128, 512], BF16), Buf("pt%d" % i)) for i in range(4)])
    wt_ring = Ring([(sb("wt%d" % i, [128, 2048], BF16), Buf("wt%d" % i)) for i in range(4)])
    rope_ring = Ring([(sb("rp%d" % i, [128, 1024], F32), Buf("rp%d" % i)) for i in range(1)])
    sm_ring = Ring([(sb("sm%d" % i, [128, 64], F32), Buf("sm%d" % i)) for i in range(8)])

    pj_ring = Ring([(ps("pj%d" % i, [128, 512], F32), Buf("pj%d" % i)) for i in range(2)])
    st_ring = Ring([(ps("st%d" % i, [128, 512], F32), Buf("st%d" % i)) for i in range(2)])
    sa_ring = Ring([st_ring.items[0], pj_ring.items[0], st_ring.items[1], pj_ring.items[1]])
    oa_ring = Ring([(ps("oa%d" % i, [128, 512], F32), Buf("oa%d" % i)) for i in range(2)])
    tp_ring = Ring([(ps("tp%d" % i, [128, 512], F32), Buf("tp%d" % i)) for i in range(2)])
    y_ring = Ring([st_ring.items[0], oa_ring.items[0], st_ring.items[1], oa_ring.items[1]])

    def mm(out, lhsT, rhs, start, stop, reads, writes):
        P.add("pe", "matmul", dict(out=out, lhsT=lhsT, rhs=rhs, start=start, stop=stop,
                                   skip_group_check=True), reads, writes)

    def tr(out, in_, ident, reads, writes):
        P.add("pe", "transpose", dict(out=out, in_=in_, identity=ident), reads, writes)

    def act(out, in_, func, reads, writes, **kw):
        P.add("act", "activation", dict(out=out, in_=in_, func=func, **kw), reads, writes)

    def dma(q, out, in_, reads, writes, **kw):
        extra = {}
        for k in ("prefetch", "final"):
            if k in kw:
                extra[k] = kw.pop(k)
        return P.add(q, "dma_start", dict(out=out, in_=in_, **kw), reads, writes, dma=True, **extra)

    def tt(eng, out, in0, in1, op, reads, writes):
        P.add(eng, "tensor_tensor", dict(out=out, in0=in0, in1=in1, op=op), reads, writes)

    def ts(eng, out, in0, s1, s2, op0, op1, reads, writes):
        kw = dict(out=out, in0=in0, scalar1=s1, scalar2=s2, op0=op0)
        if op1 is not None:
            kw["op1"] = op1
        P.add(eng, "tensor_scalar", kw, reads, writes)

    def stt(out, in0, scalar, in1, op0, op1, reads, writes):
        P.add("dve", "scalar_tensor_tensor", dict(out=out, in0=in0, scalar=scalar, in1=in1,
                                                  op0=op0, op1=op1), reads, writes)

    def cp(eng, out, in_, reads, writes):
        if eng == "act":
            P.add("act", "activation", dict(out=out, in_=in_, func=AF.Copy), reads, writes)
        else:
            P.add(eng, "tensor_copy", dict(out=out, in_=in_), reads, writes)

    def memset(eng, ap, val, writes):
        P.add(eng, "memset", dict(ap=ap, constant=val), (), writes)

    dma("sp", ident_f[:], ident_d[:, :], (), [const_b])
    dma("pool", ident_b[:], ident_d[:, :], (), [const_b])
    dma("sp", u_f[:], u_d[:, :], (), [const_b])
    dma("pool", tri_b[:], tri_d[:, :], (), [const_b])
    dma("pool", cmpmask_b[:], cmpmask_d[:, :], (), [const_b])
    dma("sp", fbt[:].rearrange("p a b -> p (a b)"), fbt_d[:, :], (), [const_b])
    dma("sp", gpre1[:], gpre1_d[:, :], (), [const_b])
    dma("sp", gpre2[:], gpre2_d[:, :], (), [const_b])
    dma("sp", bfor[:], bfor_d.partition_broadcast(128), (), [const_b])
    dma("pool", w1k[:].rearrange("p a b -> p (a b)"), w1k_d[:, :], (), [const_b])
    dma("pool", w1v[:].rearrange("p a b -> p (a b)"), w1v_d[:, :], (), [const_b])
    dma("pool", w2k[:], w2k_d[:, :], (), [const_b])
    dma("pool", w2v[:], w2v_d[:, :], (), [const_b])
    dma("pool", posk[:], posk_d[:, :], (), [const_b])
    dma("pool", posv[:], posv_d[:, :], (), [const_b])
    memset("pool", ones_f[:], 1.0, [const_b])
    memset("pool", zeros_b[:], 0.0, [const_b])
    memset("pool", onescol[:], 1.0, [const_b])
    memset("pool", caugT[32:33, :], 1.0, [const_b])
    memset("pool", vcmp[:].rearrange("p a b -> p (a b)"), 1.0, [const_b])
    for g in range(2):
        dma("pool", vcmp[:, g, 65:97], ov_d[:, :], (), [const_b])
    memset("pool", sTk[:].rearrange("p a b -> p (a b)"), 0.0, [const_b])
    memset("pool", sTv[:].rearrange("p a b -> p (a b)"), 0.0, [const_b])
    memset("pool", mbw[:].rearrange("p g a b -> p (g a b)"), 0.0, [const_b])
    memset("pool", lsum[:, 0, :], 0.0, [const_b])
    memset("pool", kcmp[64:96, :, :].rearrange("p a b -> p (a b)"), 0.0, [const_b])
    for (w1, pos, cb) in ((w1k, posk, cbk), (w1v, posv, cbv)):
        bank, bb = tp_ring.next()
        for l in range(32):
            mm(bank[0:64, 0:1], w1[0:64, l, :], pos[0:64, l:l + 1], l == 0, l == 31, [const_b], [bb])
        cp("dve", cb[:], bank[0:64, 0:1], [bb], [const_b])

    def aview(off, n, dt=BF16):
        v = arena[:, off:off + n]
        if dt == F32:
            v = v.bitcast(F32)
        return v

    junk = aview(49152, 1024)
    yt_ring = Ring([(aview(40960 + i * 2048, 2048, F32), Buf("yt%d" % i)) for i in range(2)])
    gpost1 = aview(45056, 2048, F32)
    gpost2 = aview(47104, 2048, F32)
    gpost_b = Buf("gpost")

    def norm_to_hT(src, src_b, tt_i, gpre):
        sm, smb = sm_ring.next()
        act(junk[:], src, AF.Square, [src_b], [junk_b, smb], accum_out=sm[:, 0:1])
        ts("dve", sm[:, 1:2], sm[:, 0:1], 1.0 / D, 1e-6, ALU.mult, ALU.add, [smb], [smb])
        act(sm[:, 2:3], sm[:, 1:2], AF.Ln, [smb], [smb])
        act(sm[:, 3:4], sm[:, 2:3], AF.Exp, [smb], [smb], scale=-0.5)
        hn, hnb = hn_ring.next()
        ts("dve", hn[:], src, sm[:, 3:4], None, ALU.mult, None, [src_b, smb], [hnb])
        bank, bb = tp_ring.next()
        bk = bank[:].bitcast(BF16)
        for kc in range(8):
            tr(bk[:, kc * 128:(kc + 1) * 128], hn[:, kc * 128:(kc + 1) * 128], ident_b[:], [hnb, const_b], [bb])
        c = tt_i // 4
        for kc in range(8):
            act(hT[:, kc, tt_i * 128:(tt_i + 1) * 128], bk[:, kc * 128:(kc + 1) * 128], AF.Copy,
                [bb, const_b], [hT_b[kc][c]], scale=gpre[:, kc:kc + 1])

    def load_w(q, src2d, ncols_elems, prefetch=True):
        wt, wb = wt_ring.next()
        dma(q, wt[:, 0:ncols_elems], src2d, (), [wb], prefetch=prefetch)
        return wt, wb

    pipe = []
    HEAT = 0

    def noop_():
        return None

    def attn_flush():
        n = len(pipe)
        if n == 0:
            return
        LA = 2
        for j in range(min(LA, n)):
            pipe[j]["S"]()
        for i, t in enumerate(pipe):
            t["E"]()
            if i + LA < n:
                pipe[i + LA]["S"]()
            if t["pre"] is not None:
                t["pre"]()
            t["PV"]()
            if HEAT and t["S"] is not noop_:
                hb, hbb = tp_ring.items[1]
                mm(hb[:, 0:HEAT], zeros_b[:, 0:128], zeros_b[:, 0:HEAT], True, True, [const_b], [hbb])
            if t["post"] is not None:
                t["post"]()
        del pipe[:]

    def attn_chunk(qc, qtile, q_b, ktile, k_b, rows, tiles, vget, vw, v_b, bias_get, finalize):
        obank, ob = oa_ring.next()
        o3 = obank[:, 0:4 * vw].rearrange("p (a b) -> p a b", b=vw)
        last = {}
        for i, (kt, c0, c1, mask) in enumerate(tiles):
            for qs in range(c0 // 128, c1 // 128):
                last[qs] = i
        ntl = len(tiles)
        for i, (kt, c0, c1, mask) in enumerate(tiles):
            sbank, sbb = sa_ring.next()
            pt, ptb = pt_ring.next()

            def fS(kt=kt, c0=c0, c1=c1, mask=mask, sbank=sbank, sbb=sbb):
                mm(sbank[:, c0:c1], ktile[rows, kt * 128:(kt + 1) * 128],
                   qtile[rows, qc * 512 + c0:qc * 512 + c1], True, mask is None, [q_b, k_b], [sbb])
                if mask is not None:
                    map_, m0, mw = mask
                    mm(sbank[:, m0:m0 + mw], ident_b[:], map_, False, True, [const_b], [sbb])

            def fE(kt=kt, c0=c0, c1=c1, sbank=sbank, sbb=sbb, pt=pt, ptb=ptb):
                kw = dict(scale=0.125)
                rd = [sbb]
                if bias_get is not None:
                    bap, bbuf = bias_get(kt)
                    kw["bias"] = bap
                    rd.append(bbuf)
                act(pt[:, c0:c1], sbank[:, c0:c1], AF.Exp, rd, [ptb], **kw)

            def fPV(i=i, kt=kt, c0=c0, c1=c1, pt=pt, ptb=ptb):
                for qs in range(c0 // 128, c1 // 128):
                    mm(o3[:, qs, :], pt[:, qs * 128:(qs + 1) * 128], vget(kt), False, last[qs] == i,
                       [ptb, v_b], [ob])

            pre = None
            post = None
            if i == 0:
                def pre():
                    mm(obank[:, :], zeros_b[:, 0:128], zeros_b[:, 0:512], True, False, [const_b], [ob])
            if i == ntl - 1:
                def post():
                    finalize(qc, o3, ob)
            pipe.append(dict(S=fS, E=fE, PV=fPV, pre=pre, post=post))

    def causal_tiles(qc):
        tl = [(kt, 0, 512, None) for kt in range(4 * qc)]
        for j in range(4):
            tl.append((4 * qc + j, 128 * j, 512, (tri_b[:, 0:128], 128 * j, 128)))
        return tl

    def window_tiles(qc):
        tl = []
        for j in range(-4, 4):
            kt = 4 * qc + j
            if kt < 0:
                continue
            if j >= 0:
                tl.append((kt, 128 * j, 512, (tri_b[:, 0:128], 128 * j, 128)))
            else:
                jj = j + 4
                tl.append((kt, 0, 128 * jj + 128, (tri_b[:, 128:256], 128 * jj, 128)))
        return tl

    for s in range(nseq):
        xs = x_d[s * T:(s + 1) * T, :]
        outs = out_d[s * T:(s + 1) * T, :]
        outD_b = [Buf("outD%d_%d" % (s, i)) for i in range(NT)]


        VF = aview(0, 8320).rearrange("p (a b c) -> p a b c", a=16, b=8)
        vf_b = Buf("VF")
        QK = [[aview(8320 + (st * 4 + i) * 2048, 2048) for i in range(4)] for st in range(2)]
        QK_b = [[Buf("QK%d_%d" % (st, i)) for i in range(4)] for st in range(2)]
        fox_tm = aview(8320 + 16384, 8192).rearrange("p (a b) -> p a b", b=512)
        fox_b = [Buf("fox%d" % i) for i in range(NT)]
        FOXT_OFF = 32896
        foxT = aview(FOXT_OFF, 8192).rearrange("p (a b) -> p a b", b=T)
        foxT_b = Buf("foxT")
        nsaT = aview(FOXT_OFF + 8192, 8192).rearrange("p (a b) -> p a b", b=T)
        nsaT_b = Buf("nsaT")

        wfvh = [load_w("pool", w_fv[hf, :, :], 2048) for hf in range(2)]
        memset("pool", VF[:, :, :, 64:65], 1.0, [vf_b])
        wg, wg_b = load_w("pool", w_ffng[:, :], 256)
        wg3 = wg[:, 0:256].rearrange("p (a b) -> p a b", b=32)
        wfv3 = [w[:, 0:2048].rearrange("p (a b) -> p a b", b=256) for (w, _) in wfvh]
        for ti in range(NT):
            c = ti // 4
            xt, xb = xt_ring.next()
            dma("sp", xt[:], xs[ti * 128:(ti + 1) * 128, :], (), [xb])
            norm_to_hT(xt[:], xb, ti, gpre1)
            for hf in range(2):
                bank, bb = pj_ring.next()
                for kc in range(8):
                    mm(bank[:, 0:256], hT[:, kc, ti * 128:(ti + 1) * 128], wfv3[hf][:, kc, :], kc == 0, kc == 7,
                       [hT_b[kc][c], wfvh[hf][1]], [bb])
                cp("dve", VF[:, ti, 4 * hf:4 * hf + 4, 0:64], bank[:, 0:256].rearrange("p (a b) -> p a b", b=64),
                   [bb], [vf_b])
            bank2, bb2 = tp_ring.next()
            for kc in range(8):
                mm(bank2[:, 0:32], hT[:, kc, ti * 128:(ti + 1) * 128], wg3[:, kc, :], kc == 0, kc == 7,
                   [hT_b[kc][c], wg_b], [bb2])
            sm, smb = sm_ring.next()
            tt("dve", sm[:, 0:8], bank2[:, 0:8], bfor[:], ALU.add, [bb2, const_b], [smb])
            act(sm[:, 8:16], sm[:, 0:8], AF.Exp, [smb], [smb], scale=-1.0)
            act(lp[:, ti, :], sm[:, 8:16], AF.Ln, [smb, const_b], [const_b], bias=onescol[:])
            tt("dve", lsum[:, ti + 1, :], lsum[:, ti, :], lp[:, ti, :], ALU.add, [const_b], [const_b])
            act(sm[:, 16:40], bank2[:, 8:32], AF.Exp, [bb2], [smb], scale=-1.0)
            ts("dve", sm[:, 16:40], sm[:, 16:40], 1.0, None, ALU.add, None, [smb], [smb])
            P.add("dve", "reciprocal", dict(out=gates[:, ti, :], in_=sm[:, 16:40]), [smb], [const_b])
        bank, bb = tp_ring.next()
        for ti in range(NT):
            mm(bank[:, ti * 8:(ti + 1) * 8], u_f[:], lp[:, ti, :], True, False, [const_b], [bb])
            mm(bank[:, ti * 8:(ti + 1) * 8], ones_f[:], lsum[:, ti, :], False, True, [const_b], [bb])
        cp("dve", negc[:].rearrange("p a b -> p (a b)"), bank[:, 0:128], [bb], [const_b])
        for c in range(4):
            bank, bb = tp_ring.next()
            for j in range(4):
                ti = 4 * c + j
                mm(bank[0:8, j * 128:(j + 1) * 128], lp[:, ti, :], u_f[:], True, False, [const_b], [bb])
                mm(bank[0:8, j * 128:(j + 1) * 128], lsum[:, ti, :], ones_f[:], False, True, [const_b], [bb])
            act(caugT[0:8, c * 512:(c + 1) * 512], bank[0:8, :], AF.Copy, [bb], [const_b], scale=-8.0)

        for p in range(4):
            st = p % 2
            QA, KA, QB, KB = QK[st]
            QA_b, KA_b, QB_b, KB_b = QK_b[st]
            dma("sp", QA[64:65, :], caugT[2 * p:2 * p + 1, :], [const_b], [QA_b])
            dma("sp", QB[64:65, :], caugT[2 * p + 1:2 * p + 2, :], [const_b], [QB_b])
            dma("sp", KA[64:65, :], caugT[32:33, :], [const_b], [KA_b])
            dma("sp", KB[64:65, :], caugT[32:33, :], [const_b], [KB_b])
            for (blk, TA, TA_b, TB, TB_b) in ((p, QA, QA_b, QB, QB_b), (4 + p, KA, KA_b, KB, KB_b)):
                wt, wb = load_w("pool", w_fqk[blk, :, :], 1024)
                w3 = wt[:, 0:1024].rearrange("p (a b) -> p a b", b=128)
                for c in range(4):
                    bank, bb = pj_ring.next()
                    for kc in range(8):
                        mm(bank[:, :], w3[:, kc, :], hT[:, kc, c * 512:(c + 1) * 512], kc == 0, kc == 7,
                           [wb, hT_b[kc][c]], [bb])
                    cp("dve", TA[0:64, c * 512:(c + 1) * 512], bank[0:64, :], [bb], [TA_b])
                    cp("act", TB[0:64, c * 512:(c + 1) * 512], bank[64:128, :], [bb], [TB_b])
            for hh in range(2):
                h = 2 * p + hh
                qtile, q_b, ktile, k_b = (QA, QA_b, KA, KA_b) if hh == 0 else (QB, QB_b, KB, KB_b)
                rows = slice(0, 65)

                def fin(qc, o3, ob, h=h):
                    sm, smb = sm_ring.next()
                    P.add("dve", "reciprocal", dict(out=sm[:, 0:4], in_=o3[:, :, 64]), [ob], [smb])
                    tt("dve", fox_tm[:, qc * 4:qc * 4 + 4, h * 64:(h + 1) * 64], o3[:, :, 0:64],
                       sm[:, 0:4].unsqueeze(2).broadcast_to([128, 4, 64]), ALU.mult, [ob, smb],
                       fox_b[qc * 4:qc * 4 + 4])

                for qc in range(4):
                    attn_chunk(qc, qtile, q_b, ktile, k_b, rows, causal_tiles(qc),
                               lambda kt, h=h: VF[:, kt, h, :], 65, vf_b,
                               lambda kt, h=h: (negc[:, kt, h:h + 1], const_b), fin)
            attn_flush()
        for ti in range(NT):
            bank, bb = tp_ring.next()
            bk = bank[:].bitcast(BF16)
            for kc in range(4):
                tr(bk[:, kc * 128:(kc + 1) * 128], fox_tm[:, ti, kc * 128:(kc + 1) * 128], ident_b[:],
                   [fox_b[ti], const_b], [bb])
            cp("dve", foxT[:, :, ti * 128:(ti + 1) * 128],
               bk[:, 0:512].rearrange("p (a b) -> p a b", b=128), [bb], [foxT_b])
        if dbg and s == 0:
            for ti in range(NT):
                yt, yb = xt_ring.next()
                cp("dve", yt[:, 0:512], fox_tm[:, ti, :], [fox_b[ti]], [yb])
                dma("sp", dbg_d["d_fox"][:, ti * 512:(ti + 1) * 512], yt[:, 0:512], [yb], [], final=True)
            dma("sp", dbg_d["d_negc"][:, :], negc[:].rearrange("p a b -> p (a b)"), [const_b], [], final=True)
        P.barrier()

        QS = [[aview((g * 4 + p) * 2048, 2048) for p in range(4)] for g in range(2)]
        QS_b = [[[Buf("QS%d_%d_%d" % (g, p, c)) for c in range(4)] for p in range(4)] for g in range(2)]
        KS = [aview(16384 + g * 2048, 2048) for g in range(2)]
        KS_b = [Buf("KS%d" % g) for g in range(2)]
        KW = [aview(16384 + 4096 + g * 2048, 2048) for g in range(2)]
        KW_b = [Buf("KW%d" % g) for g in range(2)]
        KC = aview(16384 + 8192, 2048)
        KC_b = Buf("KC")
        VC = aview(16384 + 10240, 2048)
        VC_b = Buf("VC")
        VSW = aview(28672, 4160).rearrange("p (a b c) -> p a b c", a=16, b=4)
        vsw_b = Buf("VSW")
        nsa_acc = aview(16384 + 8192, 4096, F32).rearrange("p (a b) -> p a b", b=512)
        nacc_b = [Buf("nacc%d" % i) for i in range(4)]

        memset("pool", VSW[:, :, :, 64:65], 1.0, [vsw_b])
        for g in range(2):
            memset("pool", KW[g][64:96, :], 0.0, [KW_b[g]])
            for p in range(4):
                memset("pool", QS[g][p][64:96, :], 0.0, QS_b[g][p])
        dma("pool", KS[0][64:96, :], erows_d[:, :], (), [KS_b[0]])
        dma("pool", KS[1][64:96, :], erows_d[:, :], (), [KS_b[1]])
        wv, wv_b = load_w("pool", w_nv[:, :], 2048)
        wv3 = wv[:, 0:2048].rearrange("p (a b) -> p a b", b=256)
        for ti in range(NT):
            c = ti // 4
            bank, bb = pj_ring.next()
            for kc in range(8):
                mm(bank[:, 0:256], hT[:, kc, ti * 128:(ti + 1) * 128], wv3[:, kc, :], kc == 0, kc == 7,
                   [hT_b[kc][c], wv_b], [bb])
            cp("dve", VSW[:, ti, :, 0:64], bank[:, 0:256].rearrange("p (a b) -> p a b", b=64), [bb], [vsw_b])

        def proj_fm(blk_plain, blk_swap, wsrc, dests):
            wp, wpb = load_w("pool", wsrc[blk_plain, :, :], 1024)
            wp3 = wp[:, 0:1024].rearrange("p (a b) -> p a b", b=128)
            if blk_swap is not None:
                wq, wqb = load_w("pool", wsrc[blk_swap, :, :], 1024)
                wq3 = wq[:, 0:1024].rearrange("p (a b) -> p a b", b=128)
            for c in range(4):
                b1, bb1 = pj_ring.next()
                for kc in range(8):
                    mm(b1[:, :], wp3[:, kc, :], hT[:, kc, c * 512:(c + 1) * 512], kc == 0, kc == 7,
                       [wpb, hT_b[kc][c]], [bb1])
                dests_c = [(t_, (b_[c] if isinstance(b_, list) else b_), r1_, r2_) for (t_, b_, r1_, r2_) in dests]
                if blk_swap is None:
                    for (tile_, tb_, rs, rd_) in dests_c:
                        cp("act", tile_[rd_, c * 512:(c + 1) * 512], b1[rs, :], [bb1], [tb_])
                    continue
                b2, bb2 = pj_ring.next()
                for kc in range(8):
                    mm(b2[:, :], wq3[:, kc, :], hT[:, kc, c * 512:(c + 1) * 512], kc == 0, kc == 7,
                       [wqb, hT_b[kc][c]], [bb2])
                rp, rpb = rope_ring.next()
                dma("sp", rp[:], rope_d[c, :, :], (), [rpb])
                ta, tab = t1_ring.next()
                tb, tbb = t1_ring.next()
                tt("dve", ta[:], b1[:, :], rp[:, 0:512], ALU.mult, [bb1, rpb], [tab])
                tt("dve", tb[:], b2[:, :], rp[:, 512:1024], ALU.mult, [bb2, rpb], [tbb])
                for (tile_, tb_, rs, rd_) in dests_c:
                    if rs == rd_:
                        tt("pool", tile_[rd_, c * 512:(c + 1) * 512], ta[rs, :], tb[rs, :], ALU.add, [tab, tbb], [tb_])
                    else:
                        tt("pool", ta[rs, :], ta[rs, :], tb[rs, :], ALU.add, [tab, tbb], [tab])
                        cp("act", tile_[rd_, c * 512:(c + 1) * 512], ta[rs, :], [tab], [tb_])

        for p in range(4):
            proj_fm(p, 4 + p, w_nq, [(QS[0][p], QS_b[0][p], slice(0, 64), slice(0, 64)),
                                     (QS[1][p], QS_b[1][p], slice(64, 128), slice(0, 64))])
        proj_fm(0, 1, w_nk, [(KC, KC_b, slice(0, 128), slice(0, 128))])
        proj_fm(2, 3, w_nk, [(KS[0], KS_b[0], slice(0, 64), slice(0, 64)), (KS[1], KS_b[1], slice(64, 128), slice(0, 64))])
        proj_fm(4, 5, w_nk, [(KW[0], KW_b[0], slice(0, 64), slice(0, 64)), (KW[1], KW_b[1], slice(64, 128), slice(0, 64))])
        proj_fm(6, None, w_nk, [(VC, VC_b, slice(0, 128), slice(0, 128))])

        for (src, src_b, w1, cb, sT) in ((KC, KC_b, w1k, cbk, sTk), (VC, VC_b, w1v, cbv, sTv)):
            for g in range(2):
                rs = slice(64 * g, 64 * g + 64)
                bank, bb = tp_ring.next()
                for l in range(32):
                    mm(bank[0:64, 0:127], w1[rs, l, :], src[rs, l:l + 2017:16], l == 0, l == 31,
                       [const_b, src_b], [bb])
                act(sT[:, g, 0:127], bank[0:64, 0:127], AF.Silu, [bb, const_b], [const_b], bias=cb[:])
        for g in range(2):
            bank, bb = tp_ring.next()
            mm(bank[0:64, 0:128], w2k[:], sTk[:, g, :], True, True, [const_b], [bb])
            cp("dve", kcmp[0:64, g, :], bank[0:64, 0:128], [bb], [const_b])
        for g in range(2):
            bank, bb = tp_ring.next()
            mm(bank[:, 0:64], sTv[:, g, :], w2v[:], True, True, [const_b], [bb])
            cp("dve", vcmp[:, g, 0:64], bank[:, 0:64], [bb], [const_b])

        P.barrier()
        psl_b = Buf("psl")
        mbw_b = Buf("mbw")
        mbT_b = Buf("mbT")
        ocmp = [xt_ring.items[k][0][:, :].rearrange("p (a b) -> p a b", b=512) for k in range(2)]
        ocmp_b = [xt_ring.items[k][1] for k in range(2)]

        def bc(ap2, n):
            return ap2.unsqueeze(2).broadcast_to([128, ap2.shape[1], n])

        noop = noop_

        def add_cmp_sel(qn):
            for g in range(2):
                for p in range(4):
                    h = 4 * g + p

                    def fin_cmp(qc, o3, ob, h=h, p=p):
                        sm, smb = sm_ring.next()
                        hs = slice(h * 64, (h + 1) * 64)
                        ts("dve", sm[:, 0:4], o3[:, :, 64], 1e-30, None, ALU.max, None, [ob], [smb])
                        P.add("dve", "reciprocal", dict(out=sm[:, 4:8], in_=sm[:, 0:4]), [smb], [smb])
                        tt("dve", sm[:, 8:12], sm[:, 4:8], gates[:, qc * 4:qc * 4 + 4, 3 * h], ALU.mult,
                           [smb, const_b], [smb])
                        for k in range(2):
                            tt("dve", ocmp[k][:, :, hs], o3[:, 2 * k:2 * k + 2, 0:64], bc(sm[:, 8 + 2 * k:10 + 2 * k], 64),
                               ALU.mult, [ob, smb], [ocmp_b[k]])
                        if p == 0:
                            tt("dve", psl[:, :, :], o3[:, :, 65:97], bc(sm[:, 4:8], 32), ALU.mult, [ob, smb], [psl_b])
                        else:
                            tmp, tmpb = t1_ring.next()
                            tmp3 = tmp[:, 0:128].rearrange("p (a b) -> p a b", b=32)
                            tt("dve", tmp3, o3[:, :, 65:97], bc(sm[:, 4:8], 32), ALU.mult, [ob, smb], [tmpb])
                            tt("dve", psl[:, :, :], psl[:, :, :], tmp3, ALU.add, [psl_b, tmpb], [psl_b])

                    attn_chunk(qn, QS[g][p], QS_b[g][p][qn], kcmp[:, g, :], const_b, slice(0, 96),
                               [(0, 0, 512, (cmpmask_b[:, qn * 512:(qn + 1) * 512], 0, 512))],
                               lambda kt, g=g: vcmp[:, g, :], 97, const_b, None, fin_cmp)

                def selection(g=g):
                    sc, scb = t1_ring.next()
                    sc3 = sc[:, 0:128].rearrange("p (a b) -> p a b", b=32)
                    sm, smb = sm_ring.next()
                    tt("dve", sc3, psl[:, :, :], fbt[:, qn * 4:qn * 4 + 4, :], ALU.add, [psl_b, const_b], [scb])
                    for qs in range(4):
                        P.add("dve", "max", dict(out=sm[:, 8 * qs:8 * qs + 8], in_=sc3[:, qs, :]), [scb], [smb])
                    tt("dve", sc3, sc3, bc(sm[:, 7:32:8], 32), ALU.is_lt, [scb, smb], [scb])
                    ts("dve", mbw[:, g, :, 64:96], sc3, NEGM, None, ALU.mult, None, [scb], [mbw_b])
                    bank, bb = tp_ring.next()
                    bk = bank[:].bitcast(BF16)
                    for qs in range(4):
                        tr(bk[0:96, qs * 128:(qs + 1) * 128], mbw[:, g, qs, :], ident_b[:], [mbw_b, const_b], [bb])
                    cp("dve", mbT[:, g, :], bk[0:96, 0:512], [bb], [mbT_b])
                    for p in range(4):
                        cp("pool", QS[g][p][64:96, qn * 512:(qn + 1) * 512], mbT[64:96, g, :], [mbT_b],
                           [QS_b[g][p][qn]])

                pipe.append(dict(S=noop, E=noop, PV=noop, pre=None, post=selection))

        def add_sel_win(qc, which):
            for g in range(2):
                for p in range(4):
                    h = 4 * g + p

                    def fin_acc(br, h=h):
                        def f(qc, o3, ob):
                            sm, smb = sm_ring.next()
                            hs = slice(h * 64, (h + 1) * 64)
                            P.add("dve", "reciprocal", dict(out=sm[:, 0:4], in_=o3[:, :, 64]), [ob], [smb])
                            tt("dve", sm[:, 4:8], sm[:, 0:4], gates[:, qc * 4:qc * 4 + 4, 3 * h + br], ALU.mult,
                               [smb, const_b], [smb])
                            tmp, tmpb = t1_ring.next()
                            tmp3 = tmp[:, 0:256].rearrange("p (a b) -> p a b", b=64)
                            tt("dve", tmp3, o3[:, :, 0:64], bc(sm[:, 4:8], 64), ALU.mult, [ob, smb], [tmpb])
                            if br == 1:
                                for k in range(2):
                                    tt("dve", nsa_acc[:, 2 * k:2 * k + 2, hs], tmp3[:, 2 * k:2 * k + 2, :],
                                       ocmp[k][:, :, hs], ALU.add, [tmpb, ocmp_b[k]], nacc_b[2 * k:2 * k + 2])
                            else:
                                tt("dve", nsa_acc[:, :, hs], nsa_acc[:, :, hs], tmp3, ALU.add, [tmpb] + nacc_b, nacc_b)
                        return f

                    if which == 1:
                        attn_chunk(qc, QS[g][p], QS_b[g][p][qc], KS[g], KS_b[g], slice(0, 96), causal_tiles(qc),
                                   lambda kt, g=g: VSW[:, kt, g, :], 65, vsw_b, None, fin_acc(1))
                    else:
                        attn_chunk(qc, QS[g][p], QS_b[g][p][qc], KW[g], KW_b[g], slice(0, 96), window_tiles(qc),
                                   lambda kt, g=g: VSW[:, kt, 2 + g, :], 65, vsw_b, None, fin_acc(2))

        add_cmp_sel(0)
        attn_flush()
        for qc in range(4):
            add_sel_win(qc, 1)
            if qc + 1 < 4:
                add_cmp_sel(qc + 1)
            add_sel_win(qc, 2)
            attn_flush()
            for qs in range(4):
                ti = qc * 4 + qs
                hn, hnb = hn_ring.next()
                cp("pool", hn[:, 0:512], nsa_acc[:, qs, :], [nacc_b[qs]], [hnb])
                if dbg and s == 0:
                    dma("sp", dbg_d["d_nsa"][:, ti * 512:(ti + 1) * 512], nsa_acc[:, qs, :], [nacc_b[qs]], [],
                        final=True)
                bank, bb = tp_ring.next()
                bk = bank[:].bitcast(BF16)
                for kc in range(4):
                    tr(bk[:, kc * 128:(kc + 1) * 128], hn[:, kc * 128:(kc + 1) * 128], ident_b[:],
                       [hnb, const_b], [bb])
                cp("dve", nsaT[:, :, ti * 128:(ti + 1) * 128],
                   bk[:, 0:512].rearrange("p (a b) -> p a b", b=128), [bb], [nsaT_b])
        P.barrier()

        mixT = aview(0, 16384).rearrange("p (a b) -> p a b", b=T)
        mixT_b = [[Buf("mix%d_%d" % (c, t4)) for t4 in range(4)] for c in range(8)]
        for cb_ in range(8):
            wga, wga_b = load_w("pool", w_gab[cb_, :, :], 1024)
            wgb, wgb_b = load_w("pool", w_gab[8 + cb_, :, :], 1024)
            wfo, wfo_b = load_w("pool", w_fo[cb_, :, :], 512)
            wno, wno_b = load_w("pool", w_no[cb_, :, :], 512)
            wga3 = wga[:, 0:1024].rearrange("p (a b) -> p a b", b=128)
            wgb3 = wgb[:, 0:1024].rearrange("p (a b) -> p a b", b=128)
            wfo3 = wfo[:, 0:512].rearrange("p (a b) -> p a b", b=128)
            wno3 = wno[:, 0:512].rearrange("p (a b) -> p a b", b=128)
            for c in range(4):
                cs = slice(c * 512, (c + 1) * 512)
                res = []
                for (wg3_, wgb_, wo3_, wob_, srcT, srcb) in ((wga3, wga_b, wfo3, wfo_b, foxT, foxT_b),
                                                           (wgb3, wgb_b, wno3, wno_b, nsaT, nsaT_b)):
                    b1, bb1 = pj_ring.next()
                    for kc in range(8):
                        mm(b1[:, :], wg3_[:, kc, :], hT[:, kc, cs], kc == 0, kc == 7, [wgb_, hT_b[kc][c]], [bb1])
                    sg, sgb = t1_ring.next()
                    act(sg[:], b1[:, :], AF.Sigmoid, [bb1], [sgb])
                    b2, bb2 = pj_ring.next()
                    for kc in range(4):
                        mm(b2[:, :], wo3_[:, kc, :], srcT[:, kc, cs], kc == 0, kc == 3, [wob_, srcb], [bb2])
                    tt("dve", sg[:], sg[:], b2[:, :], ALU.mult, [sgb, bb2], [sgb])
                    res.append((sg, sgb))
                tt("pool", mixT[:, cb_, cs], res[0][0][:], res[1][0][:], ALU.add, [res[0][1], res[1][1]],
                   [mixT_b[cb_][c]])
        P.barrier()

        dma("sp", gpost1[:], gpost1_d.partition_broadcast(128), (), [gpost_b])
        WO = aview(16384, 8192).rearrange("p (h a b) -> p h a b", h=2, a=8)
        wo_b = Buf("WO")
        for hf in range(2):
            dma("pool", WO[:, hf, :, :].rearrange("p a b -> p (a b)"), w_o[hf, :, :], (), [wo_b])
        for ti in range(NT):
            c = ti // 4
            yt, yb = yt_ring.next()
            for hf in range(2):
                bank, bb = pj_ring.next()
                for kc in range(8):
                    mm(bank[:, :], mixT[:, kc, ti * 128:(ti + 1) * 128], WO[:, hf, kc, :], kc == 0, kc == 7,
                       [mixT_b[kc][c], wo_b], [bb])
                cp("act", yt[:, hf * 512:(hf + 1) * 512], bank[:, :], [bb], [yb])
            sm, smb = sm_ring.next()
            act(junk[:], yt[:], AF.Square, [yb], [junk_b, smb], accum_out=sm[:, 0:1])
            ts("dve", sm[:, 1:2], sm[:, 0:1], 1.0 / D, 1e-6, ALU.mult, ALU.add, [smb], [smb])
            act(sm[:, 2:3], sm[:, 1:2], AF.Ln, [smb], [smb])
            act(sm[:, 3:4], sm[:, 2:3], AF.Exp, [smb], [smb], scale=-0.5)
            stt(yt[:], yt[:], sm[:, 3:4], gpost1[:], ALU.mult, ALU.mult, [yb, smb, gpost_b], [yb])
            xt, xb = xt_ring.next()
            dma("sp", xt[:], xs[ti * 128:(ti + 1) * 128, :], (), [xb])
            tt("pool", xt[:], xt[:], yt[:], ALU.add, [xb, yb], [xb])
            dma("sp", outs[ti * 128:(ti + 1) * 128, :], xt[:], [xb], [outD_b[ti]])
            norm_to_hT(xt[:], xb, ti, gpre2)
        P.barrier()

        dma("sp", gpost2[:], gpost2_d.partition_broadcast(128), (), [gpost_b])
        yacc = aview(0, 32768, F32).rearrange("p (a b) -> p a b", b=1024)
        yacc_b = [[Buf("yacc%d_%d" % (i, hf)) for hf in range(2)] for i in range(NT)]
        uT = [aview(32768 + i * 4096, 4096).rearrange("p (a b) -> p a b", b=T) for i in range(2)]
        uT_b = [[[Buf("uT%d_%d_%d" % (i, f, c)) for c in range(4)] for f in range(2)] for i in range(2)]
        def mlp_up(fg):
            wu, wu_b = load_w("pool", w_up[fg, :, :], 2048)
            wu3 = wu[:, 0:2048].rearrange("p (a b) -> p a b", b=256)
            ub = fg % 2
            for fc in range(2):
                for c in range(4):
                    cs = slice(c * 512, (c + 1) * 512)
                    bank, bb = pj_ring.next()
                    for kc in range(8):
                        mm(bank[:, :], wu3[:, kc, fc * 128:(fc + 1) * 128], hT[:, kc, cs], kc == 0, kc == 7,
                           [wu_b, hT_b[kc][c]], [bb])
                    r, rb = t1_ring.next()
                    act(r[:], bank[:, :], AF.Relu, [bb], [rb])
                    act(uT[ub][:, fc, cs], r[:], AF.Square, [rb], [uT_b[ub][fc][c]])

        mlp_up(0)
        for fg in range(16):
            wd, wd_b = load_w("pool", w_dn[fg, :, :], 2048)
            wd3 = wd[:, 0:2048].rearrange("p (a b) -> p a b", b=1024)
            ub = fg % 2
            if fg + 1 < 16:
                mlp_up(fg + 1)
            for ti in range(NT):
                c = ti // 4
                for hf in range(2):
                    bank, bb = y_ring.next()
                    for fc in range(2):
                        mm(bank[:, :], uT[ub][:, fc, ti * 128:(ti + 1) * 128], wd3[:, fc, hf * 512:(hf + 1) * 512],
                           fc == 0, fc == 1, [uT_b[ub][fc][c], wd_b], [bb])
                    ya = yacc[:, ti, hf * 512:(hf + 1) * 512]
                    if fg == 0:
                        cp("act", ya, bank[:, :], [bb], [yacc_b[ti][hf]])
                    else:
                        tt("dve", ya, bank[:, :], ya, ALU.add, [bb, yacc_b[ti][hf]], [yacc_b[ti][hf]])
        for ti in range(NT):
            sm, smb = sm_ring.next()
            act(junk[:], yacc[:, ti, :], AF.Square, yacc_b[ti], [junk_b, smb], accum_out=sm[:, 0:1])
            ts("dve", sm[:, 1:2], sm[:, 0:1], 1.0 / D, 1e-6, ALU.mult, ALU.add, [smb], [smb])
            act(sm[:, 2:3], sm[:, 1:2], AF.Ln, [smb], [smb])
            act(sm[:, 3:4], sm[:, 2:3], AF.Exp, [smb], [smb], scale=-0.5)
            yt, yb = yt_ring.next()
            stt(yt[:], yacc[:, ti, :], sm[:, 3:4], gpost2[:], ALU.mult, ALU.mult, yacc_b[ti] + [smb, gpost_b], [yb])
            xt, xb = xt_ring.next()
            dma("sp", xt[:], outs[ti * 128:(ti + 1) * 128, :], [outD_b[ti]], [xb])
            tt("pool", xt[:], xt[:], yt[:], ALU.add, [xb, yb], [xb])
            dma("sp", outs[ti * 128:(ti + 1) * 128, :], xt[:], [xb], [outD_b[ti]], final=True)
        P.barrier()

    P.finalize()
    P.emit(nc, es)
    es.close()
    return nc


def _blk(w, cols):
    sub = w[:, cols]
    n = sub.shape[1]
    return np.ascontiguousarray(sub.reshape(8, 128, n).transpose(1, 0, 2).reshape(128, 8 * n))


def _rowblk(w, nkc):
    n = w.shape[1]
    return np.ascontiguousarray(w.reshape(nkc, 128, n).transpose(1, 0, 2).reshape(128, nkc * n))


def _const_tables():
    f32 = np.float32
    inv = np.power(f32(10000.0), -np.arange(0, 64, 2, dtype=f32) / f32(64)).astype(f32)
    ang = (np.arange(T, dtype=f32)[:, None] * inv[None, :]).astype(f32)
    cos = np.cos(ang).astype(f32).T
    sin = np.sin(ang).astype(f32).T
    rope = np.zeros((4, 128, 1024), f32)
    for r in range(128):
        j = r % 32
        sgn = -1.0 if (r % 64) < 32 else 1.0
        for c in range(4):
            rope[c, r, 0:512] = cos[j, c * 512:(c + 1) * 512]
            rope[c, r, 512:1024] = sgn * sin[j, c * 512:(c + 1) * 512]
    n = np.arange(128)[:, None]
    t = np.arange(T)[None, :]
    cmpmask = np.where((16 * n + 31 <= t) & (n < 127), 0.0, NEGM).astype(f32)
    tt_ = np.arange(T)
    cur = tt_ // 64
    jj = np.arange(32)[None, :]
    forced = (jj == 0) | (jj == cur[:, None]) | (jj == cur[:, None] - 1)
    valid = jj <= cur[:, None]
    fb = np.where(valid, 1e4 * forced.astype(f32), -1e30).astype(f32)
    fbt = np.ascontiguousarray(fb.reshape(16, 128, 32).transpose(1, 0, 2).reshape(128, 512))
    sI = np.arange(128)[:, None]
    tI = np.arange(128)[None, :]
    tri = np.concatenate([np.where(sI > tI, NEGM, 0.0), np.where(tI >= sI, NEGM, 0.0)], axis=1).astype(f32)
    ident = np.eye(128, dtype=f32)
    umat = (sI <= tI).astype(f32)
    ci = np.arange(128)[:, None] * 16
    sj = np.arange(32)[None, :] * 64
    ov = (((ci < sj + 64) & (ci + 32 > sj)) & (np.arange(128)[:, None] < 127)).astype(f32)
    erows = (np.arange(32)[:, None] == (np.arange(T)[None, :] // 64)).astype(f32)
    return dict(rope=rope, cmpmask=cmpmask, fbt=fbt, tri=tri, ident=ident, umat=umat, ov=ov, erows=erows)


def _layout_weights(inp):
    f = lambda a: np.asarray(a, dtype=np.float32)
    w_in = f(inp["w_in"])[0]
    o = {}
    ar = np.arange
    FQ, FK, FV, FF, NQ = 0, 512, 1024, 1536, 1544
    KCo, VCo, KSo, VSo, KWo, VWo, NG, GA, GB = 2056, 2184, 2312, 2440, 2568, 2696, 2824, 2848, 3872
    o["w_fqk"] = np.stack([_blk(w_in, FQ + 128 * p + ar(128)) for p in range(4)] +
                          [_blk(w_in, FK + 128 * p + ar(128)) for p in range(4)])
    o["w_fv"] = np.stack([_blk(w_in, FV + 256 * hf + ar(256)) for hf in range(2)])
    o["w_ffng"] = _blk(w_in, np.concatenate([FF + ar(8), NG + ar(24)]))
    sw = (ar(64) + 32) % 64
    nq_plain, nq_swap = [], []
    for p in range(4):
        nq_plain.append(_blk(w_in, np.concatenate([NQ + 64 * p + ar(64), NQ + 64 * (4 + p) + ar(64)])))
        nq_swap.append(_blk(w_in, np.concatenate([NQ + 64 * p + sw, NQ + 64 * (4 + p) + sw])))
    o["w_nq"] = np.stack(nq_plain + nq_swap)
    sw2 = np.concatenate([sw, 64 + sw])
    o["w_nk"] = np.stack([_blk(w_in, KCo + ar(128)), _blk(w_in, KCo + sw2),
                          _blk(w_in, KSo + ar(128)), _blk(w_in, KSo + sw2),
                          _blk(w_in, KWo + ar(128)), _blk(w_in, KWo + sw2),
                          _blk(w_in, VCo + ar(128))])
    o["w_nv"] = _blk(w_in, np.concatenate([VSo + ar(128), VWo + ar(128)]))
    o["w_gab"] = np.stack([_blk(w_in, GA + 128 * c + ar(128)) for c in range(8)] +
                          [_blk(w_in, GB + 128 * c + ar(128)) for c in range(8)])
    wfo = f(inp["w_fox_out"])[0]
    wno = f(inp["w_nsa_out"])[0]
    o["w_fo"] = np.stack([_rowblk(wfo[:, 128 * c:128 * (c + 1)], 4) for c in range(8)])
    o["w_no"] = np.stack([_rowblk(wno[:, 128 * c:128 * (c + 1)], 4) for c in range(8)])
    wo = f(inp["w_o"])[0]
    o["w_o"] = np.stack([_rowblk(wo[:, 512 * h:512 * (h + 1)], 8) for h in range(2)])
    wup = f(inp["w_up"])[0]
    wdn = f(inp["w_down"])[0]
    o["w_up"] = np.stack([_blk(wup, 256 * g + ar(256)) for g in range(16)])
    o["w_dn"] = np.stack([_rowblk(wdn[256 * g:256 * (g + 1), :], 2) for g in range(16)])
    for nm, key in (("w1k", "cmp_w1_k"), ("w1v", "cmp_w1_v")):
        w1 = f(inp[key])[0].reshape(32, 64, 64).transpose(1, 0, 2).reshape(64, 2048)
        o[nm] = np.ascontiguousarray(np.concatenate([w1, w1], axis=0))
    o["w2k"] = np.ascontiguousarray(f(inp["cmp_w2_k"])[0])
    o["w2v"] = np.ascontiguousarray(f(inp["cmp_w2_v"])[0])
    o["posk"] = np.ascontiguousarray(f(inp["cmp_pos_k"])[0].T)
    o["posv"] = np.ascontiguousarray(f(inp["cmp_pos_v"])[0].T)
    o["gpre1"] = np.ascontiguousarray(f(inp["norm_mix_pre"])[0].reshape(8, 128).T)
    o["gpre2"] = np.ascontiguousarray(f(inp["norm_mlp_pre"])[0].reshape(8, 128).T)
    o["gpost1"] = np.ascontiguousarray(f(inp["norm_mix_post"])[0].reshape(1, 1024))
    o["gpost2"] = np.ascontiguousarray(f(inp["norm_mlp_post"])[0].reshape(1, 1024))
    o["bfor"] = np.ascontiguousarray(f(inp["b_forget"])[0].reshape(1, 8))
    o.update(_const_tables())
    return o


_NC_CACHE = {}


def kernel(**inputs):
    x = np.asarray(inputs["x"], dtype=np.float32)
    B = x.shape[0]
    per = B // NCORES
    shared = _layout_weights(inputs)
    if per not in _NC_CACHE:
        _NC_CACHE[per] = build_program(per)
    nc = _NC_CACHE[per]
    in_maps = []
    for c in range(NCORES):
        m = dict(shared)
        m["x"] = np.ascontiguousarray(x[c * per:(c + 1) * per].reshape(per * T, D))
        in_maps.append(m)
    res = run_bass_kernel_spmd(nc, in_maps, core_ids=list(range(NCORES)))
    outs = [np.asarray(r["out"]).reshape(per, T, D) for r in res.results]
    return np.concatenate(outs, axis=0).astype(np.float32)
```

```python
import numpy as np
from contextlib import ExitStack
import concourse.bass as bass
import concourse.mybir as mybir
from concourse.bass_utils import run_bass_kernel_spmd

F32 = mybir.dt.float32
BF16 = mybir.dt.bfloat16
AF = mybir.ActivationFunctionType
ALU = mybir.AluOpType

T = 2048
D = 1024
NT = 16
NEGM = -30000.0
SEQ_PER_CORE = 4
NCORES = 8


class Buf:
    __slots__ = ("name", "last_w", "readers", "dma_readers")

    def __init__(self, name):
        self.name = name
        self.last_w = None
        self.readers = {}
        self.dma_readers = []


class Op:
    __slots__ = ("eng", "meth", "kw", "deps", "idx", "dma", "tok", "marked", "semval")


COMPUTE = ("pe", "act", "dve", "pool")
QUEUES = ("sp", "pool")
NDSEM = 12


class Prog:
    def __init__(self):
        self.streams = {e: [] for e in ("pe", "act", "dve", "pool", "sp")}
        self.ndma = {q: 0 for q in QUEUES}
        self.dma_ops = {q: [] for q in QUEUES}
        self.barrier_deps = {e: [] for e in self.streams}
        self.dmas_since_barrier = []
        self.final_deps = []

    def add(self, eng, meth, kw, reads=(), writes=(), dma=False, prefetch=False, final=False):
        op = Op()
        op.eng, op.meth, op.kw, op.dma = eng, meth, kw, dma
        op.marked = False
        op.semval = None
        stream = self.streams[eng]
        op.idx = len(stream)
        deps = set()
        for b in reads:
            if b.last_w is not None:
                deps.add(b.last_w)
        for b in writes:
            if b.last_w is not None:
                deps.add(b.last_w)
            for e, i in b.readers.items():
                deps.add(("c", e, i))
            for t in b.dma_readers:
                deps.add(t)
        for t in self.barrier_deps[eng]:
            deps.add(t)
        self.barrier_deps[eng] = []
        if dma:
            j = self.ndma[eng]
            self.ndma[eng] += 1
            op.tok = ("d", eng, j)
            if j >= NDSEM:
                deps.add(("d", eng, j - NDSEM))
            self.dma_ops[eng].append(op)
            if not prefetch:
                self.dmas_since_barrier.append(op.tok)
            if final:
                self.final_deps.append(op.tok)
        else:
            op.tok = ("c", eng, op.idx)
        fdeps = []
        for d in deps:
            if d[0] == "c" and d[1] == eng and not dma:
                if eng == "pe":
                    continue
                fdeps.append(d)
            else:
                fdeps.append(d)
        op.deps = fdeps
        for b in reads:
            if dma:
                b.dma_readers.append(op.tok)
            else:
                b.readers[eng] = op.idx
        for b in writes:
            b.last_w = op.tok
            b.readers = {}
            b.dma_readers = []
        stream.append(op)
        return op

    def barrier(self):
        toks = []
        for e in COMPUTE:
            if self.streams[e]:
                last = self.streams[e][-1]
                if not last.dma:
                    toks.append(last.tok)
                else:
                    for o in reversed(self.streams[e]):
                        if not o.dma:
                            toks.append(o.tok)
                            break
        toks += self.dmas_since_barrier
        self.dmas_since_barrier = []
        for e in self.streams:
            self.barrier_deps[e] = list(self.barrier_deps[e]) + toks

    def finalize(self):
        op = self.add("sp", "nop_final", {}, (), ())
        op.deps = list(self.final_deps)
        for e, stream in self.streams.items():
            for op in stream:
                for d in op.deps:
                    if d[0] == "c":
                        self.streams[d[1]][d[2]].marked = True
        for e in COMPUTE:
            n = 0
            for op in self.streams[e]:
                if op.dma:
                    continue
                if op.marked:
                    n += 1
                    op.semval = n

    def emit(self, nc, es):
        esem = {e: es.enter_context(nc.semaphore("s_" + e)) for e in COMPUTE}
        dsem = {q: [es.enter_context(nc.semaphore("d_%s%d" % (q, i))) for i in range(NDSEM)]
                for q in QUEUES}
        block = es.enter_context(nc.Block())
        streams = self.streams

        def tokwait(d):
            if d[0] == "c":
                return ("c" + d[1], esem[d[1]], streams[d[1]][d[2]].semval)
            q, j = d[1], d[2]
            return ("d%s%d" % (q, j % NDSEM), dsem[q][j % NDSEM], 16 * (j // NDSEM + 1))

        def run(eng, name):
            waited = {}
            for op in streams[name]:
                for d in op.deps:
                    key, sem, val = tokwait(d)
                    if waited.get(key, 0) >= val:
                        continue
                    waited[key] = val
                    eng.wait_ge(sem, val)
                if op.meth == "nop_final":
                    continue
                ins = getattr(eng, op.meth)(**op.kw)
                if op.dma:
                    j = op.tok[2]
                    ins.then_inc(dsem[name][j % NDSEM], 16)
                elif op.marked:
                    ins.then_inc(esem[name], 1)

        @block.sync
        def _(e):
            run(e, "sp")

        @block.gpsimd
        def _(e):
            run(e, "pool")

        @block.scalar
        def _(e):
            run(e, "act")

        @block.vector
        def _(e):
            run(e, "dve")

        @block.tensor
        def _(e):
            run(e, "pe")


class Ring:
    def __init__(self, items):
        self.items = items
        self.i = 0

    def next(self):
        it = self.items[self.i % len(self.items)]
        self.i += 1
        return it


def build_program(nseq=SEQ_PER_CORE, dbg=False):
    nc = bass.Bass("TRN2", target_bir_lowering=False)
    P = Prog()
    es = ExitStack()

    def din(name, shape, dt=F32):
        return nc.dram_tensor(name, list(shape), dt, kind="ExternalInput").ap()

    x_d = din("x", [nseq * T, D])
    out_d = nc.dram_tensor("out", [nseq * T, D], F32, kind="ExternalOutput").ap()
    w_fqk = din("w_fqk", [8, 128, 1024])
    w_fv = din("w_fv", [2, 128, 8 * 256])
    w_ffng = din("w_ffng", [128, 8 * 32])
    w_nq = din("w_nq", [8, 128, 1024])
    w_nk = din("w_nk", [7, 128, 1024])
    w_nv = din("w_nv", [128, 8 * 256])
    w_gab = din("w_gab", [16, 128, 1024])
    w_fo = din("w_fo", [8, 128, 512])
    w_no = din("w_no", [8, 128, 512])
    w_o = din("w_o", [2, 128, 8 * 512])
    w_up = din("w_up", [16, 128, 8 * 256])
    w_dn = din("w_dn", [16, 128, 2 * 1024])
    w1k_d = din("w1k", [128, 2048])
    w1v_d = din("w1v", [128, 2048])
    w2k_d = din("w2k", [64, 64])
    w2v_d = din("w2v", [64, 64])
    posk_d = din("posk", [64, 32])
    posv_d = din("posv", [64, 32])
    rope_d = din("rope", [4, 128, 1024])
    cmpmask_d = din("cmpmask", [128, 2048])
    fbt_d = din("fbt", [128, 16 * 32])
    tri_d = din("tri", [128, 256])
    ident_d = din("ident", [128, 128])
    u_d = din("umat", [128, 128])
    ov_d = din("ov", [128, 32])
    erows_d = din("erows", [32, 2048])
    gpre1_d = din("gpre1", [128, 8])
    gpre2_d = din("gpre2", [128, 8])
    gpost1_d = din("gpost1", [1, 1024])
    gpost2_d = din("gpost2", [1, 1024])
    bfor_d = din("bfor", [1, 8])
    dbg_d = {}
    if dbg:
        for nm, shp in (("d_fox", [128, 16 * 512]), ("d_nsa", [128, 16 * 512]),
                        ("d_negc", [128, 128])):
            dbg_d[nm] = nc.dram_tensor(nm, shp, F32, kind="ExternalOutput").ap()

    def sb(name, shape, dt):
        return es.enter_context(nc.sbuf_tensor(name, list(shape), dt))

    def ps(name, shape, dt):
        return es.enter_context(nc.psum_tensor(name, list(shape), dt))

    hT = sb("hT", [128, 8, T], BF16)
    hT_b = [[Buf("hT%d_%d" % (k, c)) for c in range(4)] for k in range(8)]
    ARENA_ELEMS = 50176
    arena = sb("arena", [128, ARENA_ELEMS], BF16)
    ident_f = sb("ident_f", [128, 128], F32)
    ident_b = sb("ident_b", [128, 128], BF16)
    u_f = sb("u_f", [128, 128], F32)
    ones_f = sb("ones_f", [128, 128], F32)
    zeros_b = sb("zeros_b", [128, 512], BF16)
    tri_b = sb("tri_b", [128, 256], BF16)
    cmpmask_b = sb("cmpmask_b", [128, T], BF16)
    fbt = sb("fbt_s", [128, 16, 32], F32)
    gpre1 = sb("gpre1_s", [128, 8], F32)
    gpre2 = sb("gpre2_s", [128, 8], F32)
    bfor = sb("bfor_s", [128, 8], F32)
    onescol = sb("onescol", [128, 1], F32)
    w1k = sb("w1k_s", [128, 32, 64], BF16)
    w1v = sb("w1v_s", [128, 32, 64], BF16)
    w2k = sb("w2k_s", [64, 64], BF16)
    w2v = sb("w2v_s", [64, 64], BF16)
    posk = sb("posk_s", [64, 32], BF16)
    posv = sb("posv_s", [64, 32], BF16)
    cbk = sb("cbk", [64, 1], F32)
    cbv = sb("cbv", [64, 1], F32)
    vcmp = sb("vcmp", [128, 2, 97], BF16)
    kcmp = sb("kcmp", [96, 2, 128], BF16)
    sTk = sb("sTk", [64, 2, 128], BF16)
    sTv = sb("sTv", [64, 2, 128], BF16)
    mbw = sb("mbw", [128, 2, 4, 96], BF16)
    mbT = sb("mbT", [96, 2, 512], BF16)
    const_b = Buf("consts")

    lp = sb("lp", [128, 16, 8], F32)
    lsum = sb("lsum", [128, 17, 8], F32)
    negc = sb("negc", [128, 16, 8], F32)
    caugT = sb("caugT", [33, T], BF16)
    gates = sb("gates", [128, 16, 24], F32)
    psl = sb("psl", [128, 4, 32], F32)

    xt_ring = Ring([(sb("xt%d" % i, [128, 1024], F32), Buf("xt%d" % i)) for i in range(2)])
    t1_ring = Ring([(sb("t1_%d" % i, [128, 512], F32), Buf("t1_%d" % i)) for i in range(3)])
    hn_ring = Ring([(sb("hn%d" % i, [128, 1024], BF16), Buf("hn%d" % i)) for i in range(2)])
    junk_b = Buf("junk")
    pt_ring = Ring([(sb("pt%d" % i, [128, 512], BF16), Buf("pt%d" % i)) for i in range(4)])
    wt_ring = Ring([(sb("wt%d" % i, [128, 2048], BF16), Buf("wt%d" % i)) for i in range(4)])
    rope_ring = Ring([(sb("rp%d" % i, [128, 1024], F32), Buf("rp%d" % i)) for i in range(1)])
    sm_ring = Ring([(sb("sm%d" % i, [128, 64], F32), Buf("sm%d" % i)) for i in range(8)])

    pj_ring = Ring([(ps("pj%d" % i, [128, 512], F32), Buf("pj%d" % i)) for i in range(2)])
    st_ring = Ring([(ps("st%d" % i, [128, 512], F32), Buf("st%d" % i)) for i in range(2)])
    sa_ring = Ring([st_ring.items[0], pj_ring.items[0], st_ring.items[1], pj_ring.items[1]])
    oa_ring = Ring([(ps("oa%d" % i, [128, 512], F32), Buf("oa%d" % i)) for i in range(2)])
    tp_ring = Ring([(ps("tp%d" % i, [128, 512], F32), Buf("tp%d" % i)) for i in range(2)])
    y_ring = Ring([st_ring.items[0], oa_ring.items[0], st_ring.items[1], oa_ring.items[1]])

    def mm(out, lhsT, rhs, start, stop, reads, writes):
        P.add("pe", "matmul", dict(out=out, lhsT=lhsT, rhs=rhs, start=start, stop=stop,
                                   skip_group_check=True), reads, writes)

    def tr(out, in_, ident, reads, writes):
        P.add("pe", "transpose", dict(out=out, in_=in_, identity=ident), reads, writes)

    def act(out, in_, func, reads, writes, **kw):
        P.add("act", "activation", dict(out=out, in_=in_, func=func, **kw), reads, writes)

    def dma(q, out, in_, reads, writes, **kw):
        extra = {}
        for k in ("prefetch", "final"):
            if k in kw:
                extra[k] = kw.pop(k)
        return P.add(q, "dma_start", dict(out=out, in_=in_, **kw), reads, writes, dma=True, **extra)

    def tt(eng, out, in0, in1, op, reads, writes):
        P.add(eng, "tensor_tensor", dict(out=out, in0=in0, in1=in1, op=op), reads, writes)

    def ts(eng, out, in0, s1, s2, op0, op1, reads, writes):
        kw = dict(out=out, in0=in0, scalar1=s1, scalar2=s2, op0=op0)
        if op1 is not None:
            kw["op1"] = op1
        P.add(eng, "tensor_scalar", kw, reads, writes)

    def stt(out, in0, scalar, in1, op0, op1, reads, writes):
        P.add("dve", "scalar_tensor_tensor", dict(out=out, in0=in0, scalar=scalar, in1=in1,
                                                  op0=op0, op1=op1), reads, writes)

    def cp(eng, out, in_, reads, writes):
        if eng == "act":
            P.add("act", "activation", dict(out=out, in_=in_, func=AF.Copy), reads, writes)
        else:
            P.add(eng, "tensor_copy", dict(out=out, in_=in_), reads, writes)

    def memset(eng, ap, val, writes):
        P.add(eng, "memset", dict(ap=ap, constant=val), (), writes)

    dma("sp", ident_f[:], ident_d[:, :], (), [const_b])
    dma("pool", ident_b[:], ident_d[:, :], (), [const_b])
    dma("sp", u_f[:], u_d[:, :], (), [const_b])
    dma("pool", tri_b[:], tri_d[:, :], (), [const_b])
    dma("pool", cmpmask_b[:], cmpmask_d[:, :], (), [const_b])
    dma("sp", fbt[:].rearrange("p a b -> p (a b)"), fbt_d[:, :], (), [const_b])
    dma("sp", gpre1[:], gpre1_d[:, :], (), [const_b])
    dma("sp", gpre2[:], gpre2_d[:, :], (), [const_b])
    dma("sp", bfor[:], bfor_d.partition_broadcast(128), (), [const_b])
    dma("pool", w1k[:].rearrange("p a b -> p (a b)"), w1k_d[:, :], (), [const_b])
    dma("pool", w1v[:].rearrange("p a b -> p (a b)"), w1v_d[:, :], (), [const_b])
    dma("pool", w2k[:], w2k_d[:, :], (), [const_b])
    dma("pool", w2v[:], w2v_d[:, :], (), [const_b])
    dma("pool", posk[:], posk_d[:, :], (), [const_b])
    dma("pool", posv[:], posv_d[:, :], (), [const_b])
    memset("pool", ones_f[:], 1.0, [const_b])
    memset("pool", zeros_b[:], 0.0, [const_b])
    memset("pool", onescol[:], 1.0, [const_b])
    memset("pool", caugT[32:33, :], 1.0, [const_b])
    memset("pool", vcmp[:].rearrange("p a b -> p (a b)"), 1.0, [const_b])
    for g in range(2):
        dma("pool", vcmp[:, g, 65:97], ov_d[:, :], (), [const_b])
    memset("pool", sTk[:].rearrange("p a b -> p (a b)"), 0.0, [const_b])
    memset("pool", sTv[:].rearrange("p a b -> p (a b)"), 0.0, [const_b])
    memset("pool", mbw[:].rearrange("p g a b -> p (g a b)"), 0.0, [const_b])
    memset("pool", lsum[:, 0, :], 0.0, [const_b])
    memset("pool", kcmp[64:96, :, :].rearrange("p a b -> p (a b)"), 0.0, [const_b])
    for (w1, pos, cb) in ((w1k, posk, cbk), (w1v, posv, cbv)):
        bank, bb = tp_ring.next()
        for l in range(32):
            mm(bank[0:64, 0:1], w1[0:64, l, :], pos[0:64, l:l + 1], l == 0, l == 31, [const_b], [bb])
        cp("dve", cb[:], bank[0:64, 0:1], [bb], [const_b])

    def aview(off, n, dt=BF16):
        v = arena[:, off:off + n]
        if dt == F32:
            v = v.bitcast(F32)
        return v

    junk = aview(49152, 1024)
    yt_ring = Ring([(aview(40960 + i * 2048, 2048, F32), Buf("yt%d" % i)) for i in range(2)])
    gpost1 = aview(45056, 2048, F32)
    gpost2 = aview(47104, 2048, F32)
    gpost_b = Buf("gpost")

    def norm_to_hT(src, src_b, tt_i, gpre):
        sm, smb = sm_ring.next()
        act(junk[:], src, AF.Square, [src_b], [junk_b, smb], accum_out=sm[:, 0:1])
        ts("dve", sm[:, 1:2], sm[:, 0:1], 1.0 / D, 1e-6, ALU.mult, ALU.add, [smb], [smb])
        act(sm[:, 2:3], sm[:, 1:2], AF.Ln, [smb], [smb])
        act(sm[:, 3:4], sm[:, 2:3], AF.Exp, [smb], [smb], scale=-0.5)
        hn, hnb = hn_ring.next()
        ts("dve", hn[:], src, sm[:, 3:4], None, ALU.mult, None, [src_b, smb], [hnb])
        bank, bb = tp_ring.next()
        bk = bank[:].bitcast(BF16)
        for kc in range(8):
            tr(bk[:, kc * 128:(kc + 1) * 128], hn[:, kc * 128:(kc + 1) * 128], ident_b[:], [hnb, const_b], [bb])
        c = tt_i // 4
        for kc in range(8):
            act(hT[:, kc, tt_i * 128:(tt_i + 1) * 128], bk[:, kc * 128:(kc + 1) * 128], AF.Copy,
                [bb, const_b], [hT_b[kc][c]], scale=gpre[:, kc:kc + 1])

    def load_w(q, src2d, ncols_elems, prefetch=True):
        wt, wb = wt_ring.next()
        dma(q, wt[:, 0:ncols_elems], src2d, (), [wb], prefetch=prefetch)
        return wt, wb

    pipe = []
    HEAT = 0

    def noop_():
        return None

    def attn_flush():
        n = len(pipe)
        if n == 0:
            return
        LA = 2
        for j in range(min(LA, n)):
            pipe[j]["S"]()
        for i, t in enumerate(pipe):
            t["E"]()
            if i + LA < n:
                pipe[i + LA]["S"]()
            if t["pre"] is not None:
                t["pre"]()
            t["PV"]()
            if HEAT and t["S"] is not noop_:
                hb, hbb = tp_ring.items[1]
                mm(hb[:, 0:HEAT], zeros_b[:, 0:128], zeros_b[:, 0:HEAT], True, True, [const_b], [hbb])
            if t["post"] is not None:
                t["post"]()
        del pipe[:]

    def attn_chunk(qc, qtile, q_b, ktile, k_b, rows, tiles, vget, vw, v_b, bias_get, finalize):
        obank, ob = oa_ring.next()
        o3 = obank[:, 0:4 * vw].rearrange("p (a b) -> p a b", b=vw)
        last = {}
        for i, (kt, c0, c1, mask) in enumerate(tiles):
            for qs in range(c0 // 128, c1 // 128):
                last[qs] = i
        ntl = len(tiles)
        for i, (kt, c0, c1, mask) in enumerate(tiles):
            sbank, sbb = sa_ring.next()
            pt, ptb = pt_ring.next()

            def fS(kt=kt, c0=c0, c1=c1, mask=mask, sbank=sbank, sbb=sbb):
                mm(sbank[:, c0:c1], ktile[rows, kt * 128:(kt + 1) * 128],
                   qtile[rows, qc * 512 + c0:qc * 512 + c1], True, mask is None, [q_b, k_b], [sbb])
                if mask is not None:
                    map_, m0, mw = mask
                    mm(sbank[:, m0:m0 + mw], ident_b[:], map_, False, True, [const_b], [sbb])

            def fE(kt=kt, c0=c0, c1=c1, sbank=sbank, sbb=sbb, pt=pt, ptb=ptb):
                kw = dict(scale=0.125)
                rd = [sbb]
                if bias_get is not None:
                    bap, bbuf = bias_get(kt)
                    kw["bias"] = bap
                    rd.append(bbuf)
                act(pt[:, c0:c1], sbank[:, c0:c1], AF.Exp, rd, [ptb], **kw)

            def fPV(i=i, kt=kt, c0=c0, c1=c1, pt=pt, ptb=ptb):
                for qs in range(c0 // 128, c1 // 128):
                    mm(o3[:, qs, :], pt[:, qs * 128:(qs + 1) * 128], vget(kt), False, last[qs] == i,
                       [ptb, v_b], [ob])

            pre = None
            post = None
            if i == 0:
                def pre():
                    mm(obank[:, :], zeros_b[:, 0:128], zeros_b[:, 0:512], True, False, [const_b], [ob])
            if i == ntl - 1:
                def post():
                    finalize(qc, o3, ob)
            pipe.append(dict(S=fS, E=fE, PV=fPV, pre=pre, post=post))

    def causal_tiles(qc):
        tl = [(kt, 0, 512, None) for kt in range(4 * qc)]
        for j in range(4):
            tl.append((4 * qc + j, 128 * j, 512, (tri_b[:, 0:128], 128 * j, 128)))
        return tl

    def window_tiles(qc):
        tl = []
        for j in range(-4, 4):
            kt = 4 * qc + j
            if kt < 0:
                continue
            if j >= 0:
                tl.append((kt, 128 * j, 512, (tri_b[:, 0:128], 128 * j, 128)))
            else:
                jj = j + 4
                tl.append((kt, 0, 128 * jj + 128, (tri_b[:, 128:256], 128 * jj, 128)))
        return tl

    for s in range(nseq):
        xs = x_d[s * T:(s + 1) * T, :]
        outs = out_d[s * T:(s + 1) * T, :]
        outD_b = [Buf("outD%d_%d" % (s, i)) for i in range(NT)]


        VF = aview(0, 8320).rearrange("p (a b c) -> p a b c", a=16, b=8)
        vf_b = Buf("VF")
        QK = [[aview(8320 + (st * 4 + i) * 2048, 2048) for i in range(4)] for st in range(2)]
        QK_b = [[Buf("QK%d_%d" % (st, i)) for i in range(4)] for st in range(2)]
        fox_tm = aview(8320 + 16384, 8192).rearrange("p (a b) -> p a b", b=512)
        fox_b = [Buf("fox%d" % i) for i in range(NT)]
        FOXT_OFF = 32896
        foxT = aview(FOXT_OFF, 8192).rearrange("p (a b) -> p a b", b=T)
        foxT_b = Buf("foxT")
        nsaT = aview(FOXT_OFF + 8192, 8192).rearrange("p (a b) -> p a b", b=T)
        nsaT_b = Buf("nsaT")

        wfvh = [load_w("pool", w_fv[hf, :, :], 2048) for hf in range(2)]
        memset("pool", VF[:, :, :, 64:65], 1.0, [vf_b])
        wg, wg_b = load_w("pool", w_ffng[:, :], 256)
        wg3 = wg[:, 0:256].rearrange("p (a b) -> p a b", b=32)
        wfv3 = [w[:, 0:2048].rearrange("p (a b) -> p a b", b=256) for (w, _) in wfvh]
        for ti in range(NT):
            c = ti // 4
            xt, xb = xt_ring.next()
            dma("sp", xt[:], xs[ti * 128:(ti + 1) * 128, :], (), [xb])
            norm_to_hT(xt[:], xb, ti, gpre1)
            for hf in range(2):
                bank, bb = pj_ring.next()
                for kc in range(8):
                    mm(bank[:, 0:256], hT[:, kc, ti * 128:(ti + 1) * 128], wfv3[hf][:, kc, :], kc == 0, kc == 7,
                       [hT_b[kc][c], wfvh[hf][1]], [bb])
                cp("dve", VF[:, ti, 4 * hf:4 * hf + 4, 0:64], bank[:, 0:256].rearrange("p (a b) -> p a b", b=64),
                   [bb], [vf_b])
            bank2, bb2 = tp_ring.next()
            for kc in range(8):
                mm(bank2[:, 0:32], hT[:, kc, ti * 128:(ti + 1) * 128], wg3[:, kc, :], kc == 0, kc == 7,
                   [hT_b[kc][c], wg_b], [bb2])
            sm, smb = sm_ring.next()
            tt("dve", sm[:, 0:8], bank2[:, 0:8], bfor[:], ALU.add, [bb2, const_b], [smb])
            act(sm[:, 8:16], sm[:, 0:8], AF.Exp, [smb], [smb], scale=-1.0)
            act(lp[:, ti, :], sm[:, 8:16], AF.Ln, [smb, const_b], [const_b], bias=onescol[:])
            tt("dve", lsum[:, ti + 1, :], lsum[:, ti, :], lp[:, ti, :], ALU.add, [const_b], [const_b])
            act(sm[:, 16:40], bank2[:, 8:32], AF.Exp, [bb2], [smb], scale=-1.0)
            ts("dve", sm[:, 16:40], sm[:, 16:40], 1.0, None, ALU.add, None, [smb], [smb])
            P.add("dve", "reciprocal", dict(out=gates[:, ti, :], in_=sm[:, 16:40]), [smb], [const_b])
        bank, bb = tp_ring.next()
        for ti in range(NT):
            mm(bank[:, ti * 8:(ti + 1) * 8], u_f[:], lp[:, ti, :], True, False, [const_b], [bb])
            mm(bank[:, ti * 8:(ti + 1) * 8], ones_f[:], lsum[:, ti, :], False, True, [const_b], [bb])
        cp("dve", negc[:].rearrange("p a b -> p (a b)"), bank[:, 0:128], [bb], [const_b])
        for c in range(4):
            bank, bb = tp_ring.next()
            for j in range(4):
                ti = 4 * c + j
                mm(bank[0:8, j * 128:(j + 1) * 128], lp[:, ti, :], u_f[:], True, False, [const_b], [bb])
                mm(bank[0:8, j * 128:(j + 1) * 128], lsum[:, ti, :], ones_f[:], False, True, [const_b], [bb])
            act(caugT[0:8, c * 512:(c + 1) * 512], bank[0:8, :], AF.Copy, [bb], [const_b], scale=-8.0)

        for p in range(4):
            st = p % 2
            QA, KA, QB, KB = QK[st]
            QA_b, KA_b, QB_b, KB_b = QK_b[st]
            dma("sp", QA[64:65, :], caugT[2 * p:2 * p + 1, :], [const_b], [QA_b])
            dma("sp", QB[64:65, :], caugT[2 * p + 1:2 * p + 2, :], [const_b], [QB_b])
            dma("sp", KA[64:65, :], caugT[32:33, :], [const_b], [KA_b])
            dma("sp", KB[64:65, :], caugT[32:33, :], [const_b], [KB_b])
            for (blk, TA, TA_b, TB, TB_b) in ((p, QA, QA_b, QB, QB_b), (4 + p, KA, KA_b, KB, KB_b)):
                wt, wb = load_w("pool", w_fqk[blk, :, :], 1024)
                w3 = wt[:, 0:1024].rearrange("p (a b) -> p a b", b=128)
                for c in range(4):
                    bank, bb = pj_ring.next()
                    for kc in range(8):
                        mm(bank[:, :], w3[:, kc, :], hT[:, kc, c * 512:(c + 1) * 512], kc == 0, kc == 7,
                           [wb, hT_b[kc][c]], [bb])
                    cp("dve", TA[0:64, c * 512:(c + 1) * 512], bank[0:64, :], [bb], [TA_b])
                    cp("act", TB[0:64, c * 512:(c + 1) * 512], bank[64:128, :], [bb], [TB_b])
            for hh in range(2):
                h = 2 * p + hh
                qtile, q_b, ktile, k_b = (QA, QA_b, KA, KA_b) if hh == 0 else (QB, QB_b, KB, KB_b)
                rows = slice(0, 65)

                def fin(qc, o3, ob, h=h):
                    sm, smb = sm_ring.next()
                    P.add("dve", "reciprocal", dict(out=sm[:, 0:4], in_=o3[:, :, 64]), [ob], [smb])
                    tt("dve", fox_tm[:, qc * 4:qc * 4 + 4, h * 64:(h + 1) * 64], o3[:, :, 0:64],
                       sm[:, 0:4].unsqueeze(2).broadcast_to([128, 4, 64]), ALU.mult, [ob, smb],
                       fox_b[qc * 4:qc * 4 + 4])

                for qc in range(4):
                    attn_chunk(qc, qtile, q_b, ktile, k_b, rows, causal_tiles(qc),
                               lambda kt, h=h: VF[:, kt, h, :], 65, vf_b,
                               lambda kt, h=h: (negc[:, kt, h:h + 1], const_b), fin)
            attn_flush()
        for ti in range(NT):
            bank, bb = tp_ring.next()
            bk = bank[:].bitcast(BF16)
            for kc in range(4):
                tr(bk[:, kc * 128:(kc + 1) * 128], fox_tm[:, ti, kc * 128:(kc + 1) * 128], ident_b[:],
                   [fox_b[ti], const_b], [bb])
            cp("dve", foxT[:, :, ti * 128:(ti + 1) * 128],
               bk[:, 0:512].rearrange("p (a b) -> p a b", b=128), [bb], [foxT_b])
        if dbg and s == 0:
            for ti in range(NT):
                yt, yb = xt_ring.next()
                cp("dve", yt[:, 0:512], fox_tm[:, ti, :], [fox_b[ti]], [yb])
                dma("sp", dbg_d["d_fox"][:, ti * 512:(ti + 1) * 512], yt[:, 0:512], [yb], [], final=True)
            dma("sp", dbg_d["d_negc"][:, :], negc[:].rearrange("p a b -> p (a b)"), [const_b], [], final=True)
        P.barrier()

        QS = [[aview((g * 4 + p) * 2048, 2048) for p in range(4)] for g in range(2)]
        QS_b = [[[Buf("QS%d_%d_%d" % (g, p, c)) for c in range(4)] for p in range(4)] for g in range(2)]
        KS = [aview(16384 + g * 2048, 2048) for g in range(2)]
        KS_b = [Buf("KS%d" % g) for g in range(2)]
        KW = [aview(16384 + 4096 + g * 2048, 2048) for g in range(2)]
        KW_b = [Buf("KW%d" % g) for g in range(2)]
        KC = aview(16384 + 8192, 2048)
        KC_b = Buf("KC")
        VC = aview(16384 + 10240, 2048)
        VC_b = Buf("VC")
        VSW = aview(28672, 4160).rearrange("p (a b c) -> p a b c", a=16, b=4)
        vsw_b = Buf("VSW")
        nsa_acc = aview(16384 + 8192, 4096, F32).rearrange("p (a b) -> p a b", b=512)
        nacc_b = [Buf("nacc%d" % i) for i in range(4)]

        memset("pool", VSW[:, :, :, 64:65], 1.0, [vsw_b])
        engs = ["pool", "dve", "pool", "dve", "pool"]
        for g in range(2):
            memset(engs[0], KW[g][64:96, :], 0.0, [KW_b[g]])
            for p in range(4):
                memset(engs[1 + p], QS[g][p][64:96, :], 0.0, QS_b[g][p])
        dma("pool", KS[0][64:96, :], erows_d[:, :], (), [KS_b[0]])
        dma("pool", KS[1][64:96, :], erows_d[:, :], (), [KS_b[1]])
        wv, wv_b = load_w("pool", w_nv[:, :], 2048)
        wv3 = wv[:, 0:2048].rearrange("p (a b) -> p a b", b=256)
        for ti in range(NT):
            c = ti // 4
            bank, bb = pj_ring.next()
            for kc in range(8):
                mm(bank[:, 0:256], hT[:, kc, ti * 128:(ti + 1) * 128], wv3[:, kc, :], kc == 0, kc == 7,
                   [hT_b[kc][c], wv_b], [bb])
            cp("dve", VSW[:, ti, :, 0:64], bank[:, 0:256].rearrange("p (a b) -> p a b", b=64), [bb], [vsw_b])

        def proj_fm(blk_plain, blk_swap, wsrc, dests):
            wp, wpb = load_w("pool", wsrc[blk_plain, :, :], 1024)
            wp3 = wp[:, 0:1024].rearrange("p (a b) -> p a b", b=128)
            if blk_swap is not None:
                wq, wqb = load_w("pool", wsrc[blk_swap, :, :], 1024)
                wq3 = wq[:, 0:1024].rearrange("p (a b) -> p a b", b=128)
            for c in range(4):
                b1, bb1 = pj_ring.next()
                for kc in range(8):
                    mm(b1[:, :], wp3[:, kc, :], hT[:, kc, c * 512:(c + 1) * 512], kc == 0, kc == 7,
                       [wpb, hT_b[kc][c]], [bb1])
                dests_c = [(t_, (b_[c] if isinstance(b_, list) else b_), r1_, r2_) for (t_, b_, r1_, r2_) in dests]
                if blk_swap is None:
                    for (tile_, tb_, rs, rd_) in dests_c:
                        cp("act", tile_[rd_, c * 512:(c + 1) * 512], b1[rs, :], [bb1], [tb_])
                    continue
                b2, bb2 = pj_ring.next()
                for kc in range(8):
                    mm(b2[:, :], wq3[:, kc, :], hT[:, kc, c * 512:(c + 1) * 512], kc == 0, kc == 7,
                       [wqb, hT_b[kc][c]], [bb2])
                rp, rpb = rope_ring.next()
                dma("sp", rp[:], rope_d[c, :, :], (), [rpb])
                ta, tab = t1_ring.next()
                tb, tbb = t1_ring.next()
                tt("dve", ta[:], b1[:, :], rp[:, 0:512], ALU.mult, [bb1, rpb], [tab])
                tt("dve", tb[:], b2[:, :], rp[:, 512:1024], ALU.mult, [bb2, rpb], [tbb])
                for (tile_, tb_, rs, rd_) in dests_c:
                    if rs == rd_:
                        tt("dve" if len(dests_c) > 1 else "pool", tile_[rd_, c * 512:(c + 1) * 512], ta[rs, :], tb[rs, :],
                           ALU.add, [tab, tbb], [tb_])
                    else:
                        tt("pool", ta[rs, :], ta[rs, :], tb[rs, :], ALU.add, [tab, tbb], [tab])
                        cp("act", tile_[rd_, c * 512:(c + 1) * 512], ta[rs, :], [tab], [tb_])

        for p in range(4):
            proj_fm(p, 4 + p, w_nq, [(QS[0][p], QS_b[0][p], slice(0, 64), slice(0, 64)),
                                     (QS[1][p], QS_b[1][p], slice(64, 128), slice(0, 64))])
        proj_fm(0, 1, w_nk, [(KC, KC_b, slice(0, 128), slice(0, 128))])
        proj_fm(2, 3, w_nk, [(KS[0], KS_b[0], slice(0, 64), slice(0, 64)), (KS[1], KS_b[1], slice(64, 128), slice(0, 64))])
        proj_fm(4, 5, w_nk, [(KW[0], KW_b[0], slice(0, 64), slice(0, 64)), (KW[1], KW_b[1], slice(64, 128), slice(0, 64))])
        proj_fm(6, None, w_nk, [(VC, VC_b, slice(0, 128), slice(0, 128))])

        for (src, src_b, w1, cb, sT) in ((KC, KC_b, w1k, cbk, sTk), (VC, VC_b, w1v, cbv, sTv)):
            for g in range(2):
                rs = slice(64 * g, 64 * g + 64)
                bank, bb = tp_ring.next()
                for l in range(32):
                    mm(bank[0:64, 0:127], w1[rs, l, :], src[rs, l:l + 2017:16], l == 0, l == 31,
                       [const_b, src_b], [bb])
                act(sT[:, g, 0:127], bank[0:64, 0:127], AF.Silu, [bb, const_b], [const_b], bias=cb[:])
        for g in range(2):
            bank, bb = tp_ring.next()
            mm(bank[0:64, 0:128], w2k[:], sTk[:, g, :], True, True, [const_b], [bb])
            cp("dve", kcmp[0:64, g, :], bank[0:64, 0:128], [bb], [const_b])
        for g in range(2):
            bank, bb = tp_ring.next()
            mm(bank[:, 0:64], sTv[:, g, :], w2v[:], True, True, [const_b], [bb])
            cp("dve", vcmp[:, g, 0:64], bank[:, 0:64], [bb], [const_b])

        P.barrier()
        psl_b = Buf("psl")
        mbw_b = Buf("mbw")
        mbT_b = Buf("mbT")
        ocmp = [xt_ring.items[k][0][:, :].rearrange("p (a b) -> p a b", b=512) for k in range(2)]
        ocmp_b = [xt_ring.items[k][1] for k in range(2)]

        def bc(ap2, n):
            return ap2.unsqueeze(2).broadcast_to([128, ap2.shape[1], n])

        noop = noop_

        def add_cmp_sel(qn):
            for g in range(2):
                for p in range(4):
                    h = 4 * g + p

                    def fin_cmp(qc, o3, ob, h=h, p=p):
                        sm, smb = sm_ring.next()
                        hs = slice(h * 64, (h + 1) * 64)
                        ts("dve", sm[:, 0:4], o3[:, :, 64], 1e-30, None, ALU.max, None, [ob], [smb])
                        P.add("dve", "reciprocal", dict(out=sm[:, 4:8], in_=sm[:, 0:4]), [smb], [smb])
                        tt("dve", sm[:, 8:12], sm[:, 4:8], gates[:, qc * 4:qc * 4 + 4, 3 * h], ALU.mult,
                           [smb, const_b], [smb])
                        for k in range(2):
                            tt("dve", ocmp[k][:, :, hs], o3[:, 2 * k:2 * k + 2, 0:64], bc(sm[:, 8 + 2 * k:10 + 2 * k], 64),
                               ALU.mult, [ob, smb], [ocmp_b[k]])
                        if p == 0:
                            tt("dve", psl[:, :, :], o3[:, :, 65:97], bc(sm[:, 4:8], 32), ALU.mult, [ob, smb], [psl_b])
                        else:
                            tmp, tmpb = t1_ring.next()
                            tmp3 = tmp[:, 0:128].rearrange("p (a b) -> p a b", b=32)
                            tt("dve", tmp3, o3[:, :, 65:97], bc(sm[:, 4:8], 32), ALU.mult, [ob, smb], [tmpb])
                            tt("dve", psl[:, :, :], psl[:, :, :], tmp3, ALU.add, [psl_b, tmpb], [psl_b])

                    attn_chunk(qn, QS[g][p], QS_b[g][p][qn], kcmp[:, g, :], const_b, slice(0, 96),
                               [(0, 0, 512, (cmpmask_b[:, qn * 512:(qn + 1) * 512], 0, 512))],
                               lambda kt, g=g: vcmp[:, g, :], 97, const_b, None, fin_cmp)

                def selection(g=g):
                    sc, scb = t1_ring.next()
                    sc3 = sc[:, 0:128].rearrange("p (a b) -> p a b", b=32)
                    sm, smb = sm_ring.next()
                    tt("dve", sc3, psl[:, :, :], fbt[:, qn * 4:qn * 4 + 4, :], ALU.add, [psl_b, const_b], [scb])
                    for qs in range(4):
                        P.add("dve", "max", dict(out=sm[:, 8 * qs:8 * qs + 8], in_=sc3[:, qs, :]), [scb], [smb])
                    tt("dve", sc3, sc3, bc(sm[:, 7:32:8], 32), ALU.is_lt, [scb, smb], [scb])
                    ts("dve", mbw[:, g, :, 64:96], sc3, NEGM, None, ALU.mult, None, [scb], [mbw_b])
                    bank, bb = tp_ring.next()
                    bk = bank[:].bitcast(BF16)
                    for qs in range(4):
                        tr(bk[0:96, qs * 128:(qs + 1) * 128], mbw[:, g, qs, :], ident_b[:], [mbw_b, const_b], [bb])
                    cp("dve", mbT[:, g, :], bk[0:96, 0:512], [bb], [mbT_b])
                    for p in range(4):
                        cp("pool", QS[g][p][64:96, qn * 512:(qn + 1) * 512], mbT[64:96, g, :], [mbT_b],
                           [QS_b[g][p][qn]])

                pipe.append(dict(S=noop, E=noop, PV=noop, pre=None, post=selection))

        def add_sel_win(qc, which):
            for g in range(2):
                for p in range(4):
                    h = 4 * g + p

                    def fin_acc(br, h=h):
                        def f(qc, o3, ob):
                            sm, smb = sm_ring.next()
                            hs = slice(h * 64, (h + 1) * 64)
                            P.add("dve", "reciprocal", dict(out=sm[:, 0:4], in_=o3[:, :, 64]), [ob], [smb])
                            tt("dve", sm[:, 4:8], sm[:, 0:4], gates[:, qc * 4:qc * 4 + 4, 3 * h + br], ALU.mult,
                               [smb, const_b], [smb])
                            tmp, tmpb = t1_ring.next()
                            tmp3 = tmp[:, 0:256].rearrange("p (a b) -> p a b", b=64)
                            tt("dve", tmp3, o3[:, :, 0:64], bc(sm[:, 4:8], 64), ALU.mult, [ob, smb], [tmpb])
                            if br == 1:
                                for k in range(2):
                                    tt("dve", nsa_acc[:, 2 * k:2 * k + 2, hs], tmp3[:, 2 * k:2 * k + 2, :],
                                       ocmp[k][:, :, hs], ALU.add, [tmpb, ocmp_b[k]], nacc_b[2 * k:2 * k + 2])
                            else:
                                tt("dve", nsa_acc[:, :, hs], nsa_acc[:, :, hs], tmp3, ALU.add, [tmpb] + nacc_b, nacc_b)
                        return f

                    if which == 1:
                        attn_chunk(qc, QS[g][p], QS_b[g][p][qc], KS[g], KS_b[g], slice(0, 96), causal_tiles(qc),
                                   lambda kt, g=g: VSW[:, kt, g, :], 65, vsw_b, None, fin_acc(1))
                    else:
                        attn_chunk(qc, QS[g][p], QS_b[g][p][qc], KW[g], KW_b[g], slice(0, 96), window_tiles(qc),
                                   lambda kt, g=g: VSW[:, kt, 2 + g, :], 65, vsw_b, None, fin_acc(2))

        add_cmp_sel(0)
        attn_flush()
        for qc in range(4):
            add_sel_win(qc, 1)
            if qc + 1 < 4:
                add_cmp_sel(qc + 1)
            add_sel_win(qc, 2)
            attn_flush()
            for qs in range(4):
                ti = qc * 4 + qs
                hn, hnb = hn_ring.next()
                cp("pool", hn[:, 0:512], nsa_acc[:, qs, :], [nacc_b[qs]], [hnb])
                if dbg and s == 0:
                    dma("sp", dbg_d["d_nsa"][:, ti * 512:(ti + 1) * 512], nsa_acc[:, qs, :], [nacc_b[qs]], [],
                        final=True)
                bank, bb = tp_ring.next()
                bk = bank[:].bitcast(BF16)
                for kc in range(4):
                    tr(bk[:, kc * 128:(kc + 1) * 128], hn[:, kc * 128:(kc + 1) * 128], ident_b[:],
                       [hnb, const_b], [bb])
                cp("dve", nsaT[:, :, ti * 128:(ti + 1) * 128],
                   bk[:, 0:512].rearrange("p (a b) -> p a b", b=128), [bb], [nsaT_b])
        P.barrier()

        mixT = aview(0, 16384).rearrange("p (a b) -> p a b", b=T)
        mixT_b = [[Buf("mix%d_%d" % (c, t4)) for t4 in range(4)] for c in range(8)]
        for cb_ in range(8):
            wga, wga_b = load_w("pool", w_gab[cb_, :, :], 1024)
            wgb, wgb_b = load_w("pool", w_gab[8 + cb_, :, :], 1024)
            wfo, wfo_b = load_w("pool", w_fo[cb_, :, :], 512)
            wno, wno_b = load_w("pool", w_no[cb_, :, :], 512)
            wga3 = wga[:, 0:1024].rearrange("p (a b) -> p a b", b=128)
            wgb3 = wgb[:, 0:1024].rearrange("p (a b) -> p a b", b=128)
            wfo3 = wfo[:, 0:512].rearrange("p (a b) -> p a b", b=128)
            wno3 = wno[:, 0:512].rearrange("p (a b) -> p a b", b=128)
            for c in range(4):
                cs = slice(c * 512, (c + 1) * 512)
                res = []
                for (wg3_, wgb_, wo3_, wob_, srcT, srcb) in ((wga3, wga_b, wfo3, wfo_b, foxT, foxT_b),
                                                           (wgb3, wgb_b, wno3, wno_b, nsaT, nsaT_b)):
                    b1, bb1 = pj_ring.next()
                    for kc in range(8):
                        mm(b1[:, :], wg3_[:, kc, :], hT[:, kc, cs], kc == 0, kc == 7, [wgb_, hT_b[kc][c]], [bb1])
                    sg, sgb = t1_ring.next()
                    act(sg[:], b1[:, :], AF.Sigmoid, [bb1], [sgb])
                    b2, bb2 = pj_ring.next()
                    for kc in range(4):
                        mm(b2[:, :], wo3_[:, kc, :], srcT[:, kc, cs], kc == 0, kc == 3, [wob_, srcb], [bb2])
                    tt("dve", sg[:], sg[:], b2[:, :], ALU.mult, [sgb, bb2], [sgb])
                    res.append((sg, sgb))
                tt("pool", mixT[:, cb_, cs], res[0][0][:], res[1][0][:], ALU.add, [res[0][1], res[1][1]],
                   [mixT_b[cb_][c]])
        P.barrier()

        dma("sp", gpost1[:], gpost1_d.partition_broadcast(128), (), [gpost_b])
        WO = aview(16384, 8192).rearrange("p (h a b) -> p h a b", h=2, a=8)
        wo_b = Buf("WO")
        for hf in range(2):
            dma("pool", WO[:, hf, :, :].rearrange("p a b -> p (a b)"), w_o[hf, :, :], (), [wo_b])
        for ti in range(NT):
            c = ti // 4
            yt, yb = yt_ring.next()
            for hf in range(2):
                bank, bb = pj_ring.next()
                for kc in range(8):
                    mm(bank[:, :], mixT[:, kc, ti * 128:(ti + 1) * 128], WO[:, hf, kc, :], kc == 0, kc == 7,
                       [mixT_b[kc][c], wo_b], [bb])
                cp("act" if hf == 0 else "dve", yt[:, hf * 512:(hf + 1) * 512], bank[:, :], [bb], [yb])
            sm, smb = sm_ring.next()
            act(junk[:], yt[:], AF.Square, [yb], [junk_b, smb], accum_out=sm[:, 0:1])
            ts("dve", sm[:, 1:2], sm[:, 0:1], 1.0 / D, 1e-6, ALU.mult, ALU.add, [smb], [smb])
            act(sm[:, 2:3], sm[:, 1:2], AF.Ln, [smb], [smb])
            act(sm[:, 3:4], sm[:, 2:3], AF.Exp, [smb], [smb], scale=-0.5)
            stt(yt[:], yt[:], sm[:, 3:4], gpost1[:], ALU.mult, ALU.mult, [yb, smb, gpost_b], [yb])
            xt, xb = xt_ring.next()
            dma("sp", xt[:], xs[ti * 128:(ti + 1) * 128, :], (), [xb])
            tt("pool", xt[:], xt[:], yt[:], ALU.add, [xb, yb], [xb])
            dma("sp", outs[ti * 128:(ti + 1) * 128, :], xt[:], [xb], [outD_b[ti]])
            norm_to_hT(xt[:], xb, ti, gpre2)
        P.barrier()

        dma("sp", gpost2[:], gpost2_d.partition_broadcast(128), (), [gpost_b])
        yacc = aview(0, 32768, F32).rearrange("p (a b) -> p a b", b=1024)
        yacc_b = [[Buf("yacc%d_%d" % (i, hf)) for hf in range(2)] for i in range(NT)]
        uT = [aview(32768 + i * 4096, 4096).rearrange("p (a b) -> p a b", b=T) for i in range(2)]
        uT_b = [[[Buf("uT%d_%d_%d" % (i, f, c)) for c in range(4)] for f in range(2)] for i in range(2)]
        def mlp_up(fg):
            wu, wu_b = load_w("pool", w_up[fg, :, :], 2048)
            wu3 = wu[:, 0:2048].rearrange("p (a b) -> p a b", b=256)
            ub = fg % 2
            for fc in range(2):
                for c in range(4):
                    cs = slice(c * 512, (c + 1) * 512)
                    bank, bb = pj_ring.next()
                    for kc in range(8):
                        mm(bank[:, :], wu3[:, kc, fc * 128:(fc + 1) * 128], hT[:, kc, cs], kc == 0, kc == 7,
                           [wu_b, hT_b[kc][c]], [bb])
                    r, rb = t1_ring.next()
                    act(r[:], bank[:, :], AF.Relu, [bb], [rb])
                    act(uT[ub][:, fc, cs], r[:], AF.Square, [rb], [uT_b[ub][fc][c]])

        mlp_up(0)
        for fg in range(16):
            wd, wd_b = load_w("pool", w_dn[fg, :, :], 2048)
            wd3 = wd[:, 0:2048].rearrange("p (a b) -> p a b", b=1024)
            ub = fg % 2
            if fg + 1 < 16:
                mlp_up(fg + 1)
            for ti in range(NT):
                c = ti // 4
                for hf in range(2):
                    bank, bb = y_ring.next()
                    for fc in range(2):
                        mm(bank[:, :], uT[ub][:, fc, ti * 128:(ti + 1) * 128], wd3[:, fc, hf * 512:(hf + 1) * 512],
                           fc == 0, fc == 1, [uT_b[ub][fc][c], wd_b], [bb])
                    ya = yacc[:, ti, hf * 512:(hf + 1) * 512]
                    if fg == 0:
                        cp("act", ya, bank[:, :], [bb], [yacc_b[ti][hf]])
                    else:
                        tt("dve", ya, bank[:, :], ya, ALU.add, [bb, yacc_b[ti][hf]], [yacc_b[ti][hf]])
        for ti in range(NT):
            sm, smb = sm_ring.next()
            act(junk[:], yacc[:, ti, :], AF.Square, yacc_b[ti], [junk_b, smb], accum_out=sm[:, 0:1])
            ts("dve", sm[:, 1:2], sm[:, 0:1], 1.0 / D, 1e-6, ALU.mult, ALU.add, [smb], [smb])
            act(sm[:, 2:3], sm[:, 1:2], AF.Ln, [smb], [smb])
            act(sm[:, 3:4], sm[:, 2:3], AF.Exp, [smb], [smb], scale=-0.5)
            yt, yb = yt_ring.next()
            stt(yt[:], yacc[:, ti, :], sm[:, 3:4], gpost2[:], ALU.mult, ALU.mult, yacc_b[ti] + [smb, gpost_b], [yb])
            xt, xb = xt_ring.next()
            dma("sp", xt[:], outs[ti * 128:(ti + 1) * 128, :], [outD_b[ti]], [xb])
            tt("pool", xt[:], xt[:], yt[:], ALU.add, [xb, yb], [xb])
            dma("sp", outs[ti * 128:(ti + 1) * 128, :], xt[:], [xb], [outD_b[ti]], final=True)
        P.barrier()

    P.finalize()
    P.emit(nc, es)
    es.close()
    return nc


def _blk(w, cols):
    sub = w[:, cols]
    n = sub.shape[1]
    return np.ascontiguousarray(sub.reshape(8, 128, n).transpose(1, 0, 2).reshape(128, 8 * n))


def _rowblk(w, nkc):
    n = w.shape[1]
    return np.ascontiguousarray(w.reshape(nkc, 128, n).transpose(1, 0, 2).reshape(128, nkc * n))


def _const_tables():
    f32 = np.float32
    inv = np.power(f32(10000.0), -np.arange(0, 64, 2, dtype=f32) / f32(64)).astype(f32)
    ang = (np.arange(T, dtype=f32)[:, None] * inv[None, :]).astype(f32)
    cos = np.cos(ang).astype(f32).T
    sin = np.sin(ang).astype(f32).T
    rope = np.zeros((4, 128, 1024), f32)
    for r in range(128):
        j = r % 32
        sgn = -1.0 if (r % 64) < 32 else 1.0
        for c in range(4):
            rope[c, r, 0:512] = cos[j, c * 512:(c + 1) * 512]
            rope[c, r, 512:1024] = sgn * sin[j, c * 512:(c + 1) * 512]
    n = np.arange(128)[:, None]
    t = np.arange(T)[None, :]
    cmpmask = np.where((16 * n + 31 <= t) & (n < 127), 0.0, NEGM).astype(f32)
    tt_ = np.arange(T)
    cur = tt_ // 64
    jj = np.arange(32)[None, :]
    forced = (jj == 0) | (jj == cur[:, None]) | (jj == cur[:, None] - 1)
    valid = jj <= cur[:, None]
    fb = np.where(valid, 1e4 * forced.astype(f32), -1e30).astype(f32)
    fbt = np.ascontiguousarray(fb.reshape(16, 128, 32).transpose(1, 0, 2).reshape(128, 512))
    sI = np.arange(128)[:, None]
    tI = np.arange(128)[None, :]
    tri = np.concatenate([np.where(sI > tI, NEGM, 0.0), np.where(tI >= sI, NEGM, 0.0)], axis=1).astype(f32)
    ident = np.eye(128, dtype=f32)
    umat = (sI <= tI).astype(f32)
    ci = np.arange(128)[:, None] * 16
    sj = np.arange(32)[None, :] * 64
    ov = (((ci < sj + 64) & (ci + 32 > sj)) & (np.arange(128)[:, None] < 127)).astype(f32)
    erows = (np.arange(32)[:, None] == (np.arange(T)[None, :] // 64)).astype(f32)
    return dict(rope=rope, cmpmask=cmpmask, fbt=fbt, tri=tri, ident=ident, umat=umat, ov=ov, erows=erows)


def _layout_weights(inp):
    f = lambda a: np.asarray(a, dtype=np.float32)
    w_in = f(inp["w_in"])[0]
    o = {}
    ar = np.arange
    FQ, FK, FV, FF, NQ = 0, 512, 1024, 1536, 1544
    KCo, VCo, KSo, VSo, KWo, VWo, NG, GA, GB = 2056, 2184, 2312, 2440, 2568, 2696, 2824, 2848, 3872
    o["w_fqk"] = np.stack([_blk(w_in, FQ + 128 * p + ar(128)) for p in range(4)] +
                          [_blk(w_in, FK + 128 * p + ar(128)) for p in range(4)])
    o["w_fv"] = np.stack([_blk(w_in, FV + 256 * hf + ar(256)) for hf in range(2)])
    o["w_ffng"] = _blk(w_in, np.concatenate([FF + ar(8), NG + ar(24)]))
    sw = (ar(64) + 32) % 64
    nq_plain, nq_swap = [], []
    for p in range(4):
        nq_plain.append(_blk(w_in, np.concatenate([NQ + 64 * p + ar(64), NQ + 64 * (4 + p) + ar(64)])))
        nq_swap.append(_blk(w_in, np.concatenate([NQ + 64 * p + sw, NQ + 64 * (4 + p) + sw])))
    o["w_nq"] = np.stack(nq_plain + nq_swap)
    sw2 = np.concatenate([sw, 64 + sw])
    o["w_nk"] = np.stack([_blk(w_in, KCo + ar(128)), _blk(w_in, KCo + sw2),
                          _blk(w_in, KSo + ar(128)), _blk(w_in, KSo + sw2),
                          _blk(w_in, KWo + ar(128)), _blk(w_in, KWo + sw2),
                          _blk(w_in, VCo + ar(128))])
    o["w_nv"] = _blk(w_in, np.concatenate([VSo + ar(128), VWo + ar(128)]))
    o["w_gab"] = np.stack([_blk(w_in, GA + 128 * c + ar(128)) for c in range(8)] +
                          [_blk(w_in, GB + 128 * c + ar(128)) for c in range(8)])
    wfo = f(inp["w_fox_out"])[0]
    wno = f(inp["w_nsa_out"])[0]
    o["w_fo"] = np.stack([_rowblk(wfo[:, 128 * c:128 * (c + 1)], 4) for c in range(8)])
    o["w_no"] = np.stack([_rowblk(wno[:, 128 * c:128 * (c + 1)], 4) for c in range(8)])
    wo = f(inp["w_o"])[0]
    o["w_o"] = np.stack([_rowblk(wo[:, 512 * h:512 * (h + 1)], 8) for h in range(2)])
    wup = f(inp["w_up"])[0]
    wdn = f(inp["w_down"])[0]
    o["w_up"] = np.stack([_blk(wup, 256 * g + ar(256)) for g in range(16)])
    o["w_dn"] = np.stack([_rowblk(wdn[256 * g:256 * (g + 1), :], 2) for g in range(16)])
    for nm, key in (("w1k", "cmp_w1_k"), ("w1v", "cmp_w1_v")):
        w1 = f(inp[key])[0].reshape(32, 64, 64).transpose(1, 0, 2).reshape(64, 2048)
        o[nm] = np.ascontiguousarray(np.concatenate([w1, w1], axis=0))
    o["w2k"] = np.ascontiguousarray(f(inp["cmp_w2_k"])[0])
    o["w2v"] = np.ascontiguousarray(f(inp["cmp_w2_v"])[0])
    o["posk"] = np.ascontiguousarray(f(inp["cmp_pos_k"])[0].T)
    o["posv"] = np.ascontiguousarray(f(inp["cmp_pos_v"])[0].T)
    o["gpre1"] = np.ascontiguousarray(f(inp["norm_mix_pre"])[0].reshape(8, 128).T)
    o["gpre2"] = np.ascontiguousarray(f(inp["norm_mlp_pre"])[0].reshape(8, 128).T)
    o["gpost1"] = np.ascontiguousarray(f(inp["norm_mix_post"])[0].reshape(1, 1024))
    o["gpost2"] = np.ascontiguousarray(f(inp["norm_mlp_post"])[0].reshape(1, 1024))
    o["bfor"] = np.ascontiguousarray(f(inp["b_forget"])[0].reshape(1, 8))
    o.update(_const_tables())
    return o


_NC_CACHE = {}


def kernel(**inputs):
    x = np.asarray(inputs["x"], dtype=np.float32)
    B = x.shape[0]
    per = B // NCORES
    shared = _layout_weights(inputs)
    if per not in _NC_CACHE:
        _NC_CACHE[per] = build_program(per)
    nc = _NC_CACHE[per]
    in_maps = []
    for c in range(NCORES):
        m = dict(shared)
        m["x"] = np.ascontiguousarray(x[c * per:(c + 1) * per].reshape(per * T, D))
        in_maps.append(m)
    res = run_bass_kernel_spmd(nc, in_maps, core_ids=list(range(NCORES)))
    outs = [np.asarray(r["out"]).reshape(per, T, D) for r in res.results]
    return np.concatenate(outs, axis=0).astype(np.float32)
```

```python
import numpy as np
from contextlib import ExitStack
import concourse.bass as bass
import concourse.mybir as mybir
from concourse.bass_utils import run_bass_kernel_spmd

F32 = mybir.dt.float32
BF16 = mybir.dt.bfloat16
AF = mybir.ActivationFunctionType
ALU = mybir.AluOpType

T = 2048
D = 1024
NT = 16
NEGM = -30000.0
SEQ_PER_CORE = 4
NCORES = 8


class Buf:
    __slots__ = ("name", "last_w", "readers", "dma_readers")

    def __init__(self, name):
        self.name = name
        self.last_w = None
        self.readers = {}
        self.dma_readers = []


class Op:
    __slots__ = ("eng", "meth", "kw", "deps", "idx", "dma", "tok", "marked", "semval")


COMPUTE = ("pe", "act", "dve", "pool")
QUEUES = ("sp", "pool")
NDSEM = 12


class Prog:
    def __init__(self):
        self.streams = {e: [] for e in ("pe", "act", "dve", "pool", "sp")}
        self.ndma = {q: 0 for q in QUEUES}
        self.dma_ops = {q: [] for q in QUEUES}
        self.barrier_deps = {e: [] for e in self.streams}
        self.dmas_since_barrier = []
        self.final_deps = []

    def add(self, eng, meth, kw, reads=(), writes=(), dma=False, prefetch=False, final=False):
        op = Op()
        op.eng, op.meth, op.kw, op.dma = eng, meth, kw, dma
        op.marked = False
        op.semval = None
        stream = self.streams[eng]
        op.idx = len(stream)
        deps = set()
        for b in reads:
            if b.last_w is not None:
                deps.add(b.last_w)
        for b in writes:
            if b.last_w is not None:
                deps.add(b.last_w)
            for e, i in b.readers.items():
                deps.add(("c", e, i))
            for t in b.dma_readers:
                deps.add(t)
        for t in self.barrier_deps[eng]:
            deps.add(t)
        self.barrier_deps[eng] = []
        if dma:
            j = self.ndma[eng]
            self.ndma[eng] += 1
            op.tok = ("d", eng, j)
            if j >= NDSEM:
                deps.add(("d", eng, j - NDSEM))
            self.dma_ops[eng].append(op)
            if not prefetch:
                self.dmas_since_barrier.append(op.tok)
            if final:
                self.final_deps.append(op.tok)
        else:
            op.tok = ("c", eng, op.idx)
        fdeps = []
        for d in deps:
            if d[0] == "c" and d[1] == eng and not dma:
                if eng == "pe":
                    continue
                fdeps.append(d)
            else:
                fdeps.append(d)
        op.deps = fdeps
        for b in reads:
            if dma:
                b.dma_readers.append(op.tok)
            else:
                b.readers[eng] = op.idx
        for b in writes:
            b.last_w = op.tok
            b.readers = {}
            b.dma_readers = []
        stream.append(op)
        return op

    def barrier(self):
        toks = []
        for e in COMPUTE:
            if self.streams[e]:
                last = self.streams[e][-1]
                if not last.dma:
                    toks.append(last.tok)
                else:
                    for o in reversed(self.streams[e]):
                        if not o.dma:
                            toks.append(o.tok)
                            break
        toks += self.dmas_since_barrier
        self.dmas_since_barrier = []
        for e in self.streams:
            self.barrier_deps[e] = list(self.barrier_deps[e]) + toks

    def finalize(self):
        op = self.add("sp", "nop_final", {}, (), ())
        op.deps = list(self.final_deps)
        for e, stream in self.streams.items():
            for op in stream:
                for d in op.deps:
                    if d[0] == "c":
                        self.streams[d[1]][d[2]].marked = True
        for e in COMPUTE:
            n = 0
            for op in self.streams[e]:
                if op.dma:
                    continue
                if op.marked:
                    n += 1
                    op.semval = n

    def emit(self, nc, es):
        esem = {e: es.enter_context(nc.semaphore("s_" + e)) for e in COMPUTE}
        dsem = {q: [es.enter_context(nc.semaphore("d_%s%d" % (q, i))) for i in range(NDSEM)]
                for q in QUEUES}
        block = es.enter_context(nc.Block())
        streams = self.streams

        def tokwait(d):
            if d[0] == "c":
                return ("c" + d[1], esem[d[1]], streams[d[1]][d[2]].semval)
            q, j = d[1], d[2]
            return ("d%s%d" % (q, j % NDSEM), dsem[q][j % NDSEM], 16 * (j // NDSEM + 1))

        def run(eng, name):
            waited = {}
            for op in streams[name]:
                for d in op.deps:
                    key, sem, val = tokwait(d)
                    if waited.get(key, 0) >= val:
                        continue
                    waited[key] = val
                    eng.wait_ge(sem, val)
                if op.meth == "nop_final":
                    continue
                ins = getattr(eng, op.meth)(**op.kw)
                if op.dma:
                    j = op.tok[2]
                    ins.then_inc(dsem[name][j % NDSEM], 16)
                elif op.marked:
                    ins.then_inc(esem[name], 1)

        @block.sync
        def _(e):
            run(e, "sp")

        @block.gpsimd
        def _(e):
            run(e, "pool")

        @block.scalar
        def _(e):
            run(e, "act")

        @block.vector
        def _(e):
            run(e, "dve")

        @block.tensor
        def _(e):
            run(e, "pe")


class Ring:
    def __init__(self, items):
        self.items = items
        self.i = 0

    def next(self):
        it = self.items[self.i % len(self.items)]
        self.i += 1
        return it


def build_program(nseq=SEQ_PER_CORE, dbg=False):
    nc = bass.Bass("TRN2", target_bir_lowering=False)
    P = Prog()
    es = ExitStack()

    def din(name, shape, dt=F32):
        return nc.dram_tensor(name, list(shape), dt, kind="ExternalInput").ap()

    x_d = din("x", [nseq * T, D])
    out_d = nc.dram_tensor("out", [nseq * T, D], F32, kind="ExternalOutput").ap()
    w_fqk = din("w_fqk", [8, 128, 1024])
    w_fv = din("w_fv", [2, 128, 8 * 256])
    w_ffng = din("w_ffng", [128, 8 * 32])
    w_nq = din("w_nq", [8, 128, 1024])
    w_nk = din("w_nk", [7, 128, 1024])
    w_nv = din("w_nv", [128, 8 * 256])
    w_gab = din("w_gab", [16, 128, 1024])
    w_fo = din("w_fo", [8, 128, 512])
    w_no = din("w_no", [8, 128, 512])
    w_o = din("w_o", [2, 128, 8 * 512])
    w_up = din("w_up", [16, 128, 8 * 256])
    w_dn = din("w_dn", [16, 128, 2 * 1024])
    w1k_d = din("w1k", [128, 2048])
    w1v_d = din("w1v", [128, 2048])
    w2k_d = din("w2k", [64, 64])
    w2v_d = din("w2v", [64, 64])
    posk_d = din("posk", [64, 32])
    posv_d = din("posv", [64, 32])
    rope_d = din("rope", [4, 128, 1024])
    cmpmask_d = din("cmpmask", [128, 2048])
    fbt_d = din("fbt", [128, 16 * 32])
    tri_d = din("tri", [128, 256])
    ident_d = din("ident", [128, 128])
    u_d = din("umat", [128, 128])
    ov_d = din("ov", [128, 32])
    erows_d = din("erows", [32, 2048])
    gpre1_d = din("gpre1", [128, 8])
    gpre2_d = din("gpre2", [128, 8])
    gpost1_d = din("gpost1", [1, 1024])
    gpost2_d = din("gpost2", [1, 1024])
    bfor_d = din("bfor", [1, 8])
    dbg_d = {}
    if dbg:
        for nm, shp in (("d_fox", [128, 16 * 512]), ("d_nsa", [128, 16 * 512]),
                        ("d_negc", [128, 128])):
            dbg_d[nm] = nc.dram_tensor(nm, shp, F32, kind="ExternalOutput").ap()

    def sb(name, shape, dt):
        return es.enter_context(nc.sbuf_tensor(name, list(shape), dt))

    def ps(name, shape, dt):
        return es.enter_context(nc.psum_tensor(name, list(shape), dt))

    hT = sb("hT", [128, 8, T], BF16)
    hT_b = [[Buf("hT%d_%d" % (k, c)) for c in range(4)] for k in range(8)]
    ARENA_ELEMS = 50176
    arena = sb("arena", [128, ARENA_ELEMS], BF16)
    ident_f = sb("ident_f", [128, 128], F32)
    ident_b = sb("ident_b", [128, 128], BF16)
    u_f = sb("u_f", [128, 128], F32)
    ones_f = sb("ones_f", [128, 128], F32)
    zeros_b = sb("zeros_b", [128, 512], BF16)
    tri_b = sb("tri_b", [128, 256], BF16)
    cmpmask_b = sb("cmpmask_b", [128, T], BF16)
    fbt = sb("fbt_s", [128, 16, 32], F32)
    gpre1 = sb("gpre1_s", [128, 8], F32)
    gpre2 = sb("gpre2_s", [128, 8], F32)
    bfor = sb("bfor_s", [128, 8], F32)
    onescol = sb("onescol", [128, 1], F32)
    w1k = sb("w1k_s", [128, 32, 64], BF16)
    w1v = sb("w1v_s", [128, 32, 64], BF16)
    w2k = sb("w2k_s", [64, 64], BF16)
    w2v = sb("w2v_s", [64, 64], BF16)
    posk = sb("posk_s", [64, 32], BF16)
    posv = sb("posv_s", [64, 32], BF16)
    cbk = sb("cbk", [64, 1], F32)
    cbv = sb("cbv", [64, 1], F32)
    vcmp = sb("vcmp", [128, 2, 97], BF16)
    kcmp = sb("kcmp", [96, 2, 128], BF16)
    sTk = sb("sTk", [64, 2, 128], BF16)
    sTv = sb("sTv", [64, 2, 128], BF16)
    mbw = sb("mbw", [128, 2, 4, 96], BF16)
    mbT = sb("mbT", [96, 2, 512], BF16)
    const_b = Buf("consts")

    lp = sb("lp", [128, 16, 8], F32)
    lsum = sb("lsum", [128, 17, 8], F32)
    negc = sb("negc", [128, 16, 8], F32)
    caugT = sb("caugT", [33, T], BF16)
    gates = sb("gates", [128, 16, 24], F32)
    psl = sb("psl", [128, 4, 32], F32)

    xt_ring = Ring([(sb("xt%d" % i, [128, 1024], F32), Buf("xt%d" % i)) for i in range(2)])
    t1_ring = Ring([(sb("t1_%d" % i, [128, 512], F32), Buf("t1_%d" % i)) for i in range(3)])
    hn_ring = Ring([(sb("hn%d" % i, [128, 1024], BF16), Buf("hn%d" % i)) for i in range(2)])
    junk_b = Buf("junk")
    pt_ring = Ring([(sb("pt%d" % i, [128, 512], BF16), Buf("pt%d" % i)) for i in range(4)])
    wt_ring = Ring([(sb("wt%d" % i, [128, 2048], BF16), Buf("wt%d" % i)) for i in range(4)])
    rope_ring = Ring([(sb("rp%d" % i, [128, 1024], F32), Buf("rp%d" % i)) for i in range(1)])
    sm_ring = Ring([(sb("sm%d" % i, [128, 64], F32), Buf("sm%d" % i)) for i in range(8)])

    pj_ring = Ring([(ps("pj%d" % i, [128, 512], F32), Buf("pj%d" % i)) for i in range(2)])
    st_ring = Ring([(ps("st%d" % i, [128, 512], F32), Buf("st%d" % i)) for i in range(2)])
    sa_ring = Ring([st_ring.items[0], pj_ring.items[0], st_ring.items[1], pj_ring.items[1]])
    pj4_ring = Ring([pj_ring.items[0], st_ring.items[0], pj_ring.items[1], st_ring.items[1]])
    oa_ring = Ring([(ps("oa%d" % i, [128, 512], F32), Buf("oa%d" % i)) for i in range(2)])
    tp_ring = Ring([(ps("tp%d" % i, [128, 512], F32), Buf("tp%d" % i)) for i in range(2)])
    y_ring = Ring([st_ring.items[0], oa_ring.items[0], st_ring.items[1], oa_ring.items[1]])

    def mm(out, lhsT, rhs, start, stop, reads, writes):
        P.add("pe", "matmul", dict(out=out, lhsT=lhsT, rhs=rhs, start=start, stop=stop,
                                   skip_group_check=True), reads, writes)

    def tr(out, in_, ident, reads, writes):
        P.add("pe", "transpose", dict(out=out, in_=in_, identity=ident), reads, writes)

    def act(out, in_, func, reads, writes, **kw):
        P.add("act", "activation", dict(out=out, in_=in_, func=func, **kw), reads, writes)

    def dma(q, out, in_, reads, writes, **kw):
        extra = {}
        for k in ("prefetch", "final"):
            if k in kw:
                extra[k] = kw.pop(k)
        return P.add(q, "dma_start", dict(out=out, in_=in_, **kw), reads, writes, dma=True, **extra)

    def tt(eng, out, in0, in1, op, reads, writes):
        P.add(eng, "tensor_tensor", dict(out=out, in0=in0, in1=in1, op=op), reads, writes)

    def ts(eng, out, in0, s1, s2, op0, op1, reads, writes):
        kw = dict(out=out, in0=in0, scalar1=s1, scalar2=s2, op0=op0)
        if op1 is not None:
            kw["op1"] = op1
        P.add(eng, "tensor_scalar", kw, reads, writes)

    def stt(out, in0, scalar, in1, op0, op1, reads, writes):
        P.add("dve", "scalar_tensor_tensor", dict(out=out, in0=in0, scalar=scalar, in1=in1,
                                                  op0=op0, op1=op1), reads, writes)

    def cp(eng, out, in_, reads, writes):
        if eng == "act":
            P.add("act", "activation", dict(out=out, in_=in_, func=AF.Copy), reads, writes)
        else:
            P.add(eng, "tensor_copy", dict(out=out, in_=in_), reads, writes)

    def memset(eng, ap, val, writes):
        P.add(eng, "memset", dict(ap=ap, constant=val), (), writes)

    dma("sp", ident_f[:], ident_d[:, :], (), [const_b])
    dma("pool", ident_b[:], ident_d[:, :], (), [const_b])
    dma("sp", u_f[:], u_d[:, :], (), [const_b])
    dma("pool", tri_b[:], tri_d[:, :], (), [const_b])
    dma("pool", cmpmask_b[:], cmpmask_d[:, :], (), [const_b])
    dma("sp", fbt[:].rearrange("p a b -> p (a b)"), fbt_d[:, :], (), [const_b])
    dma("sp", gpre1[:], gpre1_d[:, :], (), [const_b])
    dma("sp", gpre2[:], gpre2_d[:, :], (), [const_b])
    dma("sp", bfor[:], bfor_d.partition_broadcast(128), (), [const_b])
    dma("pool", w1k[:].rearrange("p a b -> p (a b)"), w1k_d[:, :], (), [const_b])
    dma("pool", w1v[:].rearrange("p a b -> p (a b)"), w1v_d[:, :], (), [const_b])
    dma("pool", w2k[:], w2k_d[:, :], (), [const_b])
    dma("pool", w2v[:], w2v_d[:, :], (), [const_b])
    dma("pool", posk[:], posk_d[:, :], (), [const_b])
    dma("pool", posv[:], posv_d[:, :], (), [const_b])
    memset("pool", ones_f[:], 1.0, [const_b])
    memset("pool", zeros_b[:], 0.0, [const_b])
    memset("pool", onescol[:], 1.0, [const_b])
    memset("pool", caugT[32:33, :], 1.0, [const_b])
    memset("pool", vcmp[:].rearrange("p a b -> p (a b)"), 1.0, [const_b])
    for g in range(2):
        dma("pool", vcmp[:, g, 65:97], ov_d[:, :], (), [const_b])
    memset("pool", sTk[:].rearrange("p a b -> p (a b)"), 0.0, [const_b])
    memset("pool", sTv[:].rearrange("p a b -> p (a b)"), 0.0, [const_b])
    memset("pool", mbw[:].rearrange("p g a b -> p (g a b)"), 0.0, [const_b])
    memset("pool", lsum[:, 0, :], 0.0, [const_b])
    memset("pool", kcmp[64:96, :, :].rearrange("p a b -> p (a b)"), 0.0, [const_b])
    for (w1, pos, cb) in ((w1k, posk, cbk), (w1v, posv, cbv)):
        bank, bb = tp_ring.next()
        for l in range(32):
            mm(bank[0:64, 0:1], w1[0:64, l, :], pos[0:64, l:l + 1], l == 0, l == 31, [const_b], [bb])
        cp("dve", cb[:], bank[0:64, 0:1], [bb], [const_b])

    def aview(off, n, dt=BF16):
        v = arena[:, off:off + n]
        if dt == F32:
            v = v.bitcast(F32)
        return v

    junk = aview(49152, 1024)
    yt_ring = Ring([(aview(40960 + i * 2048, 2048, F32), Buf("yt%d" % i)) for i in range(2)])
    gpost1 = aview(45056, 2048, F32)
    gpost2 = aview(47104, 2048, F32)
    gpost_b = Buf("gpost")

    def norm_to_hT(src, src_b, tt_i, gpre):
        sm, smb = sm_ring.next()
        act(junk[:], src, AF.Square, [src_b], [junk_b, smb], accum_out=sm[:, 0:1])
        ts("dve", sm[:, 1:2], sm[:, 0:1], 1.0 / D, 1e-6, ALU.mult, ALU.add, [smb], [smb])
        act(sm[:, 2:3], sm[:, 1:2], AF.Ln, [smb], [smb])
        act(sm[:, 3:4], sm[:, 2:3], AF.Exp, [smb], [smb], scale=-0.5)
        hn, hnb = hn_ring.next()
        ts("dve", hn[:], src, sm[:, 3:4], None, ALU.mult, None, [src_b, smb], [hnb])
        bank, bb = tp_ring.next()
        bk = bank[:].bitcast(BF16)
        for kc in range(8):
            tr(bk[:, kc * 128:(kc + 1) * 128], hn[:, kc * 128:(kc + 1) * 128], ident_b[:], [hnb, const_b], [bb])
        return (bk, bb, tt_i, gpre)

    def norm_B(st):
        bk, bb, tt_i, gpre = st
        c = tt_i // 4
        for kc in range(8):
            act(hT[:, kc, tt_i * 128:(tt_i + 1) * 128], bk[:, kc * 128:(kc + 1) * 128], AF.Copy,
                [bb, const_b], [hT_b[kc][c]], scale=gpre[:, kc:kc + 1])

    def load_w(q, src2d, ncols_elems, prefetch=True):
        wt, wb = wt_ring.next()
        dma(q, wt[:, 0:ncols_elems], src2d, (), [wb], prefetch=prefetch)
        return wt, wb

    pipe = []
    HEAT = 0

    def noop_():
        return None

    def attn_flush():
        n = len(pipe)
        if n == 0:
            return
        LA = 3
        for j in range(min(LA, n)):
            pipe[j]["S"]()
        for i, t in enumerate(pipe):
            t["E"]()
            if i + LA < n:
                pipe[i + LA]["S"]()
            if t["pre"] is not None:
                t["pre"]()
            t["PV"]()
            if HEAT and t["S"] is not noop_:
                hb, hbb = tp_ring.items[1]
                mm(hb[:, 0:HEAT], zeros_b[:, 0:128], zeros_b[:, 0:HEAT], True, True, [const_b], [hbb])
            if t["post"] is not None:
                t["post"]()
        del pipe[:]

    def attn_chunk(qc, qtile, q_b, ktile, k_b, rows, tiles, vget, vw, v_b, bias_get, finalize):
        obank, ob = oa_ring.next()
        o3 = obank[:, 0:4 * vw].rearrange("p (a b) -> p a b", b=vw)
        last = {}
        for i, (kt, c0, c1, mask) in enumerate(tiles):
            for qs in range(c0 // 128, c1 // 128):
                last[qs] = i
        ntl = len(tiles)
        for i, (kt, c0, c1, mask) in enumerate(tiles):
            sbank, sbb = sa_ring.next()
            pt, ptb = pt_ring.next()

            def fS(kt=kt, c0=c0, c1=c1, mask=mask, sbank=sbank, sbb=sbb):
                mm(sbank[:, c0:c1], ktile[rows, kt * 128:(kt + 1) * 128],
                   qtile[rows, qc * 512 + c0:qc * 512 + c1], True, mask is None, [q_b, k_b], [sbb])
                if mask is not None:
                    map_, m0, mw = mask
                    mm(sbank[:, m0:m0 + mw], ident_b[:], map_, False, True, [const_b], [sbb])

            def fE(kt=kt, c0=c0, c1=c1, sbank=sbank, sbb=sbb, pt=pt, ptb=ptb):
                kw = dict(scale=0.125)
                rd = [sbb]
                if bias_get is not None:
                    bap, bbuf = bias_get(kt)
                    kw["bias"] = bap
                    rd.append(bbuf)
                act(pt[:, c0:c1], sbank[:, c0:c1], AF.Exp, rd, [ptb], **kw)

            def fPV(i=i, kt=kt, c0=c0, c1=c1, pt=pt, ptb=ptb):
                for qs in range(c0 // 128, c1 // 128):
                    mm(o3[:, qs, :], pt[:, qs * 128:(qs + 1) * 128], vget(kt), False, last[qs] == i,
                       [ptb, v_b], [ob])

            pre = None
            post = None
            if i == 0:
                def pre():
                    mm(obank[:, :], zeros_b[:, 0:128], zeros_b[:, 0:512], True, False, [const_b], [ob])
            if i == ntl - 1:
                def post():
                    finalize(qc, o3, ob)
            pipe.append(dict(S=fS, E=fE, PV=fPV, pre=pre, post=post))

    def causal_tiles(qc):
        tl = [(kt, 0, 512, None) for kt in range(4 * qc)]
        for j in range(4):
            tl.append((4 * qc + j, 128 * j, 512, (tri_b[:, 0:128], 128 * j, 128)))
        return tl

    def window_tiles(qc):
        tl = []
        for j in range(-4, 4):
            kt = 4 * qc + j
            if kt < 0:
                continue
            if j >= 0:
                tl.append((kt, 128 * j, 512, (tri_b[:, 0:128], 128 * j, 128)))
            else:
                jj = j + 4
                tl.append((kt, 0, 128 * jj + 128, (tri_b[:, 128:256], 128 * jj, 128)))
        return tl

    for s in range(nseq):
        xs = x_d[s * T:(s + 1) * T, :]
        outs = out_d[s * T:(s + 1) * T, :]
        outD_b = [Buf("outD%d_%d" % (s, i)) for i in range(NT)]


        VF = aview(0, 8320).rearrange("p (a b c) -> p a b c", a=16, b=8)
        vf_b = Buf("VF")
        QK = [[aview(8320 + (st * 4 + i) * 2048, 2048) for i in range(4)] for st in range(2)]
        QK_b = [[Buf("QK%d_%d" % (st, i)) for i in range(4)] for st in range(2)]
        fox_tm = aview(8320 + 16384, 8192).rearrange("p (a b) -> p a b", b=512)
        fox_b = [Buf("fox%d" % i) for i in range(NT)]
        FOXT_OFF = 32896
        foxT = aview(FOXT_OFF, 8192).rearrange("p (a b) -> p a b", b=T)
        foxT_b = Buf("foxT")
        nsaT = aview(FOXT_OFF + 8192, 8192).rearrange("p (a b) -> p a b", b=T)
        nsaT_b = Buf("nsaT")

        wfvh = [load_w("pool", w_fv[hf, :, :], 2048) for hf in range(2)]
        memset("pool", VF[:, :, :, 64:65], 1.0, [vf_b])
        wg, wg_b = load_w("pool", w_ffng[:, :], 256)
        wg3 = wg[:, 0:256].rearrange("p (a b) -> p a b", b=32)
        wfv3 = [w[:, 0:2048].rearrange("p (a b) -> p a b", b=256) for (w, _) in wfvh]
        def tile_proj(ti):
            c = ti // 4
            for hf in range(2):
                bank, bb = pj_ring.next()
                for kc in range(8):
                    mm(bank[:, 0:256], hT[:, kc, ti * 128:(ti + 1) * 128], wfv3[hf][:, kc, :], kc == 0, kc == 7,
                       [hT_b[kc][c], wfvh[hf][1]], [bb])
                cp("dve", VF[:, ti, 4 * hf:4 * hf + 4, 0:64], bank[:, 0:256].rearrange("p (a b) -> p a b", b=64),
                   [bb], [vf_b])
            bank2, bb2 = oa_ring.next()
            for kc in range(8):
                mm(bank2[:, 0:32], hT[:, kc, ti * 128:(ti + 1) * 128], wg3[:, kc, :], kc == 0, kc == 7,
                   [hT_b[kc][c], wg_b], [bb2])
            sm, smb = sm_ring.next()
            tt("dve", sm[:, 0:8], bank2[:, 0:8], bfor[:], ALU.add, [bb2, const_b], [smb])
            act(sm[:, 8:16], sm[:, 0:8], AF.Exp, [smb], [smb], scale=-1.0)
            act(lp[:, ti, :], sm[:, 8:16], AF.Ln, [smb, const_b], [const_b], bias=onescol[:])
            tt("dve", lsum[:, ti + 1, :], lsum[:, ti, :], lp[:, ti, :], ALU.add, [const_b], [const_b])
            act(sm[:, 16:40], bank2[:, 8:32], AF.Exp, [bb2], [smb], scale=-1.0)
            ts("dve", sm[:, 16:40], sm[:, 16:40], 1.0, None, ALU.add, None, [smb], [smb])
            P.add("dve", "reciprocal", dict(out=gates[:, ti, :], in_=sm[:, 16:40]), [smb], [const_b])

        pend = None
        for ti in range(NT):
            xt, xb = xt_ring.next()
            dma("sp", xt[:], xs[ti * 128:(ti + 1) * 128, :], (), [xb])
            stA = norm_to_hT(xt[:], xb, ti, gpre1)
            if pend is not None:
                norm_B(pend)
                tile_proj(pend[2])
            pend = stA
        norm_B(pend)
        tile_proj(pend[2])
        bank, bb = tp_ring.next()
        for ti in range(NT):
            mm(bank[:, ti * 8:(ti + 1) * 8], u_f[:], lp[:, ti, :], True, False, [const_b], [bb])
            mm(bank[:, ti * 8:(ti + 1) * 8], ones_f[:], lsum[:, ti, :], False, True, [const_b], [bb])
        cp("dve", negc[:].rearrange("p a b -> p (a b)"), bank[:, 0:128], [bb], [const_b])
        for c in range(4):
            bank, bb = tp_ring.next()
            for j in range(4):
                ti = 4 * c + j
                mm(bank[0:8, j * 128:(j + 1) * 128], lp[:, ti, :], u_f[:], True, False, [const_b], [bb])
                mm(bank[0:8, j * 128:(j + 1) * 128], lsum[:, ti, :], ones_f[:], False, True, [const_b], [bb])
            act(caugT[0:8, c * 512:(c + 1) * 512], bank[0:8, :], AF.Copy, [bb], [const_b], scale=-8.0)

        for p in range(4):
            st = p % 2
            QA, KA, QB, KB = QK[st]
            QA_b, KA_b, QB_b, KB_b = QK_b[st]
            dma("sp", QA[64:65, :], caugT[2 * p:2 * p + 1, :], [const_b], [QA_b])
            dma("sp", QB[64:65, :], caugT[2 * p + 1:2 * p + 2, :], [const_b], [QB_b])
            dma("sp", KA[64:65, :], caugT[32:33, :], [const_b], [KA_b])
            dma("sp", KB[64:65, :], caugT[32:33, :], [const_b], [KB_b])
            for (blk, TA, TA_b, TB, TB_b) in ((p, QA, QA_b, QB, QB_b), (4 + p, KA, KA_b, KB, KB_b)):
                wt, wb = load_w("pool", w_fqk[blk, :, :], 1024)
                w3 = wt[:, 0:1024].rearrange("p (a b) -> p a b", b=128)
                for c in range(4):
                    bank, bb = pj_ring.next()
                    for kc in range(8):
                        mm(bank[:, :], w3[:, kc, :], hT[:, kc, c * 512:(c + 1) * 512], kc == 0, kc == 7,
                           [wb, hT_b[kc][c]], [bb])
                    cp("dve", TA[0:64, c * 512:(c + 1) * 512], bank[0:64, :], [bb], [TA_b])
                    cp("act", TB[0:64, c * 512:(c + 1) * 512], bank[64:128, :], [bb], [TB_b])
            for hh in range(2):
                h = 2 * p + hh
                qtile, q_b, ktile, k_b = (QA, QA_b, KA, KA_b) if hh == 0 else (QB, QB_b, KB, KB_b)
                rows = slice(0, 65)

                def fin(qc, o3, ob, h=h):
                    sm, smb = sm_ring.next()
                    P.add("dve", "reciprocal", dict(out=sm[:, 0:4], in_=o3[:, :, 64]), [ob], [smb])
                    tt("dve", fox_tm[:, qc * 4:qc * 4 + 4, h * 64:(h + 1) * 64], o3[:, :, 0:64],
                       sm[:, 0:4].unsqueeze(2).broadcast_to([128, 4, 64]), ALU.mult, [ob, smb],
                       fox_b[qc * 4:qc * 4 + 4])

                for qc in range(4):
                    attn_chunk(qc, qtile, q_b, ktile, k_b, rows, causal_tiles(qc),
                               lambda kt, h=h: VF[:, kt, h, :], 65, vf_b,
                               lambda kt, h=h: (negc[:, kt, h:h + 1], const_b), fin)
            attn_flush()
        for ti in range(NT):
            bank, bb = tp_ring.next()
            bk = bank[:].bitcast(BF16)
            for kc in range(4):
                tr(bk[:, kc * 128:(kc + 1) * 128], fox_tm[:, ti, kc * 128:(kc + 1) * 128], ident_b[:],
                   [fox_b[ti], const_b], [bb])
            cp("dve", foxT[:, :, ti * 128:(ti + 1) * 128],
               bk[:, 0:512].rearrange("p (a b) -> p a b", b=128), [bb], [foxT_b])
        if dbg and s == 0:
            for ti in range(NT):
                yt, yb = xt_ring.next()
                cp("dve", yt[:, 0:512], fox_tm[:, ti, :], [fox_b[ti]], [yb])
                dma("sp", dbg_d["d_fox"][:, ti * 512:(ti + 1) * 512], yt[:, 0:512], [yb], [], final=True)
            dma("sp", dbg_d["d_negc"][:, :], negc[:].rearrange("p a b -> p (a b)"), [const_b], [], final=True)
        P.barrier()

        QS = [[aview((g * 4 + p) * 2048, 2048) for p in range(4)] for g in range(2)]
        QS_b = [[[Buf("QS%d_%d_%d" % (g, p, c)) for c in range(4)] for p in range(4)] for g in range(2)]
        KS = [aview(16384 + g * 2048, 2048) for g in range(2)]
        KS_b = [Buf("KS%d" % g) for g in range(2)]
        KW = [aview(16384 + 4096 + g * 2048, 2048) for g in range(2)]
        KW_b = [Buf("KW%d" % g) for g in range(2)]
        KC = aview(16384 + 8192, 2048)
        KC_b = Buf("KC")
        VC = aview(16384 + 10240, 2048)
        VC_b = Buf("VC")
        VSW = aview(28672, 4160).rearrange("p (a b c) -> p a b c", a=16, b=4)
        vsw_b = Buf("VSW")
        nsa_acc = aview(16384 + 8192, 4096, F32).rearrange("p (a b) -> p a b", b=512)
        nacc_b = [Buf("nacc%d" % i) for i in range(4)]

        memset("pool", VSW[:, :, :, 64:65], 1.0, [vsw_b])
        engs = ["pool", "dve", "pool", "dve", "pool"]
        for g in range(2):
            memset(engs[0], KW[g][64:96, :], 0.0, [KW_b[g]])
            for p in range(4):
                memset(engs[1 + p], QS[g][p][64:96, :], 0.0, QS_b[g][p])
        dma("pool", KS[0][64:96, :], erows_d[:, :], (), [KS_b[0]])
        dma("pool", KS[1][64:96, :], erows_d[:, :], (), [KS_b[1]])
        wv, wv_b = load_w("pool", w_nv[:, :], 2048)
        wv3 = wv[:, 0:2048].rearrange("p (a b) -> p a b", b=256)
        for ti in range(NT):
            c = ti // 4
            bank, bb = pj_ring.next()
            for kc in range(8):
                mm(bank[:, 0:256], hT[:, kc, ti * 128:(ti + 1) * 128], wv3[:, kc, :], kc == 0, kc == 7,
                   [hT_b[kc][c], wv_b], [bb])
            cp("dve", VSW[:, ti, :, 0:64], bank[:, 0:256].rearrange("p (a b) -> p a b", b=64), [bb], [vsw_b])

        def proj_fm(blk_plain, blk_swap, wsrc, dests):
            wp, wpb = load_w("pool", wsrc[blk_plain, :, :], 1024)
            wp3 = wp[:, 0:1024].rearrange("p (a b) -> p a b", b=128)
            if blk_swap is not None:
                wq, wqb = load_w("pool", wsrc[blk_swap, :, :], 1024)
                wq3 = wq[:, 0:1024].rearrange("p (a b) -> p a b", b=128)
            for c in range(4):
                b1, bb1 = pj4_ring.next()
                for kc in range(8):
                    mm(b1[:, :], wp3[:, kc, :], hT[:, kc, c * 512:(c + 1) * 512], kc == 0, kc == 7,
                       [wpb, hT_b[kc][c]], [bb1])
                dests_c = [(t_, (b_[c] if isinstance(b_, list) else b_), r1_, r2_) for (t_, b_, r1_, r2_) in dests]
                if blk_swap is None:
                    for (tile_, tb_, rs, rd_) in dests_c:
                        cp("act", tile_[rd_, c * 512:(c + 1) * 512], b1[rs, :], [bb1], [tb_])
                    continue
                b2, bb2 = pj4_ring.next()
                for kc in range(8):
                    mm(b2[:, :], wq3[:, kc, :], hT[:, kc, c * 512:(c + 1) * 512], kc == 0, kc == 7,
                       [wqb, hT_b[kc][c]], [bb2])
                rp, rpb = rope_ring.next()
                dma("sp", rp[:], rope_d[c, :, :], (), [rpb])
                ta, tab = t1_ring.next()
                tb, tbb = t1_ring.next()
                tt("dve", ta[:], b1[:, :], rp[:, 0:512], ALU.mult, [bb1, rpb], [tab])
                tt("dve", tb[:], b2[:, :], rp[:, 512:1024], ALU.mult, [bb2, rpb], [tbb])
                for (tile_, tb_, rs, rd_) in dests_c:
                    if rs == rd_:
                        tt("dve" if len(dests_c) > 1 else "pool", tile_[rd_, c * 512:(c + 1) * 512], ta[rs, :], tb[rs, :],
                           ALU.add, [tab, tbb], [tb_])
                    else:
                        tt("pool", ta[rs, :], ta[rs, :], tb[rs, :], ALU.add, [tab, tbb], [tab])
                        cp("act", tile_[rd_, c * 512:(c + 1) * 512], ta[rs, :], [tab], [tb_])

        for p in range(4):
            proj_fm(p, 4 + p, w_nq, [(QS[0][p], QS_b[0][p], slice(0, 64), slice(0, 64)),
                                     (QS[1][p], QS_b[1][p], slice(64, 128), slice(0, 64))])
        proj_fm(0, 1, w_nk, [(KC, KC_b, slice(0, 128), slice(0, 128))])
        proj_fm(2, 3, w_nk, [(KS[0], KS_b[0], slice(0, 64), slice(0, 64)), (KS[1], KS_b[1], slice(64, 128), slice(0, 64))])
        proj_fm(4, 5, w_nk, [(KW[0], KW_b[0], slice(0, 64), slice(0, 64)), (KW[1], KW_b[1], slice(64, 128), slice(0, 64))])
        proj_fm(6, None, w_nk, [(VC, VC_b, slice(0, 128), slice(0, 128))])

        for (src, src_b, w1, cb, sT) in ((KC, KC_b, w1k, cbk, sTk), (VC, VC_b, w1v, cbv, sTv)):
            for g in range(2):
                rs = slice(64 * g, 64 * g + 64)
                bank, bb = tp_ring.next()
                for l in range(32):
                    mm(bank[0:64, 0:127], w1[rs, l, :], src[rs, l:l + 2017:16], l == 0, l == 31,
                       [const_b, src_b], [bb])
                act(sT[:, g, 0:127], bank[0:64, 0:127], AF.Silu, [bb, const_b], [const_b], bias=cb[:])
        for g in range(2):
            bank, bb = tp_ring.next()
            mm(bank[0:64, 0:128], w2k[:], sTk[:, g, :], True, True, [const_b], [bb])
            cp("dve", kcmp[0:64, g, :], bank[0:64, 0:128], [bb], [const_b])
        for g in range(2):
            bank, bb = tp_ring.next()
            mm(bank[:, 0:64], sTv[:, g, :], w2v[:], True, True, [const_b], [bb])
            cp("dve", vcmp[:, g, 0:64], bank[:, 0:64], [bb], [const_b])

        P.barrier()
        psl_b = Buf("psl")
        mbw_b = Buf("mbw")
        mbT_b = Buf("mbT")
        ocmp = [xt_ring.items[k][0][:, :].rearrange("p (a b) -> p a b", b=512) for k in range(2)]
        ocmp_b = [xt_ring.items[k][1] for k in range(2)]

        def bc(ap2, n):
            return ap2.unsqueeze(2).broadcast_to([128, ap2.shape[1], n])

        noop = noop_

        def add_cmp_sel(qn):
            for g in range(2):
                for p in range(4):
                    h = 4 * g + p

                    def fin_cmp(qc, o3, ob, h=h, p=p):
                        sm, smb = sm_ring.next()
                        hs = slice(h * 64, (h + 1) * 64)
                        ts("dve", sm[:, 0:4], o3[:, :, 64], 1e-30, None, ALU.max, None, [ob], [smb])
                        P.add("dve", "reciprocal", dict(out=sm[:, 4:8], in_=sm[:, 0:4]), [smb], [smb])
                        tt("dve", sm[:, 8:12], sm[:, 4:8], gates[:, qc * 4:qc * 4 + 4, 3 * h], ALU.mult,
                           [smb, const_b], [smb])
                        for k in range(2):
                            tt("dve", ocmp[k][:, :, hs], o3[:, 2 * k:2 * k + 2, 0:64], bc(sm[:, 8 + 2 * k:10 + 2 * k], 64),
                               ALU.mult, [ob, smb], [ocmp_b[k]])
                        if p == 0:
                            tt("dve", psl[:, :, :], o3[:, :, 65:97], bc(sm[:, 4:8], 32), ALU.mult, [ob, smb], [psl_b])
                        else:
                            tmp, tmpb = t1_ring.next()
                            tmp3 = tmp[:, 0:128].rearrange("p (a b) -> p a b", b=32)
                            tt("dve", tmp3, o3[:, :, 65:97], bc(sm[:, 4:8], 32), ALU.mult, [ob, smb], [tmpb])
                            tt("dve", psl[:, :, :], psl[:, :, :], tmp3, ALU.add, [psl_b, tmpb], [psl_b])

                    attn_chunk(qn, QS[g][p], QS_b[g][p][qn], kcmp[:, g, :], const_b, slice(0, 96),
                               [(0, 0, 512, (cmpmask_b[:, qn * 512:(qn + 1) * 512], 0, 512))],
                               lambda kt, g=g: vcmp[:, g, :], 97, const_b, None, fin_cmp)

                def selection(g=g):
                    sc, scb = t1_ring.next()
                    sc3 = sc[:, 0:128].rearrange("p (a b) -> p a b", b=32)
                    sm, smb = sm_ring.next()
                    tt("dve", sc3, psl[:, :, :], fbt[:, qn * 4:qn * 4 + 4, :], ALU.add, [psl_b, const_b], [scb])
                    for qs in range(4):
                        P.add("dve", "max", dict(out=sm[:, 8 * qs:8 * qs + 8], in_=sc3[:, qs, :]), [scb], [smb])
                    tt("dve", sc3, sc3, bc(sm[:, 7:32:8], 32), ALU.is_lt, [scb, smb], [scb])
                    ts("dve", mbw[:, g, :, 64:96], sc3, NEGM, None, ALU.mult, None, [scb], [mbw_b])
                    bank, bb = tp_ring.next()
                    bk = bank[:].bitcast(BF16)
                    for qs in range(4):
                        tr(bk[0:96, qs * 128:(qs + 1) * 128], mbw[:, g, qs, :], ident_b[:], [mbw_b, const_b], [bb])
                    cp("dve", mbT[:, g, :], bk[0:96, 0:512], [bb], [mbT_b])
                    for p in range(4):
                        cp("pool", QS[g][p][64:96, qn * 512:(qn + 1) * 512], mbT[64:96, g, :], [mbT_b],
                           [QS_b[g][p][qn]])

                pipe.append(dict(S=noop, E=noop, PV=noop, pre=None, post=selection))

        def add_sel_win(qc, which):
            for g in range(2):
                for p in range(4):
                    h = 4 * g + p

                    def fin_acc(br, h=h):
                        def f(qc, o3, ob):
                            sm, smb = sm_ring.next()
                            hs = slice(h * 64, (h + 1) * 64)
                            P.add("dve", "reciprocal", dict(out=sm[:, 0:4], in_=o3[:, :, 64]), [ob], [smb])
                            tt("dve", sm[:, 4:8], sm[:, 0:4], gates[:, qc * 4:qc * 4 + 4, 3 * h + br], ALU.mult,
                               [smb, const_b], [smb])
                            tmp, tmpb = t1_ring.next()
                            tmp3 = tmp[:, 0:256].rearrange("p (a b) -> p a b", b=64)
                            tt("dve", tmp3, o3[:, :, 0:64], bc(sm[:, 4:8], 64), ALU.mult, [ob, smb], [tmpb])
                            if br == 1:
                                for k in range(2):
                                    tt("dve", nsa_acc[:, 2 * k:2 * k + 2, hs], tmp3[:, 2 * k:2 * k + 2, :],
                                       ocmp[k][:, :, hs], ALU.add, [tmpb, ocmp_b[k]], nacc_b[2 * k:2 * k + 2])
                            else:
                                tt("dve", nsa_acc[:, :, hs], nsa_acc[:, :, hs], tmp3, ALU.add, [tmpb] + nacc_b, nacc_b)
                        return f

                    if which == 1:
                        attn_chunk(qc, QS[g][p], QS_b[g][p][qc], KS[g], KS_b[g], slice(0, 96), causal_tiles(qc),
                                   lambda kt, g=g: VSW[:, kt, g, :], 65, vsw_b, None, fin_acc(1))
                    else:
                        attn_chunk(qc, QS[g][p], QS_b[g][p][qc], KW[g], KW_b[g], slice(0, 96), window_tiles(qc),
                                   lambda kt, g=g: VSW[:, kt, 2 + g, :], 65, vsw_b, None, fin_acc(2))

        add_cmp_sel(0)
        attn_flush()
        for qc in range(4):
            add_sel_win(qc, 1)
            if qc + 1 < 4:
                add_cmp_sel(qc + 1)
            add_sel_win(qc, 2)
            attn_flush()
            for qs in range(4):
                ti = qc * 4 + qs
                hn, hnb = hn_ring.next()
                cp("pool", hn[:, 0:512], nsa_acc[:, qs, :], [nacc_b[qs]], [hnb])
                if dbg and s == 0:
                    dma("sp", dbg_d["d_nsa"][:, ti * 512:(ti + 1) * 512], nsa_acc[:, qs, :], [nacc_b[qs]], [],
                        final=True)
                bank, bb = tp_ring.next()
                bk = bank[:].bitcast(BF16)
                for kc in range(4):
                    tr(bk[:, kc * 128:(kc + 1) * 128], hn[:, kc * 128:(kc + 1) * 128], ident_b[:],
                       [hnb, const_b], [bb])
                cp("dve", nsaT[:, :, ti * 128:(ti + 1) * 128],
                   bk[:, 0:512].rearrange("p (a b) -> p a b", b=128), [bb], [nsaT_b])
        P.barrier()

        mixT = aview(0, 16384).rearrange("p (a b) -> p a b", b=T)
        mixT_b = [[Buf("mix%d_%d" % (c, t4)) for t4 in range(4)] for c in range(8)]
        for cb_ in range(8):
            wga, wga_b = load_w("pool", w_gab[cb_, :, :], 1024)
            wgb, wgb_b = load_w("pool", w_gab[8 + cb_, :, :], 1024)
            wfo, wfo_b = load_w("pool", w_fo[cb_, :, :], 512)
            wno, wno_b = load_w("pool", w_no[cb_, :, :], 512)
            wga3 = wga[:, 0:1024].rearrange("p (a b) -> p a b", b=128)
            wgb3 = wgb[:, 0:1024].rearrange("p (a b) -> p a b", b=128)
            wfo3 = wfo[:, 0:512].rearrange("p (a b) -> p a b", b=128)
            wno3 = wno[:, 0:512].rearrange("p (a b) -> p a b", b=128)
            for c in range(4):
                cs = slice(c * 512, (c + 1) * 512)
                res = []
                for (wg3_, wgb_, wo3_, wob_, srcT, srcb) in ((wga3, wga_b, wfo3, wfo_b, foxT, foxT_b),
                                                           (wgb3, wgb_b, wno3, wno_b, nsaT, nsaT_b)):
                    b1, bb1 = pj4_ring.next()
                    for kc in range(8):
                        mm(b1[:, :], wg3_[:, kc, :], hT[:, kc, cs], kc == 0, kc == 7, [wgb_, hT_b[kc][c]], [bb1])
                    sg, sgb = t1_ring.next()
                    act(sg[:], b1[:, :], AF.Sigmoid, [bb1], [sgb])
                    b2, bb2 = pj4_ring.next()
                    for kc in range(4):
                        mm(b2[:, :], wo3_[:, kc, :], srcT[:, kc, cs], kc == 0, kc == 3, [wob_, srcb], [bb2])
                    tt("dve", sg[:], sg[:], b2[:, :], ALU.mult, [sgb, bb2], [sgb])
                    res.append((sg, sgb))
                tt("pool", mixT[:, cb_, cs], res[0][0][:], res[1][0][:], ALU.add, [res[0][1], res[1][1]],
                   [mixT_b[cb_][c]])
        P.barrier()

        dma("sp", gpost1[:], gpost1_d.partition_broadcast(128), (), [gpost_b])
        WO = aview(16384, 8192).rearrange("p (h a b) -> p h a b", h=2, a=8)
        wo_b = Buf("WO")
        for hf in range(2):
            dma("pool", WO[:, hf, :, :].rearrange("p a b -> p (a b)"), w_o[hf, :, :], (), [wo_b])
        pend = None
        for ti in range(NT):
            c = ti // 4
            yt, yb = yt_ring.next()
            for hf in range(2):
                bank, bb = pj_ring.next()
                for kc in range(8):
                    mm(bank[:, :], mixT[:, kc, ti * 128:(ti + 1) * 128], WO[:, hf, kc, :], kc == 0, kc == 7,
                       [mixT_b[kc][c], wo_b], [bb])
                cp("act" if hf == 0 else "dve", yt[:, hf * 512:(hf + 1) * 512], bank[:, :], [bb], [yb])
            sm, smb = sm_ring.next()
            act(junk[:], yt[:], AF.Square, [yb], [junk_b, smb], accum_out=sm[:, 0:1])
            ts("dve", sm[:, 1:2], sm[:, 0:1], 1.0 / D, 1e-6, ALU.mult, ALU.add, [smb], [smb])
            act(sm[:, 2:3], sm[:, 1:2], AF.Ln, [smb], [smb])
            act(sm[:, 3:4], sm[:, 2:3], AF.Exp, [smb], [smb], scale=-0.5)
            stt(yt[:], yt[:], sm[:, 3:4], gpost1[:], ALU.mult, ALU.mult, [yb, smb, gpost_b], [yb])
            xt, xb = xt_ring.next()
            dma("sp", xt[:], xs[ti * 128:(ti + 1) * 128, :], (), [xb])
            tt("pool", xt[:], xt[:], yt[:], ALU.add, [xb, yb], [xb])
            dma("sp", outs[ti * 128:(ti + 1) * 128, :], xt[:], [xb], [outD_b[ti]])
            stA = norm_to_hT(xt[:], xb, ti, gpre2)
            if pend is not None:
                norm_B(pend)
            pend = stA
        norm_B(pend)
        P.barrier()

        dma("sp", gpost2[:], gpost2_d.partition_broadcast(128), (), [gpost_b])
        yacc = aview(0, 32768, F32).rearrange("p (a b) -> p a b", b=1024)
        yacc_b = [[Buf("yacc%d_%d" % (i, hf)) for hf in range(2)] for i in range(NT)]
        uT = [aview(32768 + i * 4096, 4096).rearrange("p (a b) -> p a b", b=T) for i in range(2)]
        uT_b = [[[Buf("uT%d_%d_%d" % (i, f, c)) for c in range(4)] for f in range(2)] for i in range(2)]
        def mlp_up(fg):
            wu, wu_b = load_w("pool", w_up[fg, :, :], 2048)
            wu3 = wu[:, 0:2048].rearrange("p (a b) -> p a b", b=256)
            ub = fg % 2
            for fc in range(2):
                for c in range(4):
                    cs = slice(c * 512, (c + 1) * 512)
                    bank, bb = pj_ring.next()
                    for kc in range(8):
                        mm(bank[:, :], wu3[:, kc, fc * 128:(fc + 1) * 128], hT[:, kc, cs], kc == 0, kc == 7,
                           [wu_b, hT_b[kc][c]], [bb])
                    r, rb = t1_ring.next()
                    act(r[:], bank[:, :], AF.Relu, [bb], [rb])
                    act(uT[ub][:, fc, cs], r[:], AF.Square, [rb], [uT_b[ub][fc][c]])

        mlp_up(0)
        for fg in range(16):
            wd, wd_b = load_w("pool", w_dn[fg, :, :], 2048)
            wd3 = wd[:, 0:2048].rearrange("p (a b) -> p a b", b=1024)
            ub = fg % 2
            if fg + 1 < 16:
                mlp_up(fg + 1)
            for ti in range(NT):
                c = ti // 4
                for hf in range(2):
                    bank, bb = y_ring.next()
                    for fc in range(2):
                        mm(bank[:, :], uT[ub][:, fc, ti * 128:(ti + 1) * 128], wd3[:, fc, hf * 512:(hf + 1) * 512],
                           fc == 0, fc == 1, [uT_b[ub][fc][c], wd_b], [bb])
                    ya = yacc[:, ti, hf * 512:(hf + 1) * 512]
                    if fg == 0:
                        cp("act", ya, bank[:, :], [bb], [yacc_b[ti][hf]])
                    else:
                        tt("dve", ya, bank[:, :], ya, ALU.add, [bb, yacc_b[ti][hf]], [yacc_b[ti][hf]])
        for ti in range(NT):
            sm, smb = sm_ring.next()
            act(junk[:], yacc[:, ti, :], AF.Square, yacc_b[ti], [junk_b, smb], accum_out=sm[:, 0:1])
            ts("dve", sm[:, 1:2], sm[:, 0:1], 1.0 / D, 1e-6, ALU.mult, ALU.add, [smb], [smb])
            act(sm[:, 2:3], sm[:, 1:2], AF.Ln, [smb], [smb])
            act(sm[:, 3:4], sm[:, 2:3], AF.Exp, [smb], [smb], scale=-0.5)
            yt, yb = yt_ring.next()
            stt(yt[:], yacc[:, ti, :], sm[:, 3:4], gpost2[:], ALU.mult, ALU.mult, yacc_b[ti] + [smb, gpost_b], [yb])
            xt, xb = xt_ring.next()
            dma("sp", xt[:], outs[ti * 128:(ti + 1) * 128, :], [outD_b[ti]], [xb])
            tt("pool", xt[:], xt[:], yt[:], ALU.add, [xb, yb], [xb])
            dma("sp", outs[ti * 128:(ti + 1) * 128, :], xt[:], [xb], [outD_b[ti]], final=True)
        P.barrier()

    P.finalize()
    P.emit(nc, es)
    es.close()
    return nc


def _blk(w, cols):
    sub = w[:, cols]
    n = sub.shape[1]
    return np.ascontiguousarray(sub.reshape(8, 128, n).transpose(1, 0, 2).reshape(128, 8 * n))


def _rowblk(w, nkc):
    n = w.shape[1]
    return np.ascontiguousarray(w.reshape(nkc, 128, n).transpose(1, 0, 2).reshape(128, nkc * n))


def _const_tables():
    f32 = np.float32
    inv = np.power(f32(10000.0), -np.arange(0, 64, 2, dtype=f32) / f32(64)).astype(f32)
    ang = (np.arange(T, dtype=f32)[:, None] * inv[None, :]).astype(f32)
    cos = np.cos(ang).astype(f32).T
    sin = np.sin(ang).astype(f32).T
    rope = np.zeros((4, 128, 1024), f32)
    for r in range(128):
        j = r % 32
        sgn = -1.0 if (r % 64) < 32 else 1.0
        for c in range(4):
            rope[c, r, 0:512] = cos[j, c * 512:(c + 1) * 512]
            rope[c, r, 512:1024] = sgn * sin[j, c * 512:(c + 1) * 512]
    n = np.arange(128)[:, None]
    t = np.arange(T)[None, :]
    cmpmask = np.where((16 * n + 31 <= t) & (n < 127), 0.0, NEGM).astype(f32)
    tt_ = np.arange(T)
    cur = tt_ // 64
    jj = np.arange(32)[None, :]
    forced = (jj == 0) | (jj == cur[:, None]) | (jj == cur[:, None] - 1)
    valid = jj <= cur[:, None]
    fb = np.where(valid, 1e4 * forced.astype(f32), -1e30).astype(f32)
    fbt = np.ascontiguousarray(fb.reshape(16, 128, 32).transpose(1, 0, 2).reshape(128, 512))
    sI = np.arange(128)[:, None]
    tI = np.arange(128)[None, :]
    tri = np.concatenate([np.where(sI > tI, NEGM, 0.0), np.where(tI >= sI, NEGM, 0.0)], axis=1).astype(f32)
    ident = np.eye(128, dtype=f32)
    umat = (sI <= tI).astype(f32)
    ci = np.arange(128)[:, None] * 16
    sj = np.arange(32)[None, :] * 64
    ov = (((ci < sj + 64) & (ci + 32 > sj)) & (np.arange(128)[:, None] < 127)).astype(f32)
    erows = (np.arange(32)[:, None] == (np.arange(T)[None, :] // 64)).astype(f32)
    return dict(rope=rope, cmpmask=cmpmask, fbt=fbt, tri=tri, ident=ident, umat=umat, ov=ov, erows=erows)


def _layout_weights(inp):
    f = lambda a: np.asarray(a, dtype=np.float32)
    w_in = f(inp["w_in"])[0]
    o = {}
    ar = np.arange
    FQ, FK, FV, FF, NQ = 0, 512, 1024, 1536, 1544
    KCo, VCo, KSo, VSo, KWo, VWo, NG, GA, GB = 2056, 2184, 2312, 2440, 2568, 2696, 2824, 2848, 3872
    o["w_fqk"] = np.stack([_blk(w_in, FQ + 128 * p + ar(128)) for p in range(4)] +
                          [_blk(w_in, FK + 128 * p + ar(128)) for p in range(4)])
    o["w_fv"] = np.stack([_blk(w_in, FV + 256 * hf + ar(256)) for hf in range(2)])
    o["w_ffng"] = _blk(w_in, np.concatenate([FF + ar(8), NG + ar(24)]))
    sw = (ar(64) + 32) % 64
    nq_plain, nq_swap = [], []
    for p in range(4):
        nq_plain.append(_blk(w_in, np.concatenate([NQ + 64 * p + ar(64), NQ + 64 * (4 + p) + ar(64)])))
        nq_swap.append(_blk(w_in, np.concatenate([NQ + 64 * p + sw, NQ + 64 * (4 + p) + sw])))
    o["w_nq"] = np.stack(nq_plain + nq_swap)
    sw2 = np.concatenate([sw, 64 + sw])
    o["w_nk"] = np.stack([_blk(w_in, KCo + ar(128)), _blk(w_in, KCo + sw2),
                          _blk(w_in, KSo + ar(128)), _blk(w_in, KSo + sw2),
                          _blk(w_in, KWo + ar(128)), _blk(w_in, KWo + sw2),
                          _blk(w_in, VCo + ar(128))])
    o["w_nv"] = _blk(w_in, np.concatenate([VSo + ar(128), VWo + ar(128)]))
    o["w_gab"] = np.stack([_blk(w_in, GA + 128 * c + ar(128)) for c in range(8)] +
                          [_blk(w_in, GB + 128 * c + ar(128)) for c in range(8)])
    wfo = f(inp["w_fox_out"])[0]
    wno = f(inp["w_nsa_out"])[0]
    o["w_fo"] = np.stack([_rowblk(wfo[:, 128 * c:128 * (c + 1)], 4) for c in range(8)])
    o["w_no"] = np.stack([_rowblk(wno[:, 128 * c:128 * (c + 1)], 4) for c in range(8)])
    wo = f(inp["w_o"])[0]
    o["w_o"] = np.stack([_rowblk(wo[:, 512 * h:512 * (h + 1)], 8) for h in range(2)])
    wup = f(inp["w_up"])[0]
    wdn = f(inp["w_down"])[0]
    o["w_up"] = np.stack([_blk(wup, 256 * g + ar(256)) for g in range(16)])
    o["w_dn"] = np.stack([_rowblk(wdn[256 * g:256 * (g + 1), :], 2) for g in range(16)])
    for nm, key in (("w1k", "cmp_w1_k"), ("w1v", "cmp_w1_v")):
        w1 = f(inp[key])[0].reshape(32, 64, 64).transpose(1, 0, 2).reshape(64, 2048)
        o[nm] = np.ascontiguousarray(np.concatenate([w1, w1], axis=0))
    o["w2k"] = np.ascontiguousarray(f(inp["cmp_w2_k"])[0])
    o["w2v"] = np.ascontiguousarray(f(inp["cmp_w2_v"])[0])
    o["posk"] = np.ascontiguousarray(f(inp["cmp_pos_k"])[0].T)
    o["posv"] = np.ascontiguousarray(f(inp["cmp_pos_v"])[0].T)
    o["gpre1"] = np.ascontiguousarray(f(inp["norm_mix_pre"])[0].reshape(8, 128).T)
    o["gpre2"] = np.ascontiguousarray(f(inp["norm_mlp_pre"])[0].reshape(8, 128).T)
    o["gpost1"] = np.ascontiguousarray(f(inp["norm_mix_post"])[0].reshape(1, 1024))
    o["gpost2"] = np.ascontiguousarray(f(inp["norm_mlp_post"])[0].reshape(1, 1024))
    o["bfor"] = np.ascontiguousarray(f(inp["b_forget"])[0].reshape(1, 8))
    o.update(_const_tables())
    return o


_NC_CACHE = {}


def kernel(**inputs):
    x = np.asarray(inputs["x"], dtype=np.float32)
    B = x.shape[0]
    per = B // NCORES
    shared = _layout_weights(inputs)
    if per not in _NC_CACHE:
        _NC_CACHE[per] = build_program(per)
    nc = _NC_CACHE[per]
    in_maps = []
    for c in range(NCORES):
        m = dict(shared)
        m["x"] = np.ascontiguousarray(x[c * per:(c + 1) * per].reshape(per * T, D))
        in_maps.append(m)
    res = run_bass_kernel_spmd(nc, in_maps, core_ids=list(range(NCORES)))
    outs = [np.asarray(r["out"]).reshape(per, T, D) for r in res.results]
    return np.concatenate(outs, axis=0).astype(np.float32)
```

```python
import numpy as np
from contextlib import ExitStack
import concourse.bass as bass
import concourse.mybir as mybir
from concourse.bass_utils import run_bass_kernel_spmd

F32 = mybir.dt.float32
BF16 = mybir.dt.bfloat16
AF = mybir.ActivationFunctionType
ALU = mybir.AluOpType

T = 2048
D = 1024
NT = 16
NEGM = -30000.0
SEQ_PER_CORE = 4
NCORES = 8


class Buf:
    __slots__ = ("name", "last_w", "readers", "dma_readers")

    def __init__(self, name):
        self.name = name
        self.last_w = None
        self.readers = {}
        self.dma_readers = []


class Op:
    __slots__ = ("eng", "meth", "kw", "deps", "idx", "dma", "tok", "marked", "semval")


COMPUTE = ("pe", "act", "dve", "pool")
QUEUES = ("sp", "pool")
NDSEM = 12


class Prog:
    def __init__(self):
        self.streams = {e: [] for e in ("pe", "act", "dve", "pool", "sp")}
        self.ndma = {q: 0 for q in QUEUES}
        self.dma_ops = {q: [] for q in QUEUES}
        self.barrier_deps = {e: [] for e in self.streams}
        self.dmas_since_barrier = []
        self.final_deps = []

    def add(self, eng, meth, kw, reads=(), writes=(), dma=False, prefetch=False, final=False):
        op = Op()
        op.eng, op.meth, op.kw, op.dma = eng, meth, kw, dma
        op.marked = False
        op.semval = None
        stream = self.streams[eng]
        op.idx = len(stream)
        deps = set()
        for b in reads:
            if b.last_w is not None:
                deps.add(b.last_w)
        for b in writes:
            if b.last_w is not None:
                deps.add(b.last_w)
            for e, i in b.readers.items():
                deps.add(("c", e, i))
            for t in b.dma_readers:
                deps.add(t)
        for t in self.barrier_deps[eng]:
            deps.add(t)
        self.barrier_deps[eng] = []
        if dma:
            j = self.ndma[eng]
            self.ndma[eng] += 1
            op.tok = ("d", eng, j)
            if j >= NDSEM:
                deps.add(("d", eng, j - NDSEM))
            self.dma_ops[eng].append(op)
            if not prefetch:
                self.dmas_since_barrier.append(op.tok)
            if final:
                self.final_deps.append(op.tok)
        else:
            op.tok = ("c", eng, op.idx)
        fdeps = []
        for d in deps:
            if d[0] == "c" and d[1] == eng and not dma:
                if eng == "pe":
                    continue
                fdeps.append(d)
            else:
                fdeps.append(d)
        op.deps = fdeps
        for b in reads:
            if dma:
                b.dma_readers.append(op.tok)
            else:
                b.readers[eng] = op.idx
        for b in writes:
            b.last_w = op.tok
            b.readers = {}
            b.dma_readers = []
        stream.append(op)
        return op

    def barrier(self):
        toks = []
        for e in COMPUTE:
            if self.streams[e]:
                last = self.streams[e][-1]
                if not last.dma:
                    toks.append(last.tok)
                else:
                    for o in reversed(self.streams[e]):
                        if not o.dma:
                            toks.append(o.tok)
                            break
        toks += self.dmas_since_barrier
        self.dmas_since_barrier = []
        for e in self.streams:
            self.barrier_deps[e] = list(self.barrier_deps[e]) + toks

    def finalize(self):
        op = self.add("sp", "nop_final", {}, (), ())
        op.deps = list(self.final_deps)
        for e, stream in self.streams.items():
            for op in stream:
                for d in op.deps:
                    if d[0] == "c":
                        self.streams[d[1]][d[2]].marked = True
        for e in COMPUTE:
            n = 0
            for op in self.streams[e]:
                if op.dma:
                    continue
                if op.marked:
                    n += 1
                    op.semval = n

    def emit(self, nc, es):
        esem = {e: es.enter_context(nc.semaphore("s_" + e)) for e in COMPUTE}
        dsem = {q: [es.enter_context(nc.semaphore("d_%s%d" % (q, i))) for i in range(NDSEM)]
                for q in QUEUES}
        block = es.enter_context(nc.Block())
        streams = self.streams

        def tokwait(d):
            if d[0] == "c":
                return ("c" + d[1], esem[d[1]], streams[d[1]][d[2]].semval)
            q, j = d[1], d[2]
            return ("d%s%d" % (q, j % NDSEM), dsem[q][j % NDSEM], 16 * (j // NDSEM + 1))

        def run(eng, name):
            waited = {}
            for op in streams[name]:
                for d in op.deps:
                    key, sem, val = tokwait(d)
                    if waited.get(key, 0) >= val:
                        continue
                    waited[key] = val
                    eng.wait_ge(sem, val)
                if op.meth == "nop_final":
                    continue
                ins = getattr(eng, op.meth)(**op.kw)
                if op.dma:
                    j = op.tok[2]
                    ins.then_inc(dsem[name][j % NDSEM], 16)
                elif op.marked:
                    ins.then_inc(esem[name], 1)

        @block.sync
        def _(e):
            run(e, "sp")

        @block.gpsimd
        def _(e):
            run(e, "pool")

        @block.scalar
        def _(e):
            run(e, "act")

        @block.vector
        def _(e):
            run(e, "dve")

        @block.tensor
        def _(e):
            run(e, "pe")


class Ring:
    def __init__(self, items):
        self.items = items
        self.i = 0

    def next(self):
        it = self.items[self.i % len(self.items)]
        self.i += 1
        return it


def build_program(nseq=SEQ_PER_CORE, dbg=False):
    nc = bass.Bass("TRN2", target_bir_lowering=False)
    P = Prog()
    es = ExitStack()

    def din(name, shape, dt=F32):
        return nc.dram_tensor(name, list(shape), dt, kind="ExternalInput").ap()

    x_d = din("x", [nseq * T, D])
    out_d = nc.dram_tensor("out", [nseq * T, D], F32, kind="ExternalOutput").ap()
    w_fqk = din("w_fqk", [8, 128, 1024])
    w_fv = din("w_fv", [2, 128, 8 * 256])
    w_ffng = din("w_ffng", [128, 8 * 32])
    w_nq = din("w_nq", [8, 128, 1024])
    w_nk = din("w_nk", [7, 128, 1024])
    w_nv = din("w_nv", [128, 8 * 256])
    w_gab = din("w_gab", [16, 128, 1024])
    w_fo = din("w_fo", [8, 128, 512])
    w_no = din("w_no", [8, 128, 512])
    w_o = din("w_o", [2, 128, 8 * 512])
    w_up = din("w_up", [16, 128, 8 * 256])
    w_dn = din("w_dn", [16, 128, 2 * 1024])
    w1k_d = din("w1k", [128, 2048])
    w1v_d = din("w1v", [128, 2048])
    w2k_d = din("w2k", [64, 64])
    w2v_d = din("w2v", [64, 64])
    posk_d = din("posk", [64, 32])
    posv_d = din("posv", [64, 32])
    rope_d = din("rope", [4, 128, 1024])
    cmpmask_d = din("cmpmask", [128, 2048])
    fbt_d = din("fbt", [128, 16 * 32])
    tri_d = din("tri", [128, 256])
    ident_d = din("ident", [128, 128])
    u_d = din("umat", [128, 128])
    ov_d = din("ov", [128, 32])
    erows_d = din("erows", [32, 2048])
    gpre1_d = din("gpre1", [128, 8])
    gpre2_d = din("gpre2", [128, 8])
    gpost1_d = din("gpost1", [1, 1024])
    gpost2_d = din("gpost2", [1, 1024])
    bfor_d = din("bfor", [1, 8])
    dbg_d = {}
    if dbg:
        for nm, shp in (("d_fox", [128, 16 * 512]), ("d_nsa", [128, 16 * 512]),
                        ("d_negc", [128, 128])):
            dbg_d[nm] = nc.dram_tensor(nm, shp, F32, kind="ExternalOutput").ap()

    def sb(name, shape, dt):
        return es.enter_context(nc.sbuf_tensor(name, list(shape), dt))

    def ps(name, shape, dt):
        return es.enter_context(nc.psum_tensor(name, list(shape), dt))

    hT = sb("hT", [128, 8, T], BF16)
    hT_b = [[Buf("hT%d_%d" % (k, c)) for c in range(4)] for k in range(8)]
    ARENA_ELEMS = 50176
    arena = sb("arena", [128, ARENA_ELEMS], BF16)
    ident_f = sb("ident_f", [128, 128], F32)
    ident_b = sb("ident_b", [128, 128], BF16)
    u_f = sb("u_f", [128, 128], F32)
    ones_f = sb("ones_f", [128, 128], F32)
    zeros_b = sb("zeros_b", [128, 512], BF16)
    tri_b = sb("tri_b", [128, 256], BF16)
    cmpmask_b = sb("cmpmask_b", [128, T], BF16)
    fbt = sb("fbt_s", [128, 16, 32], F32)
    gpre1 = sb("gpre1_s", [128, 8], F32)
    gpre2 = sb("gpre2_s", [128, 8], F32)
    bfor = sb("bfor_s", [128, 8], F32)
    onescol = sb("onescol", [128, 1], F32)
    w1k = sb("w1k_s", [128, 32, 64], BF16)
    w1v = sb("w1v_s", [128, 32, 64], BF16)
    w2k = sb("w2k_s", [64, 64], BF16)
    w2v = sb("w2v_s", [64, 64], BF16)
    posk = sb("posk_s", [64, 32], BF16)
    posv = sb("posv_s", [64, 32], BF16)
    cbk = sb("cbk", [64, 1], F32)
    cbv = sb("cbv", [64, 1], F32)
    vcmp = sb("vcmp", [128, 2, 97], BF16)
    kcmp = sb("kcmp", [96, 2, 128], BF16)
    sTk = sb("sTk", [64, 2, 128], BF16)
    sTv = sb("sTv", [64, 2, 128], BF16)
    mbw = sb("mbw", [128, 2, 4, 96], BF16)
    mbT = sb("mbT", [96, 2, 512], BF16)
    const_b = Buf("consts")

    lp = sb("lp", [128, 16, 8], F32)
    lsum = sb("lsum", [128, 17, 8], F32)
    negc = sb("negc", [128, 16, 8], F32)
    caugT = sb("caugT", [33, T], BF16)
    gates = sb("gates", [128, 16, 24], F32)
    psl = sb("psl", [128, 4, 32], F32)

    xt_ring = Ring([(sb("xt%d" % i, [128, 1024], F32), Buf("xt%d" % i)) for i in range(2)])
    t1_ring = Ring([(sb("t1_%d" % i, [128, 512], F32), Buf("t1_%d" % i)) for i in range(3)])
    hn_ring = Ring([(sb("hn%d" % i, [128, 1024], BF16), Buf("hn%d" % i)) for i in range(2)])
    junk_b = Buf("junk")
    pt_ring = Ring([(sb("pt%d" % i, [128, 512], BF16), Buf("pt%d" % i)) for i in range(4)])
    wt_ring = Ring([(sb("wt%d" % i, [128, 2048], BF16), Buf("wt%d" % i)) for i in range(4)])
    rope_ring = Ring([(sb("rp%d" % i, [128, 1024], F32), Buf("rp%d" % i)) for i in range(1)])
    sm_ring = Ring([(sb("sm%d" % i, [128, 64], F32), Buf("sm%d" % i)) for i in range(8)])

    pj_ring = Ring([(ps("pj%d" % i, [128, 512], F32), Buf("pj%d" % i)) for i in range(2)])
    st_ring = Ring([(ps("st%d" % i, [128, 512], F32), Buf("st%d" % i)) for i in range(2)])
    sa_ring = Ring([st_ring.items[0], pj_ring.items[0], st_ring.items[1], pj_ring.items[1]])
    pj4_ring = Ring([pj_ring.items[0], st_ring.items[0], pj_ring.items[1], st_ring.items[1]])
    oa4_items = None
    oa_ring = Ring([(ps("oa%d" % i, [128, 512], F32), Buf("oa%d" % i)) for i in range(2)])
    tp_ring = Ring([(ps("tp%d" % i, [128, 512], F32), Buf("tp%d" % i)) for i in range(2)])
    y_ring = Ring([st_ring.items[0], oa_ring.items[0], st_ring.items[1], oa_ring.items[1]])
    oa4_ring = Ring([oa_ring.items[0], tp_ring.items[0], oa_ring.items[1], tp_ring.items[1]])

    def mm(out, lhsT, rhs, start, stop, reads, writes):
        P.add("pe", "matmul", dict(out=out, lhsT=lhsT, rhs=rhs, start=start, stop=stop,
                                   skip_group_check=True), reads, writes)

    def tr(out, in_, ident, reads, writes):
        P.add("pe", "transpose", dict(out=out, in_=in_, identity=ident), reads, writes)

    def act(out, in_, func, reads, writes, **kw):
        P.add("act", "activation", dict(out=out, in_=in_, func=func, **kw), reads, writes)

    def dma(q, out, in_, reads, writes, **kw):
        extra = {}
        for k in ("prefetch", "final"):
            if k in kw:
                extra[k] = kw.pop(k)
        return P.add(q, "dma_start", dict(out=out, in_=in_, **kw), reads, writes, dma=True, **extra)

    def tt(eng, out, in0, in1, op, reads, writes):
        P.add(eng, "tensor_tensor", dict(out=out, in0=in0, in1=in1, op=op), reads, writes)

    def ts(eng, out, in0, s1, s2, op0, op1, reads, writes):
        kw = dict(out=out, in0=in0, scalar1=s1, scalar2=s2, op0=op0)
        if op1 is not None:
            kw["op1"] = op1
        P.add(eng, "tensor_scalar", kw, reads, writes)

    def stt(out, in0, scalar, in1, op0, op1, reads, writes):
        P.add("dve", "scalar_tensor_tensor", dict(out=out, in0=in0, scalar=scalar, in1=in1,
                                                  op0=op0, op1=op1), reads, writes)

    def cp(eng, out, in_, reads, writes):
        if eng == "act":
            P.add("act", "activation", dict(out=out, in_=in_, func=AF.Copy), reads, writes)
        else:
            P.add(eng, "tensor_copy", dict(out=out, in_=in_), reads, writes)

    def memset(eng, ap, val, writes):
        P.add(eng, "memset", dict(ap=ap, constant=val), (), writes)

    dma("sp", ident_f[:], ident_d[:, :], (), [const_b])
    dma("pool", ident_b[:], ident_d[:, :], (), [const_b])
    dma("sp", u_f[:], u_d[:, :], (), [const_b])
    dma("pool", tri_b[:], tri_d[:, :], (), [const_b])
    dma("pool", cmpmask_b[:], cmpmask_d[:, :], (), [const_b])
    dma("sp", fbt[:].rearrange("p a b -> p (a b)"), fbt_d[:, :], (), [const_b])
    dma("sp", gpre1[:], gpre1_d[:, :], (), [const_b])
    dma("sp", gpre2[:], gpre2_d[:, :], (), [const_b])
    dma("sp", bfor[:], bfor_d.partition_broadcast(128), (), [const_b])
    dma("pool", w1k[:].rearrange("p a b -> p (a b)"), w1k_d[:, :], (), [const_b])
    dma("pool", w1v[:].rearrange("p a b -> p (a b)"), w1v_d[:, :], (), [const_b])
    dma("pool", w2k[:], w2k_d[:, :], (), [const_b])
    dma("pool", w2v[:], w2v_d[:, :], (), [const_b])
    dma("pool", posk[:], posk_d[:, :], (), [const_b])
    dma("pool", posv[:], posv_d[:, :], (), [const_b])
    memset("pool", ones_f[:], 1.0, [const_b])
    memset("pool", zeros_b[:], 0.0, [const_b])
    memset("pool", onescol[:], 1.0, [const_b])
    memset("pool", caugT[32:33, :], 1.0, [const_b])
    memset("pool", vcmp[:].rearrange("p a b -> p (a b)"), 1.0, [const_b])
    for g in range(2):
        dma("pool", vcmp[:, g, 65:97], ov_d[:, :], (), [const_b])
    memset("pool", sTk[:].rearrange("p a b -> p (a b)"), 0.0, [const_b])
    memset("pool", sTv[:].rearrange("p a b -> p (a b)"), 0.0, [const_b])
    memset("pool", mbw[:].rearrange("p g a b -> p (g a b)"), 0.0, [const_b])
    memset("pool", lsum[:, 0, :], 0.0, [const_b])
    memset("pool", kcmp[64:96, :, :].rearrange("p a b -> p (a b)"), 0.0, [const_b])
    for (w1, pos, cb) in ((w1k, posk, cbk), (w1v, posv, cbv)):
        bank, bb = tp_ring.next()
        for l in range(32):
            mm(bank[0:64, 0:1], w1[0:64, l, :], pos[0:64, l:l + 1], l == 0, l == 31, [const_b], [bb])
        cp("dve", cb[:], bank[0:64, 0:1], [bb], [const_b])

    def aview(off, n, dt=BF16):
        v = arena[:, off:off + n]
        if dt == F32:
            v = v.bitcast(F32)
        return v

    junk = aview(49152, 1024)
    yt_ring = Ring([(aview(40960 + i * 2048, 2048, F32), Buf("yt%d" % i)) for i in range(2)])
    gpost1 = aview(45056, 2048, F32)
    gpost2 = aview(47104, 2048, F32)
    gpost_b = Buf("gpost")

    def norm_to_hT(src, src_b, tt_i, gpre):
        sm, smb = sm_ring.next()
        act(junk[:], src, AF.Square, [src_b], [junk_b, smb], accum_out=sm[:, 0:1])
        ts("dve", sm[:, 1:2], sm[:, 0:1], 1.0 / D, 1e-6, ALU.mult, ALU.add, [smb], [smb])
        act(sm[:, 2:3], sm[:, 1:2], AF.Ln, [smb], [smb])
        act(sm[:, 3:4], sm[:, 2:3], AF.Exp, [smb], [smb], scale=-0.5)
        hn, hnb = hn_ring.next()
        ts("dve", hn[:], src, sm[:, 3:4], None, ALU.mult, None, [src_b, smb], [hnb])
        bank, bb = tp_ring.next()
        bk = bank[:].bitcast(BF16)
        for kc in range(8):
            tr(bk[:, kc * 128:(kc + 1) * 128], hn[:, kc * 128:(kc + 1) * 128], ident_b[:], [hnb, const_b], [bb])
        return (bk, bb, tt_i, gpre)

    def norm_B(st):
        bk, bb, tt_i, gpre = st
        c = tt_i // 4
        for kc in range(8):
            act(hT[:, kc, tt_i * 128:(tt_i + 1) * 128], bk[:, kc * 128:(kc + 1) * 128], AF.Copy,
                [bb, const_b], [hT_b[kc][c]], scale=gpre[:, kc:kc + 1])

    def load_w(q, src2d, ncols_elems, prefetch=True):
        wt, wb = wt_ring.next()
        dma(q, wt[:, 0:ncols_elems], src2d, (), [wb], prefetch=prefetch)
        return wt, wb

    pipe = []
    HEAT = 0

    def noop_():
        return None

    def attn_flush():
        n = len(pipe)
        if n == 0:
            return
        LA = 3
        for j in range(min(LA, n)):
            pipe[j]["S"]()
        for i, t in enumerate(pipe):
            t["E"]()
            if i + LA < n:
                pipe[i + LA]["S"]()
            if t["pre"] is not None:
                t["pre"]()
            t["PV"]()
            if HEAT and t["S"] is not noop_:
                hb, hbb = tp_ring.items[1]
                mm(hb[:, 0:HEAT], zeros_b[:, 0:128], zeros_b[:, 0:HEAT], True, True, [const_b], [hbb])
            if t["post"] is not None:
                t["post"]()
        del pipe[:]

    def attn_chunk(qc, qtile, q_b, ktile, k_b, rows, tiles, vget, vw, v_b, bias_get, finalize):
        obank, ob = oa4_ring.next()
        o3 = obank[:, 0:4 * vw].rearrange("p (a b) -> p a b", b=vw)
        last = {}
        for i, (kt, c0, c1, mask) in enumerate(tiles):
            for qs in range(c0 // 128, c1 // 128):
                last[qs] = i
        ntl = len(tiles)
        for i, (kt, c0, c1, mask) in enumerate(tiles):
            sbank, sbb = sa_ring.next()
            pt, ptb = pt_ring.next()

            def fS(kt=kt, c0=c0, c1=c1, mask=mask, sbank=sbank, sbb=sbb):
                mm(sbank[:, c0:c1], ktile[rows, kt * 128:(kt + 1) * 128],
                   qtile[rows, qc * 512 + c0:qc * 512 + c1], True, mask is None, [q_b, k_b], [sbb])
                if mask is not None:
                    map_, m0, mw = mask
                    mm(sbank[:, m0:m0 + mw], ident_b[:], map_, False, True, [const_b], [sbb])

            def fE(kt=kt, c0=c0, c1=c1, sbank=sbank, sbb=sbb, pt=pt, ptb=ptb):
                kw = dict(scale=0.125)
                rd = [sbb]
                if bias_get is not None:
                    bap, bbuf = bias_get(kt)
                    kw["bias"] = bap
                    rd.append(bbuf)
                act(pt[:, c0:c1], sbank[:, c0:c1], AF.Exp, rd, [ptb], **kw)

            def fPV(i=i, kt=kt, c0=c0, c1=c1, pt=pt, ptb=ptb):
                for qs in range(c0 // 128, c1 // 128):
                    mm(o3[:, qs, :], pt[:, qs * 128:(qs + 1) * 128], vget(kt), False, last[qs] == i,
                       [ptb, v_b], [ob])

            pre = None
            post = None
            if i == 0:
                def pre():
                    mm(obank[:, :], zeros_b[:, 0:128], zeros_b[:, 0:512], True, False, [const_b], [ob])
            if i == ntl - 1:
                def post():
                    finalize(qc, o3, ob)
            pipe.append(dict(S=fS, E=fE, PV=fPV, pre=pre, post=post))

    def causal_tiles(qc):
        tl = [(kt, 0, 512, None) for kt in range(4 * qc)]
        for j in range(4):
            tl.append((4 * qc + j, 128 * j, 512, (tri_b[:, 0:128], 128 * j, 128)))
        return tl

    def window_tiles(qc):
        tl = []
        for j in range(-4, 4):
            kt = 4 * qc + j
            if kt < 0:
                continue
            if j >= 0:
                tl.append((kt, 128 * j, 512, (tri_b[:, 0:128], 128 * j, 128)))
            else:
                jj = j + 4
                tl.append((kt, 0, 128 * jj + 128, (tri_b[:, 128:256], 128 * jj, 128)))
        return tl

    for s in range(nseq):
        xs = x_d[s * T:(s + 1) * T, :]
        outs = out_d[s * T:(s + 1) * T, :]
        outD_b = [Buf("outD%d_%d" % (s, i)) for i in range(NT)]


        VF = aview(0, 8320).rearrange("p (a b c) -> p a b c", a=16, b=8)
        vf_b = Buf("VF")
        QK = [[aview(8320 + (st * 4 + i) * 2048, 2048) for i in range(4)] for st in range(2)]
        QK_b = [[Buf("QK%d_%d" % (st, i)) for i in range(4)] for st in range(2)]
        fox_tm = aview(8320 + 16384, 8192).rearrange("p (a b) -> p a b", b=512)
        fox_b = [Buf("fox%d" % i) for i in range(NT)]
        FOXT_OFF = 32896
        foxT = aview(FOXT_OFF, 8192).rearrange("p (a b) -> p a b", b=T)
        foxT_b = Buf("foxT")
        nsaT = aview(FOXT_OFF + 8192, 8192).rearrange("p (a b) -> p a b", b=T)
        nsaT_b = Buf("nsaT")

        wfvh = [load_w("pool", w_fv[hf, :, :], 2048) for hf in range(2)]
        memset("pool", VF[:, :, :, 64:65], 1.0, [vf_b])
        wg, wg_b = load_w("pool", w_ffng[:, :], 256)
        wg3 = wg[:, 0:256].rearrange("p (a b) -> p a b", b=32)
        wfv3 = [w[:, 0:2048].rearrange("p (a b) -> p a b", b=256) for (w, _) in wfvh]
        def tile_proj(ti):
            c = ti // 4
            for hf in range(2):
                bank, bb = pj4_ring.next()
                for kc in range(8):
                    mm(bank[:, 0:256], hT[:, kc, ti * 128:(ti + 1) * 128], wfv3[hf][:, kc, :], kc == 0, kc == 7,
                       [hT_b[kc][c], wfvh[hf][1]], [bb])
                cp("dve", VF[:, ti, 4 * hf:4 * hf + 4, 0:64], bank[:, 0:256].rearrange("p (a b) -> p a b", b=64),
                   [bb], [vf_b])
            bank2, bb2 = oa_ring.next()
            for kc in range(8):
                mm(bank2[:, 0:32], hT[:, kc, ti * 128:(ti + 1) * 128], wg3[:, kc, :], kc == 0, kc == 7,
                   [hT_b[kc][c], wg_b], [bb2])
            sm, smb = sm_ring.next()
            tt("dve", sm[:, 0:8], bank2[:, 0:8], bfor[:], ALU.add, [bb2, const_b], [smb])
            act(sm[:, 8:16], sm[:, 0:8], AF.Exp, [smb], [smb], scale=-1.0)
            act(lp[:, ti, :], sm[:, 8:16], AF.Ln, [smb, const_b], [const_b], bias=onescol[:])
            tt("dve", lsum[:, ti + 1, :], lsum[:, ti, :], lp[:, ti, :], ALU.add, [const_b], [const_b])
            act(sm[:, 16:40], bank2[:, 8:32], AF.Exp, [bb2], [smb], scale=-1.0)
            ts("dve", sm[:, 16:40], sm[:, 16:40], 1.0, None, ALU.add, None, [smb], [smb])
            P.add("dve", "reciprocal", dict(out=gates[:, ti, :], in_=sm[:, 16:40]), [smb], [const_b])

        pend = None
        for ti in range(NT):
            xt, xb = xt_ring.next()
            dma("sp", xt[:], xs[ti * 128:(ti + 1) * 128, :], (), [xb])
            stA = norm_to_hT(xt[:], xb, ti, gpre1)
            if pend is not None:
                norm_B(pend)
                tile_proj(pend[2])
            pend = stA
        norm_B(pend)
        tile_proj(pend[2])
        bank, bb = tp_ring.next()
        for ti in range(NT):
            mm(bank[:, ti * 8:(ti + 1) * 8], u_f[:], lp[:, ti, :], True, False, [const_b], [bb])
            mm(bank[:, ti * 8:(ti + 1) * 8], ones_f[:], lsum[:, ti, :], False, True, [const_b], [bb])
        cp("dve", negc[:].rearrange("p a b -> p (a b)"), bank[:, 0:128], [bb], [const_b])
        for c in range(4):
            bank, bb = tp_ring.next()
            for j in range(4):
                ti = 4 * c + j
                mm(bank[0:8, j * 128:(j + 1) * 128], lp[:, ti, :], u_f[:], True, False, [const_b], [bb])
                mm(bank[0:8, j * 128:(j + 1) * 128], lsum[:, ti, :], ones_f[:], False, True, [const_b], [bb])
            act(caugT[0:8, c * 512:(c + 1) * 512], bank[0:8, :], AF.Copy, [bb], [const_b], scale=-8.0)

        for p in range(4):
            st = p % 2
            QA, KA, QB, KB = QK[st]
            QA_b, KA_b, QB_b, KB_b = QK_b[st]
            dma("sp", QA[64:65, :], caugT[2 * p:2 * p + 1, :], [const_b], [QA_b])
            dma("sp", QB[64:65, :], caugT[2 * p + 1:2 * p + 2, :], [const_b], [QB_b])
            dma("sp", KA[64:65, :], caugT[32:33, :], [const_b], [KA_b])
            dma("sp", KB[64:65, :], caugT[32:33, :], [const_b], [KB_b])
            for (blk, TA, TA_b, TB, TB_b) in ((p, QA, QA_b, QB, QB_b), (4 + p, KA, KA_b, KB, KB_b)):
                wt, wb = load_w("pool", w_fqk[blk, :, :], 1024)
                w3 = wt[:, 0:1024].rearrange("p (a b) -> p a b", b=128)
                for c in range(4):
                    bank, bb = pj4_ring.next()
                    for kc in range(8):
                        mm(bank[:, :], w3[:, kc, :], hT[:, kc, c * 512:(c + 1) * 512], kc == 0, kc == 7,
                           [wb, hT_b[kc][c]], [bb])
                    cp("dve", TA[0:64, c * 512:(c + 1) * 512], bank[0:64, :], [bb], [TA_b])
                    cp("act", TB[0:64, c * 512:(c + 1) * 512], bank[64:128, :], [bb], [TB_b])
            for hh in range(2):
                h = 2 * p + hh
                qtile, q_b, ktile, k_b = (QA, QA_b, KA, KA_b) if hh == 0 else (QB, QB_b, KB, KB_b)
                rows = slice(0, 65)

                def fin(qc, o3, ob, h=h):
                    sm, smb = sm_ring.next()
                    P.add("dve", "reciprocal", dict(out=sm[:, 0:4], in_=o3[:, :, 64]), [ob], [smb])
                    tt("dve", fox_tm[:, qc * 4:qc * 4 + 4, h * 64:(h + 1) * 64], o3[:, :, 0:64],
                       sm[:, 0:4].unsqueeze(2).broadcast_to([128, 4, 64]), ALU.mult, [ob, smb],
                       fox_b[qc * 4:qc * 4 + 4])

                for qc in range(4):
                    attn_chunk(qc, qtile, q_b, ktile, k_b, rows, causal_tiles(qc),
                               lambda kt, h=h: VF[:, kt, h, :], 65, vf_b,
                               lambda kt, h=h: (negc[:, kt, h:h + 1], const_b), fin)
            attn_flush()
        for ti in range(NT):
            bank, bb = tp_ring.next()
            bk = bank[:].bitcast(BF16)
            for kc in range(4):
                tr(bk[:, kc * 128:(kc + 1) * 128], fox_tm[:, ti, kc * 128:(kc + 1) * 128], ident_b[:],
                   [fox_b[ti], const_b], [bb])
            cp("dve", foxT[:, :, ti * 128:(ti + 1) * 128],
               bk[:, 0:512].rearrange("p (a b) -> p a b", b=128), [bb], [foxT_b])
        if dbg and s == 0:
            for ti in range(NT):
                yt, yb = xt_ring.next()
                cp("dve", yt[:, 0:512], fox_tm[:, ti, :], [fox_b[ti]], [yb])
                dma("sp", dbg_d["d_fox"][:, ti * 512:(ti + 1) * 512], yt[:, 0:512], [yb], [], final=True)
            dma("sp", dbg_d["d_negc"][:, :], negc[:].rearrange("p a b -> p (a b)"), [const_b], [], final=True)
        P.barrier()

        QS = [[aview((g * 4 + p) * 2048, 2048) for p in range(4)] for g in range(2)]
        QS_b = [[[Buf("QS%d_%d_%d" % (g, p, c)) for c in range(4)] for p in range(4)] for g in range(2)]
        KS = [aview(16384 + g * 2048, 2048) for g in range(2)]
        KS_b = [Buf("KS%d" % g) for g in range(2)]
        KW = [aview(16384 + 4096 + g * 2048, 2048) for g in range(2)]
        KW_b = [Buf("KW%d" % g) for g in range(2)]
        KC = aview(16384 + 8192, 2048)
        KC_b = Buf("KC")
        VC = aview(16384 + 10240, 2048)
        VC_b = Buf("VC")
        VSW = aview(28672, 4160).rearrange("p (a b c) -> p a b c", a=16, b=4)
        vsw_b = Buf("VSW")
        nsa_acc = aview(16384 + 8192, 4096, F32).rearrange("p (a b) -> p a b", b=512)
        nacc_b = [Buf("nacc%d" % i) for i in range(4)]

        memset("pool", VSW[:, :, :, 64:65], 1.0, [vsw_b])
        engs = ["pool", "dve", "pool", "dve", "pool"]
        for g in range(2):
            memset(engs[0], KW[g][64:96, :], 0.0, [KW_b[g]])
            for p in range(4):
                memset(engs[1 + p], QS[g][p][64:96, :], 0.0, QS_b[g][p])
        dma("pool", KS[0][64:96, :], erows_d[:, :], (), [KS_b[0]])
        dma("pool", KS[1][64:96, :], erows_d[:, :], (), [KS_b[1]])
        wv, wv_b = load_w("pool", w_nv[:, :], 2048)
        wv3 = wv[:, 0:2048].rearrange("p (a b) -> p a b", b=256)
        for ti in range(NT):
            c = ti // 4
            bank, bb = pj4_ring.next()
            for kc in range(8):
                mm(bank[:, 0:256], hT[:, kc, ti * 128:(ti + 1) * 128], wv3[:, kc, :], kc == 0, kc == 7,
                   [hT_b[kc][c], wv_b], [bb])
            cp("dve", VSW[:, ti, :, 0:64], bank[:, 0:256].rearrange("p (a b) -> p a b", b=64), [bb], [vsw_b])

        def proj_fm(blk_plain, blk_swap, wsrc, dests):
            wp, wpb = load_w("pool", wsrc[blk_plain, :, :], 1024)
            wp3 = wp[:, 0:1024].rearrange("p (a b) -> p a b", b=128)
            if blk_swap is not None:
                wq, wqb = load_w("pool", wsrc[blk_swap, :, :], 1024)
                wq3 = wq[:, 0:1024].rearrange("p (a b) -> p a b", b=128)
            for c in range(4):
                b1, bb1 = pj4_ring.next()
                for kc in range(8):
                    mm(b1[:, :], wp3[:, kc, :], hT[:, kc, c * 512:(c + 1) * 512], kc == 0, kc == 7,
                       [wpb, hT_b[kc][c]], [bb1])
                dests_c = [(t_, (b_[c] if isinstance(b_, list) else b_), r1_, r2_) for (t_, b_, r1_, r2_) in dests]
                if blk_swap is None:
                    for (tile_, tb_, rs, rd_) in dests_c:
                        cp("act", tile_[rd_, c * 512:(c + 1) * 512], b1[rs, :], [bb1], [tb_])
                    continue
                b2, bb2 = pj4_ring.next()
                for kc in range(8):
                    mm(b2[:, :], wq3[:, kc, :], hT[:, kc, c * 512:(c + 1) * 512], kc == 0, kc == 7,
                       [wqb, hT_b[kc][c]], [bb2])
                rp, rpb = rope_ring.next()
                dma("sp", rp[:], rope_d[c, :, :], (), [rpb])
                ta, tab = t1_ring.next()
                tb, tbb = t1_ring.next()
                tt("dve", ta[:], b1[:, :], rp[:, 0:512], ALU.mult, [bb1, rpb], [tab])
                tt("dve", tb[:], b2[:, :], rp[:, 512:1024], ALU.mult, [bb2, rpb], [tbb])
                for (tile_, tb_, rs, rd_) in dests_c:
                    if rs == rd_:
                        tt("dve" if len(dests_c) > 1 else "pool", tile_[rd_, c * 512:(c + 1) * 512], ta[rs, :], tb[rs, :],
                           ALU.add, [tab, tbb], [tb_])
                    else:
                        tt("pool", ta[rs, :], ta[rs, :], tb[rs, :], ALU.add, [tab, tbb], [tab])
                        cp("act", tile_[rd_, c * 512:(c + 1) * 512], ta[rs, :], [tab], [tb_])

        for p in range(4):
            proj_fm(p, 4 + p, w_nq, [(QS[0][p], QS_b[0][p], slice(0, 64), slice(0, 64)),
                                     (QS[1][p], QS_b[1][p], slice(64, 128), slice(0, 64))])
        proj_fm(0, 1, w_nk, [(KC, KC_b, slice(0, 128), slice(0, 128))])
        proj_fm(2, 3, w_nk, [(KS[0], KS_b[0], slice(0, 64), slice(0, 64)), (KS[1], KS_b[1], slice(64, 128), slice(0, 64))])
        proj_fm(4, 5, w_nk, [(KW[0], KW_b[0], slice(0, 64), slice(0, 64)), (KW[1], KW_b[1], slice(64, 128), slice(0, 64))])
        proj_fm(6, None, w_nk, [(VC, VC_b, slice(0, 128), slice(0, 128))])

        for (src, src_b, w1, cb, sT) in ((KC, KC_b, w1k, cbk, sTk), (VC, VC_b, w1v, cbv, sTv)):
            for g in range(2):
                rs = slice(64 * g, 64 * g + 64)
                bank, bb = tp_ring.next()
                for l in range(32):
                    mm(bank[0:64, 0:127], w1[rs, l, :], src[rs, l:l + 2017:16], l == 0, l == 31,
                       [const_b, src_b], [bb])
                act(sT[:, g, 0:127], bank[0:64, 0:127], AF.Silu, [bb, const_b], [const_b], bias=cb[:])
        for g in range(2):
            bank, bb = tp_ring.next()
            mm(bank[0:64, 0:128], w2k[:], sTk[:, g, :], True, True, [const_b], [bb])
            cp("dve", kcmp[0:64, g, :], bank[0:64, 0:128], [bb], [const_b])
        for g in range(2):
            bank, bb = tp_ring.next()
            mm(bank[:, 0:64], sTv[:, g, :], w2v[:], True, True, [const_b], [bb])
            cp("dve", vcmp[:, g, 0:64], bank[:, 0:64], [bb], [const_b])

        P.barrier()
        psl_b = Buf("psl")
        mbw_b = Buf("mbw")
        mbT_b = Buf("mbT")
        ocmp = [xt_ring.items[k][0][:, :].rearrange("p (a b) -> p a b", b=512) for k in range(2)]
        ocmp_b = [xt_ring.items[k][1] for k in range(2)]

        def bc(ap2, n):
            return ap2.unsqueeze(2).broadcast_to([128, ap2.shape[1], n])

        noop = noop_

        def add_cmp_sel(qn):
            for g in range(2):
                for p in range(4):
                    h = 4 * g + p

                    def fin_cmp(qc, o3, ob, h=h, p=p):
                        sm, smb = sm_ring.next()
                        hs = slice(h * 64, (h + 1) * 64)
                        ts("dve", sm[:, 0:4], o3[:, :, 64], 1e-30, None, ALU.max, None, [ob], [smb])
                        P.add("dve", "reciprocal", dict(out=sm[:, 4:8], in_=sm[:, 0:4]), [smb], [smb])
                        tt("dve", sm[:, 8:12], sm[:, 4:8], gates[:, qc * 4:qc * 4 + 4, 3 * h], ALU.mult,
                           [smb, const_b], [smb])
                        for k in range(2):
                            tt("dve", ocmp[k][:, :, hs], o3[:, 2 * k:2 * k + 2, 0:64], bc(sm[:, 8 + 2 * k:10 + 2 * k], 64),
                               ALU.mult, [ob, smb], [ocmp_b[k]])
                        if p == 0:
                            tt("dve", psl[:, :, :], o3[:, :, 65:97], bc(sm[:, 4:8], 32), ALU.mult, [ob, smb], [psl_b])
                        else:
                            tmp, tmpb = t1_ring.next()
                            tmp3 = tmp[:, 0:128].rearrange("p (a b) -> p a b", b=32)
                            tt("dve", tmp3, o3[:, :, 65:97], bc(sm[:, 4:8], 32), ALU.mult, [ob, smb], [tmpb])
                            tt("dve", psl[:, :, :], psl[:, :, :], tmp3, ALU.add, [psl_b, tmpb], [psl_b])

                    attn_chunk(qn, QS[g][p], QS_b[g][p][qn], kcmp[:, g, :], const_b, slice(0, 96),
                               [(0, 0, 512, (cmpmask_b[:, qn * 512:(qn + 1) * 512], 0, 512))],
                               lambda kt, g=g: vcmp[:, g, :], 97, const_b, None, fin_cmp)

                def selection(g=g):
                    sc, scb = t1_ring.next()
                    sc3 = sc[:, 0:128].rearrange("p (a b) -> p a b", b=32)
                    sm, smb = sm_ring.next()
                    tt("dve", sc3, psl[:, :, :], fbt[:, qn * 4:qn * 4 + 4, :], ALU.add, [psl_b, const_b], [scb])
                    for qs in range(4):
                        P.add("dve", "max", dict(out=sm[:, 8 * qs:8 * qs + 8], in_=sc3[:, qs, :]), [scb], [smb])
                    tt("dve", sc3, sc3, bc(sm[:, 7:32:8], 32), ALU.is_lt, [scb, smb], [scb])
                    ts("dve", mbw[:, g, :, 64:96], sc3, NEGM, None, ALU.mult, None, [scb], [mbw_b])
                    bank, bb = tp_ring.next()
                    bk = bank[:].bitcast(BF16)
                    for qs in range(4):
                        tr(bk[0:96, qs * 128:(qs + 1) * 128], mbw[:, g, qs, :], ident_b[:], [mbw_b, const_b], [bb])
                    cp("dve", mbT[:, g, :], bk[0:96, 0:512], [bb], [mbT_b])
                    for p in range(4):
                        cp("pool", QS[g][p][64:96, qn * 512:(qn + 1) * 512], mbT[64:96, g, :], [mbT_b],
                           [QS_b[g][p][qn]])

                pipe.append(dict(S=noop, E=noop, PV=noop, pre=None, post=selection))

        def add_sel_win(qc, which):
            for g in range(2):
                for p in range(4):
                    h = 4 * g + p

                    def fin_acc(br, h=h):
                        def f(qc, o3, ob):
                            sm, smb = sm_ring.next()
                            hs = slice(h * 64, (h + 1) * 64)
                            P.add("dve", "reciprocal", dict(out=sm[:, 0:4], in_=o3[:, :, 64]), [ob], [smb])
                            tt("dve", sm[:, 4:8], sm[:, 0:4], gates[:, qc * 4:qc * 4 + 4, 3 * h + br], ALU.mult,
                               [smb, const_b], [smb])
                            tmp, tmpb = t1_ring.next()
                            tmp3 = tmp[:, 0:256].rearrange("p (a b) -> p a b", b=64)
                            tt("dve", tmp3, o3[:, :, 0:64], bc(sm[:, 4:8], 64), ALU.mult, [ob, smb], [tmpb])
                            if br == 1:
                                for k in range(2):
                                    tt("dve", nsa_acc[:, 2 * k:2 * k + 2, hs], tmp3[:, 2 * k:2 * k + 2, :],
                                       ocmp[k][:, :, hs], ALU.add, [tmpb, ocmp_b[k]], nacc_b[2 * k:2 * k + 2])
                            else:
                                tt("dve", nsa_acc[:, :, hs], nsa_acc[:, :, hs], tmp3, ALU.add, [tmpb] + nacc_b, nacc_b)
                        return f

                    if which == 1:
                        attn_chunk(qc, QS[g][p], QS_b[g][p][qc], KS[g], KS_b[g], slice(0, 96), causal_tiles(qc),
                                   lambda kt, g=g: VSW[:, kt, g, :], 65, vsw_b, None, fin_acc(1))
                    else:
                        attn_chunk(qc, QS[g][p], QS_b[g][p][qc], KW[g], KW_b[g], slice(0, 96), window_tiles(qc),
                                   lambda kt, g=g: VSW[:, kt, 2 + g, :], 65, vsw_b, None, fin_acc(2))

        add_cmp_sel(0)
        attn_flush()
        for qc in range(4):
            add_sel_win(qc, 1)
            if qc + 1 < 4:
                add_cmp_sel(qc + 1)
            add_sel_win(qc, 2)
            attn_flush()
            for qs in range(4):
                ti = qc * 4 + qs
                hn, hnb = hn_ring.next()
                cp("pool", hn[:, 0:512], nsa_acc[:, qs, :], [nacc_b[qs]], [hnb])
                if dbg and s == 0:
                    dma("sp", dbg_d["d_nsa"][:, ti * 512:(ti + 1) * 512], nsa_acc[:, qs, :], [nacc_b[qs]], [],
                        final=True)
                bank, bb = tp_ring.next()
                bk = bank[:].bitcast(BF16)
                for kc in range(4):
                    tr(bk[:, kc * 128:(kc + 1) * 128], hn[:, kc * 128:(kc + 1) * 128], ident_b[:],
                       [hnb, const_b], [bb])
                cp("dve", nsaT[:, :, ti * 128:(ti + 1) * 128],
                   bk[:, 0:512].rearrange("p (a b) -> p a b", b=128), [bb], [nsaT_b])
        P.barrier()

        mixT = aview(0, 16384).rearrange("p (a b) -> p a b", b=T)
        mixT_b = [[Buf("mix%d_%d" % (c, t4)) for t4 in range(4)] for c in range(8)]
        for cb_ in range(8):
            wga, wga_b = load_w("pool", w_gab[cb_, :, :], 1024)
            wgb, wgb_b = load_w("pool", w_gab[8 + cb_, :, :], 1024)
            wfo, wfo_b = load_w("pool", w_fo[cb_, :, :], 512)
            wno, wno_b = load_w("pool", w_no[cb_, :, :], 512)
            wga3 = wga[:, 0:1024].rearrange("p (a b) -> p a b", b=128)
            wgb3 = wgb[:, 0:1024].rearrange("p (a b) -> p a b", b=128)
            wfo3 = wfo[:, 0:512].rearrange("p (a b) -> p a b", b=128)
            wno3 = wno[:, 0:512].rearrange("p (a b) -> p a b", b=128)
            for c in range(4):
                cs = slice(c * 512, (c + 1) * 512)
                res = []
                for (wg3_, wgb_, wo3_, wob_, srcT, srcb) in ((wga3, wga_b, wfo3, wfo_b, foxT, foxT_b),
                                                           (wgb3, wgb_b, wno3, wno_b, nsaT, nsaT_b)):
                    b1, bb1 = pj4_ring.next()
                    for kc in range(8):
                        mm(b1[:, :], wg3_[:, kc, :], hT[:, kc, cs], kc == 0, kc == 7, [wgb_, hT_b[kc][c]], [bb1])
                    sg, sgb = t1_ring.next()
                    act(sg[:], b1[:, :], AF.Sigmoid, [bb1], [sgb])
                    b2, bb2 = pj4_ring.next()
                    for kc in range(4):
                        mm(b2[:, :], wo3_[:, kc, :], srcT[:, kc, cs], kc == 0, kc == 3, [wob_, srcb], [bb2])
                    tt("dve", sg[:], sg[:], b2[:, :], ALU.mult, [sgb, bb2], [sgb])
                    res.append((sg, sgb))
                tt("pool", mixT[:, cb_, cs], res[0][0][:], res[1][0][:], ALU.add, [res[0][1], res[1][1]],
                   [mixT_b[cb_][c]])
        P.barrier()

        dma("sp", gpost1[:], gpost1_d.partition_broadcast(128), (), [gpost_b])
        WO = aview(16384, 8192).rearrange("p (h a b) -> p h a b", h=2, a=8)
        wo_b = Buf("WO")
        for hf in range(2):
            dma("pool", WO[:, hf, :, :].rearrange("p a b -> p (a b)"), w_o[hf, :, :], (), [wo_b])
        pend = None
        for ti in range(NT):
            c = ti // 4
            yt, yb = yt_ring.next()
            for hf in range(2):
                bank, bb = pj4_ring.next()
                for kc in range(8):
                    mm(bank[:, :], mixT[:, kc, ti * 128:(ti + 1) * 128], WO[:, hf, kc, :], kc == 0, kc == 7,
                       [mixT_b[kc][c], wo_b], [bb])
                cp("act" if hf == 0 else "dve", yt[:, hf * 512:(hf + 1) * 512], bank[:, :], [bb], [yb])
            sm, smb = sm_ring.next()
            act(junk[:], yt[:], AF.Square, [yb], [junk_b, smb], accum_out=sm[:, 0:1])
            ts("dve", sm[:, 1:2], sm[:, 0:1], 1.0 / D, 1e-6, ALU.mult, ALU.add, [smb], [smb])
            act(sm[:, 2:3], sm[:, 1:2], AF.Ln, [smb], [smb])
            act(sm[:, 3:4], sm[:, 2:3], AF.Exp, [smb], [smb], scale=-0.5)
            stt(yt[:], yt[:], sm[:, 3:4], gpost1[:], ALU.mult, ALU.mult, [yb, smb, gpost_b], [yb])
            xt, xb = xt_ring.next()
            dma("sp", xt[:], xs[ti * 128:(ti + 1) * 128, :], (), [xb])
            tt("pool", xt[:], xt[:], yt[:], ALU.add, [xb, yb], [xb])
            dma("sp", outs[ti * 128:(ti + 1) * 128, :], xt[:], [xb], [outD_b[ti]])
            stA = norm_to_hT(xt[:], xb, ti, gpre2)
            if pend is not None:
                norm_B(pend)
            pend = stA
        norm_B(pend)
        P.barrier()

        dma("sp", gpost2[:], gpost2_d.partition_broadcast(128), (), [gpost_b])
        yacc = aview(0, 32768, F32).rearrange("p (a b) -> p a b", b=1024)
        yacc_b = [[Buf("yacc%d_%d" % (i, hf)) for hf in range(2)] for i in range(NT)]
        uT = [aview(32768 + i * 4096, 4096).rearrange("p (a b) -> p a b", b=T) for i in range(2)]
        uT_b = [[[Buf("uT%d_%d_%d" % (i, f, c)) for c in range(4)] for f in range(2)] for i in range(2)]
        def mlp_up(fg):
            wu, wu_b = load_w("pool", w_up[fg, :, :], 2048)
            wu3 = wu[:, 0:2048].rearrange("p (a b) -> p a b", b=256)
            ub = fg % 2
            for fc in range(2):
                for c in range(4):
                    cs = slice(c * 512, (c + 1) * 512)
                    bank, bb = pj_ring.next()
                    for kc in range(8):
                        mm(bank[:, :], wu3[:, kc, fc * 128:(fc + 1) * 128], hT[:, kc, cs], kc == 0, kc == 7,
                           [wu_b, hT_b[kc][c]], [bb])
                    r, rb = t1_ring.next()
                    act(r[:], bank[:, :], AF.Relu, [bb], [rb])
                    act(uT[ub][:, fc, cs], r[:], AF.Square, [rb], [uT_b[ub][fc][c]])

        mlp_up(0)
        for fg in range(16):
            wd, wd_b = load_w("pool", w_dn[fg, :, :], 2048)
            wd3 = wd[:, 0:2048].rearrange("p (a b) -> p a b", b=1024)
            ub = fg % 2
            if fg + 1 < 16:
                mlp_up(fg + 1)
            for ti in range(NT):
                c = ti // 4
                for hf in range(2):
                    bank, bb = y_ring.next()
                    for fc in range(2):
                        mm(bank[:, :], uT[ub][:, fc, ti * 128:(ti + 1) * 128], wd3[:, fc, hf * 512:(hf + 1) * 512],
                           fc == 0, fc == 1, [uT_b[ub][fc][c], wd_b], [bb])
                    ya = yacc[:, ti, hf * 512:(hf + 1) * 512]
                    if fg == 0:
                        cp("act", ya, bank[:, :], [bb], [yacc_b[ti][hf]])
                    else:
                        tt("dve", ya, bank[:, :], ya, ALU.add, [bb, yacc_b[ti][hf]], [yacc_b[ti][hf]])
        for ti in range(NT):
            sm, smb = sm_ring.next()
            act(junk[:], yacc[:, ti, :], AF.Square, yacc_b[ti], [junk_b, smb], accum_out=sm[:, 0:1])
            ts("dve", sm[:, 1:2], sm[:, 0:1], 1.0 / D, 1e-6, ALU.mult, ALU.add, [smb], [smb])
            act(sm[:, 2:3], sm[:, 1:2], AF.Ln, [smb], [smb])
            act(sm[:, 3:4], sm[:, 2:3], AF.Exp, [smb], [smb], scale=-0.5)
            yt, yb = yt_ring.next()
            stt(yt[:], yacc[:, ti, :], sm[:, 3:4], gpost2[:], ALU.mult, ALU.mult, yacc_b[ti] + [smb, gpost_b], [yb])
            xt, xb = xt_ring.next()
            dma("sp", xt[:], outs[ti * 128:(ti + 1) * 128, :], [outD_b[ti]], [xb])
            tt("pool", xt[:], xt[:], yt[:], ALU.add, [xb, yb], [xb])
            dma("sp", outs[ti * 128:(ti + 1) * 128, :], xt[:], [xb], [outD_b[ti]], final=True)
        P.barrier()

    P.finalize()
    P.emit(nc, es)
    es.close()
    return nc


def _blk(w, cols):
    sub = w[:, cols]
    n = sub.shape[1]
    return np.ascontiguousarray(sub.reshape(8, 128, n).transpose(1, 0, 2).reshape(128, 8 * n))


def _rowblk(w, nkc):
    n = w.shape[1]
    return np.ascontiguousarray(w.reshape(nkc, 128, n).transpose(1, 0, 2).reshape(128, nkc * n))


def _const_tables():
    f32 = np.float32
    inv = np.power(f32(10000.0), -np.arange(0, 64, 2, dtype=f32) / f32(64)).astype(f32)
    ang = (np.arange(T, dtype=f32)[:, None] * inv[None, :]).astype(f32)
    cos = np.cos(ang).astype(f32).T
    sin = np.sin(ang).astype(f32).T
    rope = np.zeros((4, 128, 1024), f32)
    for r in range(128):
        j = r % 32
        sgn = -1.0 if (r % 64) < 32 else 1.0
        for c in range(4):
            rope[c, r, 0:512] = cos[j, c * 512:(c + 1) * 512]
            rope[c, r, 512:1024] = sgn * sin[j, c * 512:(c + 1) * 512]
    n = np.arange(128)[:, None]
    t = np.arange(T)[None, :]
    cmpmask = np.where((16 * n + 31 <= t) & (n < 127), 0.0, NEGM).astype(f32)
    tt_ = np.arange(T)
    cur = tt_ // 64
    jj = np.arange(32)[None, :]
    forced = (jj == 0) | (jj == cur[:, None]) | (jj == cur[:, None] - 1)
    valid = jj <= cur[:, None]
    fb = np.where(valid, 1e4 * forced.astype(f32), -1e30).astype(f32)
    fbt = np.ascontiguousarray(fb.reshape(16, 128, 32).transpose(1, 0, 2).reshape(128, 512))
    sI = np.arange(128)[:, None]
    tI = np.arange(128)[None, :]
    tri = np.concatenate([np.where(sI > tI, NEGM, 0.0), np.where(tI >= sI, NEGM, 0.0)], axis=1).astype(f32)
    ident = np.eye(128, dtype=f32)
    umat = (sI <= tI).astype(f32)
    ci = np.arange(128)[:, None] * 16
    sj = np.arange(32)[None, :] * 64
    ov = (((ci < sj + 64) & (ci + 32 > sj)) & (np.arange(128)[:, None] < 127)).astype(f32)
    erows = (np.arange(32)[:, None] == (np.arange(T)[None, :] // 64)).astype(f32)
    return dict(rope=rope, cmpmask=cmpmask, fbt=fbt, tri=tri, ident=ident, umat=umat, ov=ov, erows=erows)


def _layout_weights(inp):
    f = lambda a: np.asarray(a, dtype=np.float32)
    w_in = f(inp["w_in"])[0]
    o = {}
    ar = np.arange
    FQ, FK, FV, FF, NQ = 0, 512, 1024, 1536, 1544
    KCo, VCo, KSo, VSo, KWo, VWo, NG, GA, GB = 2056, 2184, 2312, 2440, 2568, 2696, 2824, 2848, 3872
    o["w_fqk"] = np.stack([_blk(w_in, FQ + 128 * p + ar(128)) for p in range(4)] +
                          [_blk(w_in, FK + 128 * p + ar(128)) for p in range(4)])
    o["w_fv"] = np.stack([_blk(w_in, FV + 256 * hf + ar(256)) for hf in range(2)])
    o["w_ffng"] = _blk(w_in, np.concatenate([FF + ar(8), NG + ar(24)]))
    sw = (ar(64) + 32) % 64
    nq_plain, nq_swap = [], []
    for p in range(4):
        nq_plain.append(_blk(w_in, np.concatenate([NQ + 64 * p + ar(64), NQ + 64 * (4 + p) + ar(64)])))
        nq_swap.append(_blk(w_in, np.concatenate([NQ + 64 * p + sw, NQ + 64 * (4 + p) + sw])))
    o["w_nq"] = np.stack(nq_plain + nq_swap)
    sw2 = np.concatenate([sw, 64 + sw])
    o["w_nk"] = np.stack([_blk(w_in, KCo + ar(128)), _blk(w_in, KCo + sw2),
                          _blk(w_in, KSo + ar(128)), _blk(w_in, KSo + sw2),
                          _blk(w_in, KWo + ar(128)), _blk(w_in, KWo + sw2),
                          _blk(w_in, VCo + ar(128))])
    o["w_nv"] = _blk(w_in, np.concatenate([VSo + ar(128), VWo + ar(128)]))
    o["w_gab"] = np.stack([_blk(w_in, GA + 128 * c + ar(128)) for c in range(8)] +
                          [_blk(w_in, GB + 128 * c + ar(128)) for c in range(8)])
    wfo = f(inp["w_fox_out"])[0]
    wno = f(inp["w_nsa_out"])[0]
    o["w_fo"] = np.stack([_rowblk(wfo[:, 128 * c:128 * (c + 1)], 4) for c in range(8)])
    o["w_no"] = np.stack([_rowblk(wno[:, 128 * c:128 * (c + 1)], 4) for c in range(8)])
    wo = f(inp["w_o"])[0]
    o["w_o"] = np.stack([_rowblk(wo[:, 512 * h:512 * (h + 1)], 8) for h in range(2)])
    wup = f(inp["w_up"])[0]
    wdn = f(inp["w_down"])[0]
    o["w_up"] = np.stack([_blk(wup, 256 * g + ar(256)) for g in range(16)])
    o["w_dn"] = np.stack([_rowblk(wdn[256 * g:256 * (g + 1), :], 2) for g in range(16)])
    for nm, key in (("w1k", "cmp_w1_k"), ("w1v", "cmp_w1_v")):
        w1 = f(inp[key])[0].reshape(32, 64, 64).transpose(1, 0, 2).reshape(64, 2048)
        o[nm] = np.ascontiguousarray(np.concatenate([w1, w1], axis=0))
    o["w2k"] = np.ascontiguousarray(f(inp["cmp_w2_k"])[0])
    o["w2v"] = np.ascontiguousarray(f(inp["cmp_w2_v"])[0])
    o["posk"] = np.ascontiguousarray(f(inp["cmp_pos_k"])[0].T)
    o["posv"] = np.ascontiguousarray(f(inp["cmp_pos_v"])[0].T)
    o["gpre1"] = np.ascontiguousarray(f(inp["norm_mix_pre"])[0].reshape(8, 128).T)
    o["gpre2"] = np.ascontiguousarray(f(inp["norm_mlp_pre"])[0].reshape(8, 128).T)
    o["gpost1"] = np.ascontiguousarray(f(inp["norm_mix_post"])[0].reshape(1, 1024))
    o["gpost2"] = np.ascontiguousarray(f(inp["norm_mlp_post"])[0].reshape(1, 1024))
    o["bfor"] = np.ascontiguousarray(f(inp["b_forget"])[0].reshape(1, 8))
    o.update(_const_tables())
    return o


_NC_CACHE = {}


def kernel(**inputs):
    x = np.asarray(inputs["x"], dtype=np.float32)
    B = x.shape[0]
    per = B // NCORES
    shared = _layout_weights(inputs)
    if per not in _NC_CACHE:
        _NC_CACHE[per] = build_program(per)
    nc = _NC_CACHE[per]
    in_maps = []
    for c in range(NCORES):
        m = dict(shared)
        m["x"] = np.ascontiguousarray(x[c * per:(c + 1) * per].reshape(per * T, D))
        in_maps.append(m)
    res = run_bass_kernel_spmd(nc, in_maps, core_ids=list(range(NCORES)))
    outs = [np.asarray(r["out"]).reshape(per, T, D) for r in res.results]
    return np.concatenate(outs, axis=0).astype(np.float32)
```
